# Optimizing a Trainium2 kernel written in Bass

```python
import jax, jax.numpy as jnp
from jax import lax
import numpy as np

D_MODEL = 2048
BATCH = 16
SEQ = 2048
DEPTH = 2

N_MIXERS = 2
N_EVEN = (DEPTH + 1) // 2
N_ODD = DEPTH // 2
CONV_WIDTH = 3
HEAD_DIM = 128
N_HEADS = D_MODEL // HEAD_DIM
Q_BLOCK = 128
D_FF = 5632
N_EXPERTS = 8
TOP_K = 2
D_FF_EXPERT = (7 * D_MODEL) // 2
N_MOD = 6
EPS = 1e-6

kernel_name = "hybrid_shortconv_fox_moe_adaln"


def rms_norm(x, g):
    xf = x.astype(jnp.float32)
    n = xf * lax.rsqrt(jnp.mean(xf * xf, axis=-1, keepdims=True) + EPS)
    return (n * g.astype(jnp.float32)).astype(x.dtype)


def modulate(n, shift, scale):
    return n * (1 + scale[:, None, :]) + shift[:, None, :]


def short_conv_mixer(h, w_in, conv_w, w_out):
    bcx = h @ w_in
    b_gate, c_gate, xv = jnp.split(bcx, 3, axis=-1)
    u = c_gate * xv
    conv = lax.conv_general_dilated(
        u, conv_w[:, None, :].astype(u.dtype),
        window_strides=(1,),
        padding=((CONV_WIDTH - 1, 0),),
        dimension_numbers=("NWC", "WIO", "NWC"),
        feature_group_count=D_MODEL)
    return (b_gate * conv) @ w_out


def forgetting_attention(h, w_in, b_f, w_out):
    bsz, seq, _ = h.shape
    proj = h @ w_in
    q, k, v, f_logit = jnp.split(proj, [D_MODEL, 2 * D_MODEL, 3 * D_MODEL], axis=-1)
    q = q.reshape(bsz, seq, N_HEADS, HEAD_DIM)
    k = k.reshape(bsz, seq, N_HEADS, HEAD_DIM)
    v = v.reshape(bsz, seq, N_HEADS, HEAD_DIM)
    log_f = jax.nn.log_sigmoid((f_logit + b_f).astype(jnp.float32))
    cum = jnp.cumsum(log_f, axis=1).transpose(0, 2, 1)
    n_blocks = seq // Q_BLOCK
    q_blocks = q.reshape(bsz, n_blocks, Q_BLOCK, N_HEADS, HEAD_DIM).transpose(1, 0, 3, 2, 4)
    cum_q = cum.reshape(bsz, N_HEADS, n_blocks, Q_BLOCK).transpose(2, 0, 1, 3)
    k_pos = jnp.arange(seq)
    scale = HEAD_DIM ** -0.5

    def block(args):
        qi, cqi, bi = args
        s = jnp.einsum("bhqd,bshd->bhqs", qi, k,
                       preferred_element_type=jnp.float32) * scale
        s = s + cqi[..., None] - cum[:, :, None, :]
        q_pos = bi * Q_BLOCK + jnp.arange(Q_BLOCK)
        causal = k_pos[None, :] <= q_pos[:, None]
        s = jnp.where(causal[None, None], s, -jnp.inf)
        p = jax.nn.softmax(s, axis=-1).astype(v.dtype)
        return jnp.einsum("bhqs,bshd->bqhd", p, v)

    out = lax.map(block, (q_blocks, cum_q, jnp.arange(n_blocks)))
    out = out.transpose(1, 0, 2, 3, 4).reshape(bsz, seq, D_MODEL)
    return out @ w_out


def swiglu(t, w1, w3, w2):
    return (jax.nn.silu(t @ w1) * (t @ w3)) @ w2


def moe_swiglu(h, w_router, w1, w3, w2):
    bsz, seq, d = h.shape
    t = h.reshape(-1, d)
    logits = (t @ w_router).astype(jnp.float32)
    top_vals, top_idx = lax.top_k(logits, TOP_K)
    top_w = jax.nn.softmax(top_vals, axis=-1)
    gates = jnp.sum(jax.nn.one_hot(top_idx, N_EXPERTS, dtype=jnp.float32)
                    * top_w[..., None], axis=1).astype(t.dtype)
    out = jnp.zeros_like(t)
    for e in range(N_EXPERTS):
        out = out + gates[:, e:e + 1] * swiglu(t, w1[e], w3[e], w2[e])
    return out.reshape(bsz, seq, d)


def setup_inputs(seed: int = 0) -> dict:
    key = jax.random.key(seed)
    ks = jax.random.split(key, 24)
    f32 = jnp.float32
    D = D_MODEL

    def nrm(k, shape, fan_in):
        return jax.random.normal(k, shape, f32) * (fan_in ** -0.5)

    def gain(k, shape):
        return 1.0 + 0.05 * jax.random.normal(k, shape, f32)

    return {
        "x": jax.random.normal(ks[0], (BATCH, SEQ, D), f32),
        "c": jax.random.normal(ks[1], (BATCH, D), f32),
        "ada_w": nrm(ks[2], (DEPTH, D, N_MOD * D), D),
        "ada_b": 0.02 * jax.random.normal(ks[3], (DEPTH, N_MOD * D), f32),
        "norm_mix": gain(ks[4], (DEPTH, D)),
        "norm_ffn": gain(ks[5], (DEPTH, D)),
        "norm_final": gain(ks[6], (D,)),
        "conv_w_in": nrm(ks[7], (N_EVEN, D, 3 * D), D),
        "conv_kernel": nrm(ks[8], (N_EVEN, CONV_WIDTH, D), CONV_WIDTH),
        "conv_w_out": nrm(ks[9], (N_EVEN, D, D), D),
        "fox_w_in": nrm(ks[10], (N_ODD, D, 3 * D + N_HEADS), D),
        "fox_b_f": jax.random.uniform(ks[11], (N_ODD, N_HEADS), f32, 1.0, 4.0),
        "fox_w_out": nrm(ks[12], (N_ODD, D, D), D),
        "ffn_w1": nrm(ks[13], (N_EVEN, D, D_FF), D),
        "ffn_w3": nrm(ks[14], (N_EVEN, D, D_FF), D),
        "ffn_w2": nrm(ks[15], (N_EVEN, D_FF, D), D_FF),
        "moe_router": nrm(ks[16], (N_ODD, D, N_EXPERTS), D),
        "moe_w1": nrm(ks[17], (N_ODD, N_EXPERTS, D, D_FF_EXPERT), D),
        "moe_w3": nrm(ks[18], (N_ODD, N_EXPERTS, D, D_FF_EXPERT), D),
        "moe_w2": nrm(ks[19], (N_ODD, N_EXPERTS, D_FF_EXPERT, D), D_FF_EXPERT),
    }


def reference(x, c, ada_w, ada_b, norm_mix, norm_ffn, norm_final,
              conv_w_in, conv_kernel, conv_w_out,
              fox_w_in, fox_b_f, fox_w_out,
              ffn_w1, ffn_w3, ffn_w2,
              moe_router, moe_w1, moe_w3, moe_w2):
    c_act = jax.nn.silu(c)
    for i in range(DEPTH):
        j = i // 2
        mod = c_act @ ada_w[i] + ada_b[i]
        sh1, sc1, g1, sh2, sc2, g2 = jnp.split(mod, N_MOD, axis=-1)
        hn = modulate(rms_norm(x, norm_mix[i]), sh1, sc1)
        if i % N_MIXERS == 0:
            y = short_conv_mixer(hn, conv_w_in[j], conv_kernel[j], conv_w_out[j])
        else:
            y = forgetting_attention(hn, fox_w_in[j], fox_b_f[j], fox_w_out[j])
        x = x + g1[:, None, :] * y
        hn = modulate(rms_norm(x, norm_ffn[i]), sh2, sc2)
        if i % 2 == 0:
            y = swiglu(hn, ffn_w1[j], ffn_w3[j], ffn_w2[j])
        else:
            y = moe_swiglu(hn, moe_router[j], moe_w1[j], moe_w3[j], moe_w2[j])
        x = x + g2[:, None, :] * y
    return rms_norm(x, norm_final)
```

```python
import numpy as np
from contextlib import ExitStack

import concourse.bass as bass
import concourse.mybir as mybir
from concourse.bass_utils import run_bass_kernel_spmd

F32 = mybir.dt.float32
BF16 = mybir.dt.bfloat16
AF = mybir.ActivationFunctionType
ALU = mybir.AluOpType
AX = mybir.AxisListType

D = 2048
KC = 16
HD = 128
NH = 16
EPS = 1e-6
SEM_LIMIT = 12000
G = 512


class Buf:
    def __init__(self, kb, t, name):
        self.kb = kb
        self.t = t
        self.name = name
        self.w = {}
        self.r = {}
        self.prev = []
        self.dsem = None
        self.dcount = 0

    def new_gen(self):
        self.prev = list(self.w.values()) + list(self.r.values())
        self.w = {}
        self.r = {}

    def dma_sig(self, ins):
        if self.dsem is None:
            self.dsem = self.kb.new_sem("d_" + self.name)
        self.dcount += 16
        ins.then_inc(self.dsem, 16)
        return (self.dsem, self.dcount)


class Eng:
    def __init__(self, kb, name, eng):
        self.kb = kb
        self.name = name
        self.eng = eng
        self.sem = None
        self.count = 0
        self.nsem = 0
        self.waited = {}

    def sig(self, ins):
        if self.sem is None or self.count >= SEM_LIMIT:
            self.sem = self.kb.new_sem(f"e_{self.name}{self.nsem}")
            self.nsem += 1
            self.count = 0
        self.count += 1
        ins.then_inc(self.sem, 1)
        return (self.sem, self.count)

    def wait(self, tok):
        if tok is None:
            return
        sem, val = tok
        key = id(sem)
        if self.waited.get(key, 0) >= val:
            return
        self.eng.wait_ge(sem, val)
        self.waited[key] = val

    def waits(self, toks):
        for t in toks:
            self.wait(t)

    def wdep(self, buf):
        self.waits(buf.prev)

    def rdep(self, buf):
        self.waits(buf.w.values())


class Ring:
    def __init__(self, bufs):
        self.bufs = bufs
        self.i = 0

    def next(self):
        b = self.bufs[self.i % len(self.bufs)]
        self.i += 1
        return b


class KB:
    def __init__(self, NSEQ=2, S=2048, NE=8, DFF=5632, DFFE=7168, phases=("ada", "conv", "ffn", "attn", "moe", "final"),
                 debug=False, moe_sparse=True):
        self.moe_sparse = moe_sparse
        self.NSEQ, self.S, self.NE, self.DFF, self.DFFE = NSEQ, S, NE, DFF, DFFE
        self.T = NSEQ * S
        self.NT = self.T // 128
        self.phases = phases
        self.debug = debug
        self.nc = bass.Bass("TRN2", target_bir_lowering=False)
        self.es = ExitStack()
        self.sems = []
        self.dma_toks = []
        nc = self.nc
        self.PE = Eng(self, "pe", nc.tensor)
        self.ACT = Eng(self, "act", nc.scalar)
        self.DVE = Eng(self, "dve", nc.vector)
        self.POOL = Eng(self, "pool", nc.gpsimd)
        self.SP = Eng(self, "sp", nc.sync)
        self.engs = [self.PE, self.ACT, self.DVE, self.POOL, self.SP]

    def new_sem(self, name):
        s = self.es.enter_context(self.nc.semaphore(name))
        self.sems.append(s)
        return s

    def sb(self, name, shape, dtype, stack=None):
        self.uid = getattr(self, "uid", 0) + 1
        name = f"{name}_{self.uid}"
        t = (stack or self.es).enter_context(self.nc.sbuf_tensor(name, shape, dtype))
        return Buf(self, t, name)

    def dram(self, name, shape, dtype, kind="Internal"):
        t = self.nc.dram_tensor(name, shape, dtype, kind=kind)
        return t.ap()

    def barrier(self):
        nc = self.nc
        toks = []
        toks.append(self.DVE.sig(nc.vector.memset(self.bar_dve.t[:], 0.0)))
        toks.append(self.ACT.sig(nc.scalar.copy(out=self.bar_act.t[:], in_=self.bar_src.t[:])))
        toks.append(self.POOL.sig(nc.gpsimd.memset(self.bar_pool.t[:], 0.0)))
        if self.last_pe_tok is not None:
            toks.append(self.last_pe_tok)
        toks += self.dma_toks
        self.dma_toks = []
        for e in self.engs:
            e.waits(toks)

    def track_dma(self, tok):
        self.dma_toks.append(tok)
        if len(self.dma_toks) > 64:
            last = {}
            for s, v in self.dma_toks:
                if id(s) not in last or last[id(s)][1] < v:
                    last[id(s)] = (s, v)
            self.dma_toks = list(last.values())

    def build(self):
        nc = self.nc
        NSEQ, T, NE = self.NSEQ, self.T, self.NE
        self.last_pe_tok = None
        P = self.phases
        self.input_names = []

        def inp(name, shape, need=True):
            if not need:
                return None
            self.input_names.append(name)
            return self.dram(name, shape, F32, "ExternalInput")

        self.x_in = inp("x", [T, D])
        self.c_in = inp("c", [NSEQ, D], "ada" in P)
        self.ada_w = inp("ada_w", [2, D, 6 * D], "ada" in P)
        self.ada_b = inp("ada_b", [2, 6 * D], "ada" in P)
        self.norm_mix = inp("norm_mix", [2, D], "ada" in P)
        self.norm_ffn = inp("norm_ffn", [2, D], "ada" in P)
        self.norm_final = inp("norm_final", [D], "final" in P)
        self.conv_w_in = inp("conv_w_in", [48, 128, D], "conv" in P)
        self.conv_kernel = inp("conv_kernel", [3, D], "conv" in P)
        self.conv_w_out = inp("conv_w_out", [D, D], "conv" in P)
        self.fox_qk = inp("fox_qk", [32, 128, D], "attn" in P)
        self.fox_v = inp("fox_v", [D, D], "attn" in P)
        self.fox_f = inp("fox_f", [D, NH], "attn" in P)
        self.fox_b_f = inp("fox_b_f", [NH], "attn" in P)
        self.fox_w_out = inp("fox_w_out", [D, D], "attn" in P)
        self.ffn_w1 = inp("ffn_w1", [self.DFF // 128, 128, D], "ffn" in P)
        self.ffn_w3 = inp("ffn_w3", [self.DFF // 128, 128, D], "ffn" in P)
        self.ffn_w2 = inp("ffn_w2", [self.DFF, D], "ffn" in P)
        self.moe_router = inp("moe_router", [D, NE], "moe" in P)
        self.moe_w1 = inp("moe_w1", [NE * (self.DFFE // 128), 128, D], "moe" in P)
        self.moe_w3 = inp("moe_w3", [NE * (self.DFFE // 128), 128, D], "moe" in P)
        self.moe_w2 = inp("moe_w2", [NE * self.DFFE, D], "moe" in P)
        self.out = self.dram("out", [T, D], F32, "ExternalOutput")
        self.xres = self.dram("xres", [T, D], F32, "ExternalOutput" if self.debug else "Internal")
        self.mod_d = self.dram("mod_d", [2, NSEQ, 6 * D], F32, "ExternalOutput" if self.debug else "Internal")
        self.xres_tok = {}
        self.x0_is_input = True

        self.ident_bf = self.sb("ident_bf", [128, 128], BF16)
        self.ident_f = self.sb("ident_f", [128, 128], F32)
        self.bar_dve = self.sb("bar_dve", [128, 1], F32)
        self.bar_act = self.sb("bar_act", [128, 1], F32)
        self.bar_pool = self.sb("bar_pool", [128, 1], F32)
        self.bar_src = self.sb("bar_src", [128, 1], F32)
        self.eps_col = self.sb("eps_col", [128, 1], F32)
        self.wring = Ring([self.sb(f"w{i}", [128, D], BF16) for i in range(8)])
        self.modcols = self.sb("modcols", [128, 2 * 2 * NSEQ * 2, KC], F32)
        self.fin_g = self.sb("fin_g", [128, D], F32)
        self.banks = []
        for i in range(8):
            t = self.es.enter_context(nc.psum_tensor(f"bank{i}", [128, 512], F32))
            self.banks.append(Buf(self, t, f"bank{i}"))
        self.bank_i = 0

        for idt in (self.ident_bf, self.ident_f):
            nc.gpsimd.memset(idt.t[:], 0.0)
            ins = nc.gpsimd.affine_select(out=idt.t[:], in_=idt.t[:], pattern=[[-1, 128]],
                                          compare_op=ALU.not_equal, fill=1.0, base=0, channel_multiplier=1)
            idt.w["pool"] = self.POOL.sig(ins)
        nc.gpsimd.memset(self.eps_col.t[:], EPS)
        ins = nc.gpsimd.memset(self.bar_src.t[:], 0.0)
        self.bar_src.w["pool"] = self.POOL.sig(ins)
        self.eps_col.w["pool"] = self.bar_src.w["pool"]
        self.ACT.rdep(self.bar_src)
        self.ACT.rdep(self.eps_col)

        if "ada" in self.phases:
            self.phase_ada()
            self.barrier()
        if "conv" in self.phases:
            self.phase_conv()
            self.barrier()
        if "ffn" in self.phases:
            self.phase_ffn()
            self.barrier()
        if "attn" in self.phases:
            self.phase_attn()
            self.barrier()
        if "moe" in self.phases:
            if self.moe_sparse:
                self.phase_moe_sparse()
            else:
                self.phase_moe()
            self.barrier()
        if "final" in self.phases:
            self.phase_final()
        self.barrier()
        self.es.close()
        return nc

    def next_bank(self):
        b = self.banks[self.bank_i % 8]
        self.bank_i += 1
        return b

    def wload(self, src_ap, cols=D):
        b = self.wring.next()
        b.new_gen()
        self.POOL.wdep(b)
        if isinstance(src_ap, tuple):
            table, idx_ap, idx_buf = src_ap
            self.POOL.rdep(idx_buf)
            ins = self.nc.gpsimd.indirect_dma_start(out=b.t[:, 0:cols], out_offset=None, in_=table,
                                                    in_offset=bass.IndirectOffsetOnAxis(ap=idx_ap, axis=0))
            idx_buf.r["pool_dma"] = None
        else:
            ins = self.nc.gpsimd.dma_start(out=b.t[:, 0:cols], in_=src_ap)
        b.w["dma"] = b.dma_sig(ins)
        if isinstance(src_ap, tuple):
            src_ap[2].r["wdma" + b.name] = b.w["dma"]
        self.track_dma(b.w["dma"])
        return b

    def pe_sig(self, ins):
        tok = self.PE.sig(ins)
        self.last_pe_tok = tok
        return tok

    def modcol(self, layer, sub, b):
        idx = ((layer * 2 + sub) * self.NSEQ + b) * 2
        return self.modcols.t[:, idx, :], self.modcols.t[:, idx + 1, :]

    def phase_ada(self):
        nc = self.nc
        NSEQ = self.NSEQ
        PE, ACT, DVE, POOL, SP = self.PE, self.ACT, self.DVE, self.POOL, self.SP
        with ExitStack() as st:
            cT = self.sb("cT", [128, KC, NSEQ], F32, st)
            cTa = self.sb("cTa", [128, KC, NSEQ], BF16, st)
            bias = Ring([self.sb(f"adab{i}", [NSEQ, D], F32, st) for i in range(2)])
            mrow = Ring([self.sb(f"mrow{i}", [NSEQ, D], F32, st) for i in range(2)])
            cols = self.sb("adacols", [128, 3, KC], F32, st)
            with nc.allow_non_contiguous_dma(reason="tiny transposed load of conditioning vector"):
                for b in range(NSEQ):
                    ins = nc.sync.dma_start(out=cT.t[:, :, b], in_=self.c_in[b, :].rearrange("(k p) -> p k", p=128))
                    cT.w["dma"] = cT.dma_sig(ins)
            ACT.rdep(cT)
            ins = nc.scalar.activation(out=cTa.t[:], in_=cT.t[:], func=AF.Silu)
            cTa.w["act"] = ACT.sig(ins)
            mod_store_toks = []
            for layer in range(2):
                for n in range(6):
                    bt = bias.next()
                    bt.new_gen()
                    SP.wdep(bt)
                    for b in range(NSEQ):
                        ins = nc.sync.dma_start(out=bt.t[b:b + 1, :], in_=self.ada_b[layer:layer + 1, n * D:(n + 1) * D])
                        bt.w["dma"] = bt.dma_sig(ins)
                    bks = [self.next_bank() for _ in range(4)]
                    for bk in bks:
                        bk.new_gen()
                        PE.wdep(bk)
                    PE.rdep(cTa)
                    for k in range(KC):
                        wb = self.wload(self.ada_w[layer, k * 128:(k + 1) * 128, n * D:(n + 1) * D])
                        PE.rdep(wb)
                        for j in range(4):
                            ins = nc.tensor.matmul(bks[j].t[0:NSEQ, :], lhsT=cTa.t[:, k, :], rhs=wb.t[:, j * 512:(j + 1) * 512],
                                                   start=(k == 0), stop=(k == KC - 1))
                        wb.r["pe"] = self.pe_sig(ins)
                    for bk in bks:
                        bk.w["pe"] = wb.r["pe"]
                    mr = mrow.next()
                    mr.new_gen()
                    DVE.wdep(mr)
                    DVE.rdep(bt)
                    for j in range(4):
                        DVE.rdep(bks[j])
                        ins = nc.vector.tensor_tensor(out=mr.t[:, j * 512:(j + 1) * 512], in0=bks[j].t[0:NSEQ, :],
                                                      in1=bt.t[:, j * 512:(j + 1) * 512], op=ALU.add)
                        tok = DVE.sig(ins)
                        bks[j].r["dve"] = tok
                    mr.w["dve"] = tok
                    bt.r["dve"] = tok
                    SP.rdep(mr)
                    ins = nc.sync.dma_start(out=self.mod_d[layer, :, n * D:(n + 1) * D], in_=mr.t[:])
                    tok = mr.dma_sig(ins)
                    mr.r["dma"] = tok
                    mod_store_toks.append(tok)
                    self.track_dma(tok)
            SP.waits(mod_store_toks)
            with nc.allow_non_contiguous_dma(reason="tiny transposed loads of modulation vectors"):
                for layer in range(2):
                    for sub in range(2):
                        gsrc = (self.norm_mix if sub == 0 else self.norm_ffn)[layer, :]
                        for b in range(NSEQ):
                            cols.new_gen()
                            SP.wdep(cols)
                            srcs = [gsrc, self.mod_d[layer, b, (3 * sub + 1) * D:(3 * sub + 2) * D],
                                    self.mod_d[layer, b, (3 * sub) * D:(3 * sub + 1) * D]]
                            for i, s_ in enumerate(srcs):
                                ins = nc.sync.dma_start(out=cols.t[:, i, :], in_=s_.rearrange("(k p) -> p k", p=128))
                                cols.w["dma"] = cols.dma_sig(ins)
                            self.track_dma(cols.w["dma"])
                            A, B = self.modcol(layer, sub, b)
                            DVE.rdep(cols)
                            ins = nc.vector.scalar_tensor_tensor(out=A, in0=cols.t[:, 1, :], scalar=1.0, in1=cols.t[:, 0, :],
                                                                 op0=ALU.add, op1=ALU.mult)
                            DVE.sig(ins)
                            ins = nc.vector.tensor_copy(out=B, in_=cols.t[:, 2, :])
                            tok = DVE.sig(ins)
                            cols.r["dve"] = tok
                            self.modcols.w["dve"] = tok
            self.mod_ready = tok

    def make_prep_bufs(self, st):
        self.xt_ring = Ring([self.sb(f"xt{i}", [128, D], F32, st) for i in range(2)])
        self.xs_ring = Ring([self.sb(f"xs{i}", [128, D], BF16, st) for i in range(2)])
        self.junk = self.sb("junk", [128, D], BF16, st)
        self.ss_ring = Ring([self.sb(f"ss{i}", [128, 2], F32, st) for i in range(4)])

    def prep(self, tile, A, B, dstT, col0, src=None):
        nc = self.nc
        PE, ACT, DVE, POOL, SP = self.PE, self.ACT, self.DVE, self.POOL, self.SP
        xt = self.xt_ring.next()
        xt.new_gen()
        SP.wdep(xt)
        if self.x0_is_input:
            srcap = self.x_in[tile * 128:(tile + 1) * 128, :]
        else:
            srcap = self.xres[tile * 128:(tile + 1) * 128, :]
            SP.waits([self.xres_tok.get((tile, 0)), self.xres_tok.get((tile, 1))])
        ins = nc.sync.dma_start(out=xt.t[:], in_=srcap)
        xt.w["dma"] = xt.dma_sig(ins)
        self.track_dma(xt.w["dma"])
        ss = self.ss_ring.next()
        ss.new_gen()
        self.junk.new_gen()
        ACT.wdep(ss)
        ACT.wdep(self.junk)
        ACT.rdep(xt)
        ins = nc.scalar.memzero(ss.t[:])
        ACT.wait(ACT.sig(ins))
        ins = nc.scalar.activation(out=self.junk.t[:], in_=xt.t[:], func=AF.Square, accum_out=ss.t[:, 0:1])
        tok = ACT.sig(ins)
        ss.w["act"] = tok
        self.junk.w["act"] = tok
        xt.r["act"] = tok
        ACT.wait(tok)
        ins = nc.scalar.activation(out=ss.t[:, 1:2], in_=ss.t[:, 0:1], func=AF.Sqrt, scale=1.0 / D, bias=self.eps_col.t[:, 0:1])
        ss.w["act"] = ACT.sig(ins)
        DVE.rdep(ss)
        ins = nc.vector.reciprocal(out=ss.t[:, 1:2], in_=ss.t[:, 1:2])
        DVE.wait(DVE.sig(ins))
        xs = self.xs_ring.next()
        xs.new_gen()
        DVE.wdep(xs)
        DVE.rdep(xt)
        ins = nc.vector.tensor_scalar(out=xs.t[:], in0=xt.t[:], scalar1=ss.t[:, 1:2], scalar2=None, op0=ALU.mult)
        tok = DVE.sig(ins)
        xs.w["dve"] = tok
        xt.r["dve"] = tok
        ss.r["dve"] = tok
        PE.rdep(xs)
        PE.rdep(self.ident_bf)
        for j in range(4):
            bk = self.next_bank()
            bk.new_gen()
            PE.wdep(bk)
            pv = bk.t[:].bitcast(BF16)
            for i in range(4):
                k = 4 * j + i
                ins = nc.tensor.transpose(out=pv[:, i * 128:(i + 1) * 128], in_=xs.t[:, k * 128:(k + 1) * 128],
                                          identity=self.ident_bf.t[:])
            tok = self.pe_sig(ins)
            bk.w["pe"] = tok
            xs.r["pe"] = tok
            E = ACT if j % 2 == 0 else DVE
            E.rdep(bk)
            E.wdep(dstT)
            E.rdep(self.modcols)
            for i in range(4):
                k = 4 * j + i
                if E is ACT:
                    ins = nc.scalar.activation(out=dstT.t[:, k, col0:col0 + 128], in_=pv[:, i * 128:(i + 1) * 128],
                                               func=AF.Identity, scale=A[:, k:k + 1], bias=B[:, k:k + 1])
                else:
                    ins = nc.vector.tensor_scalar(out=dstT.t[:, k, col0:col0 + 128], in0=pv[:, i * 128:(i + 1) * 128],
                                                  scalar1=A[:, k:k + 1], scalar2=B[:, k:k + 1], op0=ALU.mult, op1=ALU.add)
            tok = E.sig(ins)
            bk.r[E.name] = tok
            dstT.w[E.name] = tok

    def make_resid_bufs(self, st):
        self.xr_ring = Ring([self.sb(f"xr{i}", [128, 1024], F32, st) for i in range(3)])
        self.tmp_ring = Ring([self.sb(f"tmp{i}", [128, 512], F32, st) for i in range(3)])
        self.gate = self.sb("gate", [128, D], F32, st)
        self.gate_key = None

    def load_gate(self, layer, sub, b):
        key = (layer, sub, b)
        if self.gate_key == key:
            return
        self.gate_key = key
        nc = self.nc
        self.gate.new_gen()
        self.SP.wdep(self.gate)
        self.SP.wait(self.mod_ready)
        src = self.mod_d[layer, b, (3 * sub + 2) * D:(3 * sub + 3) * D].partition_broadcast(128)
        ins = nc.sync.dma_start(out=self.gate.t[:], in_=src)
        self.gate.w["dma"] = self.gate.dma_sig(ins)
        self.track_dma(self.gate.w["dma"])

    def pass_B(self, hT, nch, w2src, ntiles, evac):
        nc = self.nc
        PE = self.PE
        assert ntiles == 4
        for half in range(2):
            bks = {}
            for t in range(ntiles):
                for n in range(2):
                    bk = self.next_bank()
                    bk.new_gen()
                    PE.wdep(bk)
                    bks[(t, n)] = bk
            PE.rdep(hT)
            for c in range(nch):
                wb = self.wload(w2src(c, half), cols=1024)
                PE.rdep(wb)
                for t in range(ntiles):
                    for n in range(2):
                        ins = nc.tensor.matmul(bks[(t, n)].t[:, :], lhsT=hT.t[:, c, t * 128:(t + 1) * 128],
                                               rhs=wb.t[:, n * 512:(n + 1) * 512], start=(c == 0), stop=(c == nch - 1))
                wb.r["pe"] = self.pe_sig(ins)
            hT.r["pe"] = wb.r["pe"]
            for bk in bks.values():
                bk.w["pe"] = wb.r["pe"]
            for t in range(ntiles):
                evac(t, half, [bks[(t, 0)], bks[(t, 1)]])

    def evac_resid(self, tile0, layer, sub, rowscale=None):
        nc = self.nc
        DVE, SP = self.DVE, self.SP

        def evac(t, half, bks2):
            tile = tile0 + t
            b = (tile * 128) // self.S
            self.load_gate(layer, sub, b)
            xr = self.xr_ring.next()
            xr.new_gen()
            SP.wdep(xr)
            if self.x0_is_input:
                srcap = self.x_in[tile * 128:(tile + 1) * 128, half * 1024:(half + 1) * 1024]
            else:
                srcap = self.xres[tile * 128:(tile + 1) * 128, half * 1024:(half + 1) * 1024]
                SP.wait(self.xres_tok.get((tile, half)))
                SP.wait(self.new_xres_tok.get((tile, half)))
            ins = nc.sync.dma_start(out=xr.t[:], in_=srcap)
            xr.w["dma"] = xr.dma_sig(ins)
            DVE.rdep(xr)
            DVE.rdep(self.gate)
            for n in range(2):
                bk = bks2[n]
                DVE.rdep(bk)
                tmp = self.tmp_ring.next()
                tmp.new_gen()
                DVE.wdep(tmp)
                gsl = self.gate.t[:, half * 1024 + n * 512: half * 1024 + (n + 1) * 512]
                if rowscale is None:
                    ins = nc.vector.tensor_tensor(out=tmp.t[:], in0=bk.t[:, :], in1=gsl, op=ALU.mult)
                else:
                    ins = nc.vector.scalar_tensor_tensor(out=tmp.t[:], in0=bk.t[:, :], scalar=rowscale(t), in1=gsl,
                                                         op0=ALU.mult, op1=ALU.mult)
                tok = DVE.sig(ins)
                bk.r["dve"] = tok
                tmp.w["dve"] = tok
                DVE.wait(tok)
                ins = nc.vector.tensor_tensor(out=xr.t[:, n * 512:(n + 1) * 512], in0=xr.t[:, n * 512:(n + 1) * 512],
                                              in1=tmp.t[:], op=ALU.add)
                tok = DVE.sig(ins)
                tmp.r["dve"] = tok
            self.gate.r["dve"] = tok
            xr.w["dve"] = tok
            SP.rdep(xr)
            ins = nc.sync.dma_start(out=self.xres[tile * 128:(tile + 1) * 128, half * 1024:(half + 1) * 1024], in_=xr.t[:])
            tok = xr.dma_sig(ins)
            xr.r["dma"] = tok
            self.track_dma(tok)
            self.new_xres_tok[(tile, half)] = tok
        return evac

    def commit_xres(self):
        self.xres_tok.update(self.new_xres_tok)
        self.new_xres_tok = {}

    def phase_conv(self):
        nc = self.nc
        PE, ACT, DVE, POOL, SP = self.PE, self.ACT, self.DVE, self.POOL, self.SP
        self.new_xres_tok = {}
        with ExitStack() as st:
            self.make_prep_bufs(st)
            self.make_resid_bufs(st)
            hnT = Ring([self.sb(f"hnT{i}", [128, KC, G], BF16, st) for i in range(2)])
            zT = Ring([self.sb(f"zT{i}", [128, KC, G], BF16, st) for i in range(2)])
            csb = Ring([self.sb(f"csb{i}", [128, G], F32, st) for i in range(2)])
            ub = Ring([self.sb(f"ub{i}", [128, G + 2], F32, st) for i in range(2)])
            cv = Ring([self.sb(f"cv{i}", [128, G], F32, st) for i in range(2)])
            halo = self.sb("halo", [128, KC, 2], F32, st)
            kcol = self.sb("kcol", [128, 3, KC], F32, st)
            with nc.allow_non_contiguous_dma(reason="tiny transposed load of conv taps"):
                for w in range(3):
                    ins = nc.sync.dma_start(out=kcol.t[:, w, :], in_=self.conv_kernel[w, :].rearrange("(k p) -> p k", p=128))
                    kcol.w["dma"] = kcol.dma_sig(ins)
            self.track_dma(kcol.w["dma"])
            ngroups = self.T // G
            for g in range(ngroups):
                tile0 = g * (G // 128)
                b = (g * G) // self.S
                first_in_seq = (g * G) % self.S == 0
                A, B = self.modcol(0, 0, b)
                h = hnT.next()
                h.new_gen()
                for t in range(G // 128):
                    self.prep(tile0 + t, A, B, h, t * 128)
                z = zT.next()
                z.new_gen()
                if first_in_seq:
                    halo.new_gen()
                    POOL.wdep(halo)
                    ins = nc.gpsimd.memset(halo.t[:], 0.0)
                    halo.w = {"pool": POOL.sig(ins)}
                PE.rdep(h)
                for j in range(KC):
                    bks = []
                    for which in range(3):
                        wb = self.wload(self.conv_w_in[which * KC + j, :, :])
                        bk = self.next_bank()
                        bk.new_gen()
                        PE.wdep(bk)
                        PE.rdep(wb)
                        for k in range(KC):
                            ins = nc.tensor.matmul(bk.t[:, 0:G], lhsT=wb.t[:, k * 128:(k + 1) * 128], rhs=h.t[:, k, :],
                                                   start=(k == 0), stop=(k == KC - 1))
                        tok = self.pe_sig(ins)
                        wb.r["pe"] = tok
                        bk.w["pe"] = tok
                        bks.append(bk)
                    h.r["pe"] = tok
                    cs = csb.next()
                    cs.new_gen()
                    ACT.wdep(cs)
                    ACT.rdep(bks[1])
                    ins = nc.scalar.copy(out=cs.t[:], in_=bks[1].t[:, 0:G])
                    tok = ACT.sig(ins)
                    cs.w["act"] = tok
                    bks[1].r["act"] = tok
                    u = ub.next()
                    u.new_gen()
                    DVE.wdep(u)
                    DVE.rdep(cs)
                    DVE.rdep(bks[2])
                    DVE.rdep(halo)
                    ins = nc.vector.tensor_copy(out=u.t[:, 0:2], in_=halo.t[:, j, :])
                    DVE.sig(ins)
                    ins = nc.vector.tensor_tensor(out=u.t[:, 2:G + 2], in0=cs.t[:], in1=bks[2].t[:, 0:G], op=ALU.mult)
                    tok = DVE.sig(ins)
                    cs.r["dve"] = tok
                    bks[2].r["dve"] = tok
                    DVE.wait(tok)
                    ins = nc.vector.tensor_copy(out=halo.t[:, j, :], in_=u.t[:, G:G + 2])
                    halo.w["dve"] = DVE.sig(ins)
                    DVE.rdep(kcol)
                    c = cv.next()
                    c.new_gen()
                    DVE.wdep(c)
                    ins = nc.vector.tensor_scalar(out=c.t[:], in0=u.t[:, 2:G + 2], scalar1=kcol.t[:, 2, j:j + 1], scalar2=None,
                                                  op0=ALU.mult)
                    DVE.wait(DVE.sig(ins))
                    ins = nc.vector.scalar_tensor_tensor(out=c.t[:], in0=u.t[:, 1:G + 1], scalar=kcol.t[:, 1, j:j + 1], in1=c.t[:],
                                                         op0=ALU.mult, op1=ALU.add)
                    DVE.wait(DVE.sig(ins))
                    ins = nc.vector.scalar_tensor_tensor(out=c.t[:], in0=u.t[:, 0:G], scalar=kcol.t[:, 0, j:j + 1], in1=c.t[:],
                                                         op0=ALU.mult, op1=ALU.add)
                    tok = DVE.sig(ins)
                    u.r["dve"] = tok
                    DVE.wait(tok)
                    DVE.rdep(bks[0])
                    DVE.wdep(z)
                    ins = nc.vector.tensor_tensor(out=z.t[:, j, :], in0=c.t[:], in1=bks[0].t[:, 0:G], op=ALU.mult)
                    tok = DVE.sig(ins)
                    bks[0].r["dve"] = tok
                    c.r["dve"] = tok
                    z.w["dve"] = tok
                self.pass_B(z, KC, lambda c_, half: self.conv_w_out[c_ * 128:(c_ + 1) * 128, half * 1024:(half + 1) * 1024],
                            G // 128, self.evac_resid(tile0, 0, 0))
            self.commit_xres()
        self.x0_is_input = False

    def swiglu_group(self, h, hT, nch, w1src, w3src, sring):
        nc = self.nc
        PE, ACT, DVE = self.PE, self.ACT, self.DVE
        PE.rdep(h)
        hT.new_gen()
        for c in range(nch):
            bks = []
            for src in (w1src, w3src):
                wb = self.wload(src(c))
                bk = self.next_bank()
                bk.new_gen()
                PE.wdep(bk)
                PE.rdep(wb)
                for k in range(KC):
                    ins = self.nc.tensor.matmul(bk.t[:, 0:G], lhsT=wb.t[:, k * 128:(k + 1) * 128], rhs=h.t[:, k, :],
                                                start=(k == 0), stop=(k == KC - 1))
                tok = self.pe_sig(ins)
                wb.r["pe"] = tok
                bk.w["pe"] = tok
                bks.append(bk)
            h.r["pe"] = tok
            s = sring.next()
            s.new_gen()
            ACT.wdep(s)
            ACT.rdep(bks[0])
            ins = nc.scalar.activation(out=s.t[:], in_=bks[0].t[:, 0:G], func=AF.Silu)
            tok = ACT.sig(ins)
            s.w["act"] = tok
            bks[0].r["act"] = tok
            DVE.rdep(s)
            DVE.rdep(bks[1])
            DVE.wdep(hT)
            ins = nc.vector.tensor_tensor(out=hT.t[:, c, :], in0=s.t[:], in1=bks[1].t[:, 0:G], op=ALU.mult)
            tok = DVE.sig(ins)
            s.r["dve"] = tok
            bks[1].r["dve"] = tok
            hT.w["dve"] = tok

    def phase_ffn(self):
        nc = self.nc
        self.new_xres_tok = {}
        nch = self.DFF // 128
        with ExitStack() as st:
            self.make_prep_bufs(st)
            self.make_resid_bufs(st)
            hnT = Ring([self.sb(f"fhnT{i}", [128, KC, G], BF16, st) for i in range(2)])
            hT = self.sb("fhT", [128, nch, G], BF16, st)
            sring = Ring([self.sb(f"fs{i}", [128, G], BF16, st) for i in range(3)])
            for g in range(self.T // G):
                tile0 = g * (G // 128)
                b = (g * G) // self.S
                A, B = self.modcol(0, 1, b)
                h = hnT.next()
                h.new_gen()
                for t in range(G // 128):
                    self.prep(tile0 + t, A, B, h, t * 128)
                self.swiglu_group(h, hT, nch, lambda c: self.ffn_w1[c, :, :], lambda c: self.ffn_w3[c, :, :], sring)
                self.pass_B(hT, nch, lambda c_, half: self.ffn_w2[c_ * 128:(c_ + 1) * 128, half * 1024:(half + 1) * 1024],
                            G // 128, self.evac_resid(tile0, 0, 1))
            self.commit_xres()
        self.x0_is_input = False

    def phase_attn(self):
        nc = self.nc
        PE, ACT, DVE, POOL, SP = self.PE, self.ACT, self.DVE, self.POOL, self.SP
        S, T, NT = self.S, self.T, self.NT
        TPS = S // 128
        scale = float(HD) ** -0.5
        self.new_xres_tok = {}
        qkT_d = self.dram("qkT_d", [32, 128, T], BF16)
        v_d = self.dram("v_d", [T, D], BF16)
        o_d = self.dram("o_d", [T, D], BF16)
        cum_d = self.dram("cum_d", [NH, T], F32)
        with ExitStack() as st:
            self.make_prep_bufs(st)
            hnT = Ring([self.sb(f"ahnT{i}", [128, KC, G], BF16, st) for i in range(2)])
            qsb = Ring([self.sb(f"qsb{i}", [128, G], BF16, st) for i in range(3)])
            vsb = Ring([self.sb(f"vsb{i}", [128, 1024], BF16, st) for i in range(3)])
            wf = self.sb("wf", [128, KC, NH], BF16, st)
            bf_t = self.sb("bf_t", [128, NH], F32, st)
            one_c = self.sb("one_c", [128, 1], F32, st)
            tri = self.sb("tri", [128, 128], F32, st)
            ones = self.sb("ones", [128, 128], F32, st)
            lf = self.sb("lf", [128, NT, NH], F32, st)
            zt = Ring([self.sb(f"zt{i}", [128, NH], F32, st) for i in range(2)])
            cumt = Ring([self.sb(f"cumt{i}", [128, NH], F32, st) for i in range(2)])
            cumr = Ring([self.sb(f"cumr{i}", [NH, 128], F32, st) for i in range(2)])
            ins = nc.gpsimd.dma_start(out=wf.t[:], in_=self.fox_f.rearrange("(k p) n -> p k n", p=128))
            wf.w["dma"] = wf.dma_sig(ins)
            ins = nc.sync.dma_start(out=bf_t.t[:], in_=self.fox_b_f.partition_broadcast(128))
            bf_t.w["dma"] = bf_t.dma_sig(ins)
            nc.gpsimd.memset(one_c.t[:], 1.0)
            nc.gpsimd.memset(ones.t[:], 1.0)
            nc.gpsimd.memset(tri.t[:], 1.0)
            ins = nc.gpsimd.affine_select(out=tri.t[:], in_=tri.t[:], pattern=[[1, 128]], compare_op=ALU.is_ge, fill=0.0,
                                          base=0, channel_multiplier=-1)
            tok = POOL.sig(ins)
            tri.w["pool"] = tok
            ones.w["pool"] = tok
            one_c.w["pool"] = tok
            for g in range(T // G):
                tile0 = g * (G // 128)
                b = (g * G) // S
                A, B = self.modcol(1, 0, b)
                h = hnT.next()
                h.new_gen()
                for t in range(G // 128):
                    self.prep(tile0 + t, A, B, h, t * 128)
                PE.rdep(h)
                for ch in range(32):
                    wb = self.wload(self.fox_qk[ch, :, :])
                    bk = self.next_bank()
                    bk.new_gen()
                    PE.wdep(bk)
                    PE.rdep(wb)
                    for k in range(KC):
                        ins = nc.tensor.matmul(bk.t[:, 0:G], lhsT=wb.t[:, k * 128:(k + 1) * 128], rhs=h.t[:, k, :],
                                               start=(k == 0), stop=(k == KC - 1))
                    tok = self.pe_sig(ins)
                    wb.r["pe"] = tok
                    bk.w["pe"] = tok
                    q = qsb.next()
                    q.new_gen()
                    E = ACT if ch % 2 == 0 else DVE
                    E.wdep(q)
                    E.rdep(bk)
                    if E is ACT:
                        ins = nc.scalar.copy(out=q.t[:], in_=bk.t[:, 0:G])
                    else:
                        ins = nc.vector.tensor_copy(out=q.t[:], in_=bk.t[:, 0:G])
                    tok = E.sig(ins)
                    q.w[E.name] = tok
                    bk.r[E.name] = tok
                    SP.rdep(q)
                    ins = nc.sync.dma_start(out=qkT_d[ch, :, g * G:(g + 1) * G], in_=q.t[:])
                    tok = q.dma_sig(ins)
                    q.r["dma"] = tok
                    self.track_dma(tok)
                for t in range(G // 128):
                    tile = tile0 + t
                    bk = self.next_bank()
                    bk.new_gen()
                    PE.wdep(bk)
                    PE.rdep(wf)
                    for k in range(KC):
                        ins = nc.tensor.matmul(bk.t[:, 0:NH], lhsT=h.t[:, k, t * 128:(t + 1) * 128], rhs=wf.t[:, k, :],
                                               start=(k == 0), stop=(k == KC - 1))
                    tok = self.pe_sig(ins)
                    bk.w["pe"] = tok
                    z = zt.next()
                    z.new_gen()
                    DVE.wdep(z)
                    DVE.rdep(bk)
                    DVE.rdep(bf_t)
                    ins = nc.vector.tensor_tensor(out=z.t[:], in0=bk.t[:, 0:NH], in1=bf_t.t[:], op=ALU.add)
                    tok = DVE.sig(ins)
                    z.w["dve"] = tok
                    bk.r["dve"] = tok
                    ACT.rdep(z)
                    ACT.rdep(one_c)
                    ins = nc.scalar.activation(out=z.t[:], in_=z.t[:], func=AF.Exp, scale=-1.0)
                    ACT.wait(ACT.sig(ins))
                    ins = nc.scalar.activation(out=z.t[:], in_=z.t[:], func=AF.Ln, bias=one_c.t[:, 0:1])
                    ACT.wait(ACT.sig(ins))
                    ACT.waits(lf.prev)
                    ins = nc.scalar.mul(out=lf.t[:, tile, :], in_=z.t[:], mul=-1.0)
                    tok = ACT.sig(ins)
                    lf.w["act"] = tok
                    z.r["act"] = tok
                h.r["pe"] = self.last_pe_tok
                for t in range(G // 128):
                    tile = tile0 + t
                    seq0 = (tile // TPS) * TPS
                    bk = self.next_bank()
                    bk.new_gen()
                    PE.wdep(bk)
                    PE.rdep(lf)
                    PE.rdep(tri)
                    prevs = list(range(seq0, tile))
                    ins = nc.tensor.matmul(bk.t[:, 0:NH], lhsT=tri.t[:], rhs=lf.t[:, tile, :], start=True, stop=(len(prevs) == 0))
                    for i_, pt in enumerate(prevs):
                        ins = nc.tensor.matmul(bk.t[:, 0:NH], lhsT=ones.t[:], rhs=lf.t[:, pt, :], start=False,
                                               stop=(i_ == len(prevs) - 1))
                    tok = self.pe_sig(ins)
                    bk.w["pe"] = tok
                    lf.r["pe"] = tok
                    ct = cumt.next()
                    ct.new_gen()
                    DVE.wdep(ct)
                    DVE.rdep(bk)
                    ins = nc.vector.tensor_scalar(out=ct.t[:], in0=bk.t[:, 0:NH], scalar1=-1.0 / scale, scalar2=None, op0=ALU.mult)
                    tok = DVE.sig(ins)
                    ct.w["dve"] = tok
                    bk.r["dve"] = tok
                    bk2 = self.next_bank()
                    bk2.new_gen()
                    PE.wdep(bk2)
                    PE.rdep(ct)
                    PE.rdep(self.ident_f)
                    ins = nc.tensor.transpose(out=bk2.t[0:NH, 0:128], in_=ct.t[:], identity=self.ident_f.t[:])
                    tok = self.pe_sig(ins)
                    bk2.w["pe"] = tok
                    ct.r["pe"] = tok
                    cr = cumr.next()
                    cr.new_gen()
                    DVE.wdep(cr)
                    DVE.rdep(bk2)
                    ins = nc.vector.tensor_copy(out=cr.t[:], in_=bk2.t[0:NH, 0:128])
                    tok = DVE.sig(ins)
                    cr.w["dve"] = tok
                    bk2.r["dve"] = tok
                    SP.rdep(cr)
                    ins = nc.sync.dma_start(out=cum_d[:, tile * 128:(tile + 1) * 128], in_=cr.t[:])
                    tok = cr.dma_sig(ins)
                    cr.r["dma"] = tok
                    self.track_dma(tok)
                def evac_v(t, half, bks2, tile0=tile0):
                    vb = vsb.next()
                    vb.new_gen()
                    for n in range(2):
                        E = ACT if n == 0 else DVE
                        E.wdep(vb)
                        E.rdep(bks2[n])
                        if E is ACT:
                            ins = nc.scalar.copy(out=vb.t[:, n * 512:(n + 1) * 512], in_=bks2[n].t[:, :])
                        else:
                            ins = nc.vector.tensor_copy(out=vb.t[:, n * 512:(n + 1) * 512], in_=bks2[n].t[:, :])
                        tok = E.sig(ins)
                        vb.w[E.name] = tok
                        bks2[n].r[E.name] = tok
                    SP.rdep(vb)
                    ins = nc.sync.dma_start(out=v_d[(tile0 + t) * 128:(tile0 + t + 1) * 128, half * 1024:(half + 1) * 1024], in_=vb.t[:])
                    tok = vb.dma_sig(ins)
                    vb.r["dma"] = tok
                    self.track_dma(tok)
                self.pass_B(h, KC, lambda c_, half: self.fox_v[c_ * 128:(c_ + 1) * 128, half * 1024:(half + 1) * 1024],
                            G // 128, evac_v)
        self.barrier()
        with ExitStack() as st:
            qT = Ring([self.sb(f"qT{i}", [128, S], BF16, st) for i in range(2)])
            kT = Ring([self.sb(f"kT{i}", [128, S], BF16, st) for i in range(2)])
            vt = Ring([self.sb(f"vt{i}", [128, TPS, HD], BF16, st) for i in range(2)])
            bias = Ring([self.sb(f"abias{i}", [128, S], F32, st) for i in range(2)])
            Sb = Ring([self.sb(f"Sb{i}", [128, S], F32, st) for i in range(2)])
            Pb = Ring([self.sb(f"Pb{i}", [128, S], BF16, st) for i in range(2)])
            PT = Ring([self.sb(f"PT{i}", [128, TPS, 128], BF16, st) for i in range(2)])
            Oh = Ring([self.sb(f"Oh{i}", [128, TPS, HD], BF16, st) for i in range(2)])
            st_ring = Ring([self.sb(f"ast{i}", [128, 4], F32, st) for i in range(4)])
            neg_reg = nc.gpsimd.to_reg(-1.0e30)
            for seq in range(self.NSEQ):
                for hh in range(NH):
                    q, kk, v, bs = qT.next(), kT.next(), vt.next(), bias.next()
                    for bf_, src in ((q, qkT_d[hh, :, seq * S:(seq + 1) * S]), (kk, qkT_d[16 + hh, :, seq * S:(seq + 1) * S]),
                                     (v, v_d[seq * S:(seq + 1) * S, hh * HD:(hh + 1) * HD].rearrange("(j p) d -> p j d", p=128)),
                                     (bs, cum_d[hh, seq * S:(seq + 1) * S].partition_broadcast(128))):
                        bf_.new_gen()
                        SP.wdep(bf_)
                        ins = nc.sync.dma_start(out=bf_.t[:], in_=src)
                        bf_.w["dma"] = bf_.dma_sig(ins)
                        self.track_dma(bf_.w["dma"])
                    oh = Oh.next()
                    oh.new_gen()
                    for i in range(TPS):
                        nk = (i + 1) * 128
                        nkb = (nk + 511) // 512
                        sb_ = Sb.next()
                        sb_.new_gen()
                        PE.rdep(q)
                        PE.rdep(kk)
                        DVE.rdep(bs)
                        for kb in range(nkb):
                            w = min(512, nk - kb * 512)
                            bk = self.next_bank()
                            bk.new_gen()
                            PE.wdep(bk)
                            ins = nc.tensor.matmul(bk.t[:, 0:w], lhsT=q.t[:, i * 128:(i + 1) * 128], rhs=kk.t[:, kb * 512:kb * 512 + w],
                                                   start=True, stop=True)
                            tok = self.pe_sig(ins)
                            bk.w["pe"] = tok
                            DVE.rdep(bk)
                            DVE.wdep(sb_)
                            ins = nc.vector.tensor_tensor(out=sb_.t[:, kb * 512:kb * 512 + w], in0=bk.t[:, 0:w],
                                                          in1=bs.t[:, kb * 512:kb * 512 + w], op=ALU.add)
                            tok = DVE.sig(ins)
                            bk.r["dve"] = tok
                            sb_.w["dve"] = tok
                        q.r["pe"] = self.last_pe_tok
                        kk.r["pe"] = self.last_pe_tok
                        bs.r["dve"] = tok
                        POOL.rdep(sb_)
                        ins = nc.gpsimd.affine_select(out=sb_.t[:, i * 128:(i + 1) * 128], in_=sb_.t[:, i * 128:(i + 1) * 128],
                                                      pattern=[[-1, 128]], compare_op=ALU.is_ge, fill=neg_reg, base=0, channel_multiplier=1)
                        sb_.w["pool"] = POOL.sig(ins)
                        stt = st_ring.next()
                        stt.new_gen()
                        DVE.wdep(stt)
                        DVE.rdep(sb_)
                        ins = nc.vector.reduce_max(out=stt.t[:, 0:1], in_=sb_.t[:, 0:nk], axis=AX.X)
                        DVE.wait(DVE.sig(ins))
                        ins = nc.vector.tensor_scalar(out=stt.t[:, 1:2], in0=stt.t[:, 0:1], scalar1=-scale, scalar2=None, op0=ALU.mult)
                        stt.w["dve"] = DVE.sig(ins)
                        pb = Pb.next()
                        pb.new_gen()
                        ACT.wdep(pb)
                        ACT.rdep(stt)
                        ACT.rdep(sb_)
                        ins = nc.scalar.memzero(stt.t[:, 2:3])
                        ACT.wait(ACT.sig(ins))
                        ins = nc.scalar.activation(out=pb.t[:, 0:nk], in_=sb_.t[:, 0:nk], func=AF.Exp, scale=scale, bias=stt.t[:, 1:2],
                                                   accum_out=stt.t[:, 2:3])
                        tok = ACT.sig(ins)
                        pb.w["act"] = tok
                        sb_.r["act"] = tok
                        stt.w["act"] = tok
                        pt = PT.next()
                        pt.new_gen()
                        PE.rdep(pb)
                        PE.rdep(self.ident_bf)
                        nkt = i + 1
                        for j0 in range(0, nkt, 8):
                            bk = self.next_bank()
                            bk.new_gen()
                            PE.wdep(bk)
                            pv = bk.t[:].bitcast(BF16)
                            cnt = min(8, nkt - j0)
                            for jj in range(cnt):
                                kt = j0 + jj
                                ins = nc.tensor.transpose(out=pv[:, jj * 128:(jj + 1) * 128], in_=pb.t[:, kt * 128:(kt + 1) * 128],
                                                          identity=self.ident_bf.t[:])
                            tok = self.pe_sig(ins)
                            bk.w["pe"] = tok
                            E = ACT if (j0 // 8) % 2 == 0 else DVE
                            E.rdep(bk)
                            E.wdep(pt)
                            if E is ACT:
                                ins = nc.scalar.copy(out=pt.t[:, j0:j0 + cnt, :], in_=pv[:, 0:cnt * 128].rearrange("p (j q) -> p j q", q=128))
                            else:
                                ins = nc.vector.tensor_copy(out=pt.t[:, j0:j0 + cnt, :], in_=pv[:, 0:cnt * 128].rearrange("p (j q) -> p j q", q=128))
                            tok = E.sig(ins)
                            bk.r[E.name] = tok
                            pt.w[E.name] = tok
                        pb.r["pe"] = self.last_pe_tok
                        bk = self.next_bank()
                        bk.new_gen()
                        PE.wdep(bk)
                        PE.rdep(pt)
                        PE.rdep(v)
                        for kt in range(nkt):
                            ins = nc.tensor.matmul(bk.t[:, 0:HD], lhsT=pt.t[:, kt, :], rhs=v.t[:, kt, :], start=(kt == 0), stop=(kt == nkt - 1))
                        tok = self.pe_sig(ins)
                        bk.w["pe"] = tok
                        pt.r["pe"] = tok
                        v.r["pe"] = tok
                        DVE.rdep(stt)
                        ins = nc.vector.reciprocal(out=stt.t[:, 3:4], in_=stt.t[:, 2:3])
                        stt.w["dve"] = DVE.sig(ins)
                        ACT.rdep(stt)
                        ACT.rdep(bk)
                        ACT.wdep(oh)
                        ins = nc.scalar.activation(out=oh.t[:, i, :], in_=bk.t[:, 0:HD], func=AF.Copy, scale=stt.t[:, 3:4])
                        tok = ACT.sig(ins)
                        bk.r["act"] = tok
                        oh.w["act"] = tok
                        stt.r["act"] = tok
                    SP.rdep(oh)
                    ins = nc.sync.dma_start(out=o_d[seq * S:(seq + 1) * S, hh * HD:(hh + 1) * HD].rearrange("(j p) d -> p j d", p=128),
                                            in_=oh.t[:])
                    tok = oh.dma_sig(ins)
                    oh.r["dma"] = tok
                    self.track_dma(tok)
        self.barrier()
        with ExitStack() as st:
            self.make_resid_bufs(st)
            ot_ring = Ring([self.sb(f"otok{i}", [128, D], BF16, st) for i in range(2)])
            oT = Ring([self.sb(f"oT{i}", [128, KC, G], BF16, st) for i in range(2)])
            for g in range(T // G):
                tile0 = g * (G // 128)
                o_T = oT.next()
                o_T.new_gen()
                for t in range(G // 128):
                    ot = ot_ring.next()
                    ot.new_gen()
                    SP.wdep(ot)
                    ins = nc.sync.dma_start(out=ot.t[:], in_=o_d[(tile0 + t) * 128:(tile0 + t + 1) * 128, :])
                    ot.w["dma"] = ot.dma_sig(ins)
                    self.track_dma(ot.w["dma"])
                    PE.rdep(ot)
                    for j in range(2):
                        bk = self.next_bank()
                        bk.new_gen()
                        PE.wdep(bk)
                        pv = bk.t[:].bitcast(BF16)
                        for i in range(8):
                            k = 8 * j + i
                            ins = nc.tensor.transpose(out=pv[:, i * 128:(i + 1) * 128], in_=ot.t[:, k * 128:(k + 1) * 128],
                                                      identity=self.ident_bf.t[:])
                        tok = self.pe_sig(ins)
                        bk.w["pe"] = tok
                        ot.r["pe"] = tok
                        E = ACT if j == 0 else DVE
                        E.rdep(bk)
                        E.wdep(o_T)
                        for i in range(8):
                            k = 8 * j + i
                            if E is ACT:
                                ins = nc.scalar.copy(out=o_T.t[:, k, t * 128:(t + 1) * 128], in_=pv[:, i * 128:(i + 1) * 128])
                            else:
                                ins = nc.vector.tensor_copy(out=o_T.t[:, k, t * 128:(t + 1) * 128], in_=pv[:, i * 128:(i + 1) * 128])
                        tok = E.sig(ins)
                        bk.r[E.name] = tok
                        o_T.w[E.name] = tok
                self.pass_B(o_T, KC, lambda c_, half: self.fox_w_out[c_ * 128:(c_ + 1) * 128, half * 1024:(half + 1) * 1024],
                            G // 128, self.evac_resid(tile0, 1, 0))
            self.commit_xres()
        self.x0_is_input = False

    def phase_moe(self):
        nc = self.nc
        PE, ACT, DVE, POOL, SP = self.PE, self.ACT, self.DVE, self.POOL, self.SP
        NE, T, NT = self.NE, self.T, self.NT
        nch = self.DFFE // 128
        self.new_xres_tok = {}
        with ExitStack() as st:
            self.make_prep_bufs(st)
            self.make_resid_bufs(st)
            hnT = Ring([self.sb(f"mhnT{i}", [128, KC, G], BF16, st) for i in range(1)])
            hT = self.sb("mhT", [128, nch, G], BF16, st)
            sring = Ring([self.sb(f"ms{i}", [128, G], BF16, st) for i in range(3)])
            wr = self.sb("wr", [128, KC, NE], BF16, st)
            gates = self.sb("gates", [128, NT, NE], F32, st)
            lg = Ring([self.sb(f"lg{i}", [128, 4, NE], F32, st) for i in range(2)])
            sm = Ring([self.sb(f"sm{i}", [128, 8], F32, st) for i in range(2)])
            ins = nc.gpsimd.dma_start(out=wr.t[:], in_=self.moe_router.rearrange("(k p) n -> p k n", p=128))
            wr.w["dma"] = wr.dma_sig(ins)
            for g in range(T // G):
                tile0 = g * (G // 128)
                b = (g * G) // self.S
                A, B = self.modcol(1, 1, b)
                h = hnT.next()
                h.new_gen()
                for t in range(G // 128):
                    self.prep(tile0 + t, A, B, h, t * 128)
                PE.rdep(h)
                PE.rdep(wr)
                for t in range(G // 128):
                    tile = tile0 + t
                    bk = self.next_bank()
                    bk.new_gen()
                    PE.wdep(bk)
                    for k in range(KC):
                        ins = nc.tensor.matmul(bk.t[:, 0:NE], lhsT=h.t[:, k, t * 128:(t + 1) * 128], rhs=wr.t[:, k, :],
                                               start=(k == 0), stop=(k == KC - 1))
                    tok = self.pe_sig(ins)
                    bk.w["pe"] = tok
                    L = lg.next()
                    L.new_gen()
                    s_ = sm.next()
                    s_.new_gen()
                    DVE.wdep(L)
                    DVE.wdep(s_)
                    DVE.rdep(bk)
                    ins = nc.vector.tensor_copy(out=L.t[:, 0, :], in_=bk.t[:, 0:NE])
                    tok = DVE.sig(ins)
                    bk.r["dve"] = tok
                    DVE.wait(tok)
                    ins = nc.vector.reduce_max(out=s_.t[:, 0:1], in_=L.t[:, 0, :], axis=AX.X)
                    DVE.wait(DVE.sig(ins))
                    ins = nc.vector.tensor_scalar(out=L.t[:, 1, :], in0=L.t[:, 0, :], scalar1=s_.t[:, 0:1], scalar2=None, op0=ALU.is_equal)
                    DVE.wait(DVE.sig(ins))
                    ins = nc.vector.scalar_tensor_tensor(out=L.t[:, 2, :], in0=L.t[:, 1, :], scalar=-1.0e30, in1=L.t[:, 0, :],
                                                         op0=ALU.mult, op1=ALU.add)
                    DVE.wait(DVE.sig(ins))
                    ins = nc.vector.reduce_max(out=s_.t[:, 1:2], in_=L.t[:, 2, :], axis=AX.X)
                    DVE.wait(DVE.sig(ins))
                    ins = nc.vector.tensor_scalar(out=L.t[:, 3, :], in0=L.t[:, 2, :], scalar1=s_.t[:, 1:2], scalar2=None, op0=ALU.is_equal)
                    DVE.sig(ins)
                    ins = nc.vector.tensor_tensor(out=s_.t[:, 2:3], in0=s_.t[:, 1:2], in1=s_.t[:, 0:1], op=ALU.subtract)
                    tok = DVE.sig(ins)
                    s_.w["dve"] = tok
                    ACT.rdep(s_)
                    ins = nc.scalar.activation(out=s_.t[:, 3:4], in_=s_.t[:, 2:3], func=AF.Sigmoid, scale=-1.0)
                    ACT.sig(ins)
                    ins = nc.scalar.activation(out=s_.t[:, 4:5], in_=s_.t[:, 2:3], func=AF.Sigmoid, scale=1.0)
                    tok = ACT.sig(ins)
                    s_.w["act"] = tok
                    DVE.rdep(s_)
                    DVE.waits(gates.prev)
                    ins = nc.vector.tensor_scalar(out=gates.t[:, tile, :], in0=L.t[:, 1, :], scalar1=s_.t[:, 3:4], scalar2=None, op0=ALU.mult)
                    DVE.wait(DVE.sig(ins))
                    ins = nc.vector.scalar_tensor_tensor(out=gates.t[:, tile, :], in0=L.t[:, 3, :], scalar=s_.t[:, 4:5], in1=gates.t[:, tile, :],
                                                         op0=ALU.mult, op1=ALU.add)
                    tok = DVE.sig(ins)
                    gates.w["dve"] = tok
                    L.r["dve"] = tok
                    s_.r["dve"] = tok
                for e in range(NE):
                    self.swiglu_group(h, hT, nch, lambda c, e=e: self.moe_w1[e * nch + c, :, :], lambda c, e=e: self.moe_w3[e * nch + c, :, :], sring)
                    self.DVE.rdep(gates)
                    self.pass_B(hT, nch,
                                lambda c_, half, e=e: self.moe_w2[e * self.DFFE + c_ * 128: e * self.DFFE + (c_ + 1) * 128, half * 1024:(half + 1) * 1024],
                                G // 128, self.evac_resid(tile0, 1, 1, rowscale=lambda t, e=e, tile0=tile0: gates.t[:, tile0 + t, e:e + 1]))
                    self.commit_xres()
                    self.x0_is_input = False
            self.commit_xres()

    def phase_moe_sparse(self):
        nc = self.nc
        PE, ACT, DVE, POOL, SP = self.PE, self.ACT, self.DVE, self.POOL, self.SP
        NE, T, NT, S = self.NE, self.T, self.NT, self.S
        I32 = mybir.dt.int32
        nch = self.DFFE // 128
        NG = (2 * T) // G + NE - 1
        NSLOT = NG * G
        Xg = self.dram("Xg", [NSLOT, D], BF16)
        Yg = self.dram("Yg", [NSLOT, D], F32)
        hn_d = self.dram("hn_d", [T, D], BF16)
        w1tab = self.moe_w1.rearrange("c p f -> (c p) f")
        w3tab = self.moe_w3.rearrange("c p f -> (c p) f")
        w2tab = self.moe_w2.rearrange("r (h c) -> (r h) c", h=2)
        self.new_xres_tok = {}
        with ExitStack() as ph:
            a_bf = self.sb("a_bf", [128, NT, NE], BF16, ph)
            eq1f = self.sb("eq1f", [128, NT, NE], F32, ph)
            eq2f = self.sb("eq2f", [128, NT, NE], F32, ph)
            wts = self.sb("wts", [128, NT, 2], F32, ph)
            cnt = self.sb("cnt", [128, NT, NE], F32, ph)
            slots_f = self.sb("slots_f", [128, NT, 2], F32, ph)
            slots_i = self.sb("slots_i", [128, NT, 2], I32, ph)
            triS = self.sb("triS", [128, 128], BF16, ph)
            ones_bf = self.sb("ones_bf", [128, 128], BF16, ph)
            ntot = self.sb("ntot", [128, NE], F32, ph)
            padded = self.sb("padded", [128, NE], F32, ph)
            base = self.sb("base", [128, NE], F32, ph)
            endt = self.sb("endt", [128, NE], F32, ph)
            etab = self.sb("etab", [128, NG], F32, ph)
            iota_i = self.sb("iota_i", [128, nch], I32, ph)
            iota_f = self.sb("iota_f", [128, nch], F32, ph)
            wr = self.sb("wr", [128, KC, NE], BF16, ph)
            ins = nc.gpsimd.dma_start(out=wr.t[:], in_=self.moe_router.rearrange("(k p) n -> p k n", p=128))
            wr.w["dma"] = wr.dma_sig(ins)
            nc.gpsimd.memset(ones_bf.t[:], 1.0)
            nc.gpsimd.memset(triS.t[:], 1.0)
            nc.gpsimd.affine_select(out=triS.t[:], in_=triS.t[:], pattern=[[1, 128]], compare_op=ALU.is_ge, fill=0.0,
                                    base=-1, channel_multiplier=-1)
            ins = nc.gpsimd.iota(iota_i.t[:], pattern=[[128, nch]], base=0, channel_multiplier=1)
            tok = POOL.sig(ins)
            triS.w["pool"] = tok
            ones_bf.w["pool"] = tok
            iota_i.w["pool"] = tok
            DVE.rdep(iota_i)
            ins = nc.vector.tensor_copy(out=iota_f.t[:], in_=iota_i.t[:])
            iota_f.w["dve"] = DVE.sig(ins)
            with ExitStack() as st:
                xt_ring = Ring([self.sb(f"rxt{i}", [128, D], F32, st) for i in range(2)])
                hn_ring = Ring([self.sb(f"rhn{i}", [128, D], BF16, st) for i in range(2)])
                junk = self.sb("rjunk", [128, D], BF16, st)
                ss_ring = Ring([self.sb(f"rss{i}", [128, 2], F32, st) for i in range(4)])
                Arow = self.sb("Arow", [128, D], F32, st)
                Brow = self.sb("Brow", [128, D], F32, st)
                Trow = self.sb("Trow", [128, D], F32, st)
                hTr = Ring([self.sb(f"hTr{i}", [128, KC, 128], BF16, st) for i in range(2)])
                lg = Ring([self.sb(f"slg{i}", [128, 4, NE], F32, st) for i in range(2)])
                sm = Ring([self.sb(f"ssm{i}", [128, 8], F32, st) for i in range(2)])
                hn_tok = {}
                cur_b = None
                for tile in range(NT):
                    b = (tile * 128) // S
                    if b != cur_b:
                        cur_b = b
                        for bf_ in (Arow, Brow, Trow):
                            bf_.new_gen()
                            SP.wdep(bf_)
                        SP.wait(self.mod_ready)
                        ins = nc.sync.dma_start(out=Trow.t[:], in_=self.norm_ffn[1, :].partition_broadcast(128))
                        Trow.w["dma"] = Trow.dma_sig(ins)
                        ins = nc.sync.dma_start(out=Arow.t[:], in_=self.mod_d[1, b, 4 * D:5 * D].partition_broadcast(128))
                        Arow.w["dma"] = Arow.dma_sig(ins)
                        ins = nc.sync.dma_start(out=Brow.t[:], in_=self.mod_d[1, b, 3 * D:4 * D].partition_broadcast(128))
                        Brow.w["dma"] = Brow.dma_sig(ins)
                        DVE.rdep(Arow)
                        DVE.rdep(Trow)
                        DVE.rdep(Brow)
                        ins = nc.vector.scalar_tensor_tensor(out=Arow.t[:], in0=Arow.t[:], scalar=1.0, in1=Trow.t[:], op0=ALU.add, op1=ALU.mult)
                        tok = DVE.sig(ins)
                        Arow.w["dve"] = tok
                        Trow.r["dve"] = tok
                        DVE.wait(tok)
                    xt = xt_ring.next()
                    xt.new_gen()
                    SP.wdep(xt)
                    SP.waits([self.xres_tok.get((tile, 0)), self.xres_tok.get((tile, 1))])
                    src = self.x_in if self.x0_is_input else self.xres
                    ins = nc.sync.dma_start(out=xt.t[:], in_=src[tile * 128:(tile + 1) * 128, :])
                    xt.w["dma"] = xt.dma_sig(ins)
                    ss = ss_ring.next()
                    ss.new_gen()
                    junk.new_gen()
                    ACT.wdep(ss)
                    ACT.wdep(junk)
                    ACT.rdep(xt)
                    ins = nc.scalar.memzero(ss.t[:])
                    ACT.wait(ACT.sig(ins))
                    ins = nc.scalar.activation(out=junk.t[:], in_=xt.t[:], func=AF.Square, accum_out=ss.t[:, 0:1])
                    tok = ACT.sig(ins)
                    junk.w["act"] = tok
                    ACT.wait(tok)
                    ins = nc.scalar.activation(out=ss.t[:, 1:2], in_=ss.t[:, 0:1], func=AF.Sqrt, scale=1.0 / D, bias=self.eps_col.t[:, 0:1])
                    ss.w["act"] = ACT.sig(ins)
                    DVE.rdep(ss)
                    ins = nc.vector.reciprocal(out=ss.t[:, 1:2], in_=ss.t[:, 1:2])
                    DVE.wait(DVE.sig(ins))
                    DVE.rdep(xt)
                    ins = nc.vector.scalar_tensor_tensor(out=xt.t[:], in0=xt.t[:], scalar=ss.t[:, 1:2], in1=Arow.t[:], op0=ALU.mult, op1=ALU.mult)
                    tok = DVE.sig(ins)
                    ss.r["dve"] = tok
                    DVE.wait(tok)
                    hn = hn_ring.next()
                    hn.new_gen()
                    DVE.wdep(hn)
                    ins = nc.vector.tensor_tensor(out=hn.t[:], in0=xt.t[:], in1=Brow.t[:], op=ALU.add)
                    tok = DVE.sig(ins)
                    hn.w["dve"] = tok
                    xt.r["dve"] = tok
                    Arow.r["dve"] = tok
                    Brow.r["dve"] = tok
                    SP.rdep(hn)
                    ins = nc.sync.dma_start(out=hn_d[tile * 128:(tile + 1) * 128, :], in_=hn.t[:])
                    tok = hn.dma_sig(ins)
                    hn.r["dma"] = tok
                    hn_tok[tile] = tok
                    self.track_dma(tok)
                    hr = hTr.next()
                    hr.new_gen()
                    PE.rdep(hn)
                    PE.rdep(self.ident_bf)
                    for j in range(2):
                        bk = self.next_bank()
                        bk.new_gen()
                        PE.wdep(bk)
                        pv = bk.t[:].bitcast(BF16)
                        for i in range(8):
                            k = 8 * j + i
                            ins = nc.tensor.transpose(out=pv[:, i * 128:(i + 1) * 128], in_=hn.t[:, k * 128:(k + 1) * 128],
                                                      identity=self.ident_bf.t[:])
                        tok = self.pe_sig(ins)
                        bk.w["pe"] = tok
                        hn.r["pe"] = tok
                        E = ACT if j == 0 else DVE
                        E.rdep(bk)
                        E.wdep(hr)
                        if E is ACT:
                            ins = nc.scalar.copy(out=hr.t[:, 8 * j:8 * j + 8, :], in_=pv.rearrange("p (j q) -> p j q", q=128))
                        else:
                            ins = nc.vector.tensor_copy(out=hr.t[:, 8 * j:8 * j + 8, :], in_=pv.rearrange("p (j q) -> p j q", q=128))
                        tok = E.sig(ins)
                        bk.r[E.name] = tok
                        hr.w[E.name] = tok
                    bk = self.next_bank()
                    bk.new_gen()
                    PE.wdep(bk)
                    PE.rdep(hr)
                    PE.rdep(wr)
                    for k in range(KC):
                        ins = nc.tensor.matmul(bk.t[:, 0:NE], lhsT=hr.t[:, k, :], rhs=wr.t[:, k, :], start=(k == 0), stop=(k == KC - 1))
                    tok = self.pe_sig(ins)
                    bk.w["pe"] = tok
                    hr.r["pe"] = tok
                    L = lg.next()
                    L.new_gen()
                    s_ = sm.next()
                    s_.new_gen()
                    DVE.wdep(L)
                    DVE.wdep(s_)
                    DVE.rdep(bk)
                    ins = nc.vector.tensor_copy(out=L.t[:, 0, :], in_=bk.t[:, 0:NE])
                    tok = DVE.sig(ins)
                    bk.r["dve"] = tok
                    DVE.wait(tok)
                    ins = nc.vector.reduce_max(out=s_.t[:, 0:1], in_=L.t[:, 0, :], axis=AX.X)
                    DVE.wait(DVE.sig(ins))
                    ins = nc.vector.tensor_scalar(out=eq1f.t[:, tile, :], in0=L.t[:, 0, :], scalar1=s_.t[:, 0:1], scalar2=None, op0=ALU.is_equal)
                    DVE.wait(DVE.sig(ins))
                    ins = nc.vector.scalar_tensor_tensor(out=L.t[:, 2, :], in0=eq1f.t[:, tile, :], scalar=-1.0e30, in1=L.t[:, 0, :],
                                                         op0=ALU.mult, op1=ALU.add)
                    DVE.wait(DVE.sig(ins))
                    ins = nc.vector.reduce_max(out=s_.t[:, 1:2], in_=L.t[:, 2, :], axis=AX.X)
                    DVE.wait(DVE.sig(ins))
                    ins = nc.vector.tensor_scalar(out=eq2f.t[:, tile, :], in0=L.t[:, 2, :], scalar1=s_.t[:, 1:2], scalar2=None, op0=ALU.is_equal)
                    DVE.wait(DVE.sig(ins))
                    ins = nc.vector.tensor_tensor(out=a_bf.t[:, tile, :], in0=eq1f.t[:, tile, :], in1=eq2f.t[:, tile, :], op=ALU.add)
                    a_bf.w["dve"] = DVE.sig(ins)
                    ins = nc.vector.tensor_tensor(out=s_.t[:, 2:3], in0=s_.t[:, 1:2], in1=s_.t[:, 0:1], op=ALU.subtract)
                    tok = DVE.sig(ins)
                    s_.w["dve"] = tok
                    ACT.rdep(s_)
                    ins = nc.scalar.activation(out=wts.t[:, tile, 0:1], in_=s_.t[:, 2:3], func=AF.Sigmoid, scale=-1.0)
                    ACT.sig(ins)
                    ins = nc.scalar.activation(out=wts.t[:, tile, 1:2], in_=s_.t[:, 2:3], func=AF.Sigmoid, scale=1.0)
                    tok = ACT.sig(ins)
                    wts.w["act"] = tok
                    s_.r["act"] = tok
                    L.r["dve"] = a_bf.w["dve"]
                eq1f.w["dve"] = a_bf.w["dve"]
                eq2f.w["dve"] = a_bf.w["dve"]
                PE.rdep(a_bf)
                PE.rdep(triS)
                PE.rdep(ones_bf)
                for tile in range(NT):
                    bk = self.next_bank()
                    bk.new_gen()
                    PE.wdep(bk)
                    ins = nc.tensor.matmul(bk.t[:, 0:NE], lhsT=triS.t[:], rhs=a_bf.t[:, tile, :], start=True, stop=(tile == 0))
                    for pt in range(tile):
                        ins = nc.tensor.matmul(bk.t[:, 0:NE], lhsT=ones_bf.t[:], rhs=a_bf.t[:, pt, :], start=False, stop=(pt == tile - 1))
                    tok = self.pe_sig(ins)
                    bk.w["pe"] = tok
                    DVE.rdep(bk)
                    ins = nc.vector.tensor_copy(out=cnt.t[:, tile, :], in_=bk.t[:, 0:NE])
                    tok = DVE.sig(ins)
                    bk.r["dve"] = tok
                    cnt.w["dve"] = tok
                bk = self.next_bank()
                bk.new_gen()
                PE.wdep(bk)
                for tile in range(NT):
                    ins = nc.tensor.matmul(bk.t[:, 0:NE], lhsT=ones_bf.t[:], rhs=a_bf.t[:, tile, :], start=(tile == 0), stop=(tile == NT - 1))
                tok = self.pe_sig(ins)
                bk.w["pe"] = tok
                a_bf.r["pe"] = tok
                DVE.rdep(bk)
                ins = nc.vector.tensor_copy(out=ntot.t[:], in_=bk.t[:, 0:NE])
                tok = DVE.sig(ins)
                bk.r["dve"] = tok
                DVE.wait(tok)
                ins = nc.vector.memset(padded.t[:], 0.0)
                DVE.wait(DVE.sig(ins))
                for m in range(T // G):
                    ins = nc.vector.scalar_tensor_tensor(out=padded.t[:], in0=ntot.t[:], scalar=float(G * m), in1=padded.t[:],
                                                         op0=ALU.is_gt, op1=ALU.add)
                    DVE.wait(DVE.sig(ins))
                ins = nc.vector.tensor_scalar(out=padded.t[:], in0=padded.t[:], scalar1=float(G), scalar2=None, op0=ALU.mult)
                DVE.wait(DVE.sig(ins))
                ins = nc.vector.memset(base.t[:], 0.0)
                DVE.wait(DVE.sig(ins))
                for e in range(1, NE):
                    ins = nc.vector.tensor_tensor(out=base.t[:, e:e + 1], in0=base.t[:, e - 1:e], in1=padded.t[:, e - 1:e], op=ALU.add)
                    DVE.wait(DVE.sig(ins))
                ins = nc.vector.tensor_tensor(out=endt.t[:], in0=base.t[:], in1=padded.t[:], op=ALU.add)
                DVE.wait(DVE.sig(ins))
                tmp8 = lg.next()
                for gi in range(NG):
                    ins = nc.vector.tensor_scalar(out=tmp8.t[:, 0, :], in0=endt.t[:], scalar1=float(gi * G), scalar2=None, op0=ALU.is_le)
                    DVE.wait(DVE.sig(ins))
                    ins = nc.vector.reduce_sum(out=etab.t[:, gi:gi + 1], in_=tmp8.t[:, 0, :], axis=AX.X)
                    DVE.wait(DVE.sig(ins))
                ins = nc.vector.tensor_scalar(out=etab.t[:], in0=etab.t[:], scalar1=float(NE - 1), scalar2=None, op0=ALU.min)
                etab.w["dve"] = DVE.sig(ins)
                DVE.wait(etab.w["dve"])
                for tile in range(NT):
                    ins = nc.vector.tensor_tensor(out=tmp8.t[:, 1, :], in0=cnt.t[:, tile, :], in1=base.t[:], op=ALU.add)
                    DVE.wait(DVE.sig(ins))
                    ins = nc.vector.tensor_tensor(out=tmp8.t[:, 2, :], in0=tmp8.t[:, 1, :], in1=eq1f.t[:, tile, :], op=ALU.mult)
                    DVE.sig(ins)
                    ins = nc.vector.tensor_tensor(out=tmp8.t[:, 3, :], in0=tmp8.t[:, 1, :], in1=eq2f.t[:, tile, :], op=ALU.mult)
                    DVE.wait(DVE.sig(ins))
                    ins = nc.vector.reduce_sum(out=slots_f.t[:, tile, 0:1], in_=tmp8.t[:, 2, :], axis=AX.X)
                    DVE.sig(ins)
                    ins = nc.vector.reduce_sum(out=slots_f.t[:, tile, 1:2], in_=tmp8.t[:, 3, :], axis=AX.X)
                    DVE.wait(DVE.sig(ins))
                ins = nc.vector.tensor_copy(out=slots_i.t[:], in_=slots_f.t[:])
                slots_i.w["dve"] = DVE.sig(ins)
                POOL.rdep(slots_i)
                for tile in range(NT):
                    hn = hn_ring.next()
                    hn.new_gen()
                    SP.wdep(hn)
                    SP.wait(hn_tok[tile])
                    ins = nc.sync.dma_start(out=hn.t[:], in_=hn_d[tile * 128:(tile + 1) * 128, :])
                    hn.w["dma"] = hn.dma_sig(ins)
                    POOL.rdep(hn)
                    for j in range(2):
                        ins = nc.gpsimd.indirect_dma_start(out=Xg[:, :], out_offset=bass.IndirectOffsetOnAxis(ap=slots_i.t[:, tile, j:j + 1], axis=0),
                                                           in_=hn.t[:], in_offset=None)
                        tok = hn.dma_sig(ins)
                        hn.r["sc"] = tok
                        self.track_dma(tok)
            self.barrier()
            with ExitStack() as st:
                xg_ring = Ring([self.sb(f"xg{i}", [128, D], BF16, st) for i in range(2)])
                xT = Ring([self.sb(f"gxT{i}", [128, KC, G], BF16, st) for i in range(2)])
                hT = self.sb("ghT", [128, nch, G], BF16, st)
                sring = Ring([self.sb(f"gs{i}", [128, G], BF16, st) for i in range(3)])
                ysb = Ring([self.sb(f"ysb{i}", [128, 1024], F32, st) for i in range(3)])
                idxs = Ring([self.sb(f"idx{i}", [128, 3, nch], I32, st) for i in range(2)])
                idxf = self.sb("idxf", [128, nch], F32, st)
                ecol = self.sb("ecol", [128, 1], F32, st)
                for gi in range(NG):
                    ix = idxs.next()
                    ix.new_gen()
                    DVE.wdep(ix)
                    DVE.rdep(etab)
                    DVE.rdep(iota_f)
                    ins = nc.vector.tensor_scalar(out=ecol.t[:], in0=etab.t[:, gi:gi + 1], scalar1=float(self.DFFE), scalar2=None, op0=ALU.mult)
                    DVE.wait(DVE.sig(ins))
                    ins = nc.vector.tensor_scalar(out=idxf.t[:], in0=iota_f.t[:], scalar1=ecol.t[:, 0:1], scalar2=None, op0=ALU.add)
                    DVE.wait(DVE.sig(ins))
                    ins = nc.vector.tensor_copy(out=ix.t[:, 0, :], in_=idxf.t[:])
                    DVE.sig(ins)
                    ins = nc.vector.tensor_scalar(out=ix.t[:, 1, :], in0=idxf.t[:], scalar1=2.0, scalar2=None, op0=ALU.mult)
                    DVE.sig(ins)
                    ins = nc.vector.tensor_scalar(out=ix.t[:, 2, :], in0=idxf.t[:], scalar1=2.0, scalar2=1.0, op0=ALU.mult, op1=ALU.add)
                    tok = DVE.sig(ins)
                    ix.w["dve"] = tok
                    DVE.wait(tok)
                    x_T = xT.next()
                    x_T.new_gen()
                    for t in range(G // 128):
                        xg = xg_ring.next()
                        xg.new_gen()
                        SP.wdep(xg)
                        ins = nc.sync.dma_start(out=xg.t[:], in_=Xg[gi * G + t * 128: gi * G + (t + 1) * 128, :])
                        xg.w["dma"] = xg.dma_sig(ins)
                        PE.rdep(xg)
                        for j in range(2):
                            bk = self.next_bank()
                            bk.new_gen()
                            PE.wdep(bk)
                            pv = bk.t[:].bitcast(BF16)
                            for i in range(8):
                                k = 8 * j + i
                                ins = nc.tensor.transpose(out=pv[:, i * 128:(i + 1) * 128], in_=xg.t[:, k * 128:(k + 1) * 128],
                                                          identity=self.ident_bf.t[:])
                            tok = self.pe_sig(ins)
                            bk.w["pe"] = tok
                            xg.r["pe"] = tok
                            E = ACT if j == 0 else DVE
                            E.rdep(bk)
                            E.wdep(x_T)
                            if E is ACT:
                                ins = nc.scalar.copy(out=x_T.t[:, 8 * j:8 * j + 8, t * 128:(t + 1) * 128], in_=pv.rearrange("p (j q) -> p j q", q=128))
                            else:
                                ins = nc.vector.tensor_copy(out=x_T.t[:, 8 * j:8 * j + 8, t * 128:(t + 1) * 128], in_=pv.rearrange("p (j q) -> p j q", q=128))
                            tok = E.sig(ins)
                            bk.r[E.name] = tok
                            x_T.w[E.name] = tok
                    self.swiglu_group(x_T, hT, nch, lambda c, ix=ix: (w1tab, ix.t[:, 0, c:c + 1], ix),
                                      lambda c, ix=ix: (w3tab, ix.t[:, 0, c:c + 1], ix), sring)

                    def evac_y(t, half, bks2, gi=gi):
                        yb = ysb.next()
                        yb.new_gen()
                        for n in range(2):
                            E = ACT if n == 0 else DVE
                            E.wdep(yb)
                            E.rdep(bks2[n])
                            if E is ACT:
                                ins = nc.scalar.copy(out=yb.t[:, n * 512:(n + 1) * 512], in_=bks2[n].t[:, :])
                            else:
                                ins = nc.vector.tensor_copy(out=yb.t[:, n * 512:(n + 1) * 512], in_=bks2[n].t[:, :])
                            tok = E.sig(ins)
                            yb.w[E.name] = tok
                            bks2[n].r[E.name] = tok
                        SP.rdep(yb)
                        ins = nc.sync.dma_start(out=Yg[gi * G + t * 128: gi * G + (t + 1) * 128, half * 1024:(half + 1) * 1024], in_=yb.t[:])
                        tok = yb.dma_sig(ins)
                        yb.r["dma"] = tok
                        self.track_dma(tok)
                    self.pass_B(hT, nch, lambda c_, half, ix=ix: (w2tab, ix.t[:, 1 + half, c_:c_ + 1], ix), G // 128, evac_y)
            self.barrier()
            with ExitStack() as st:
                self.make_resid_bufs(st)
                xc = Ring([self.sb(f"cx{i}", [128, D], F32, st) for i in range(2)])
                y1r = Ring([self.sb(f"cy1{i}", [128, D], F32, st) for i in range(2)])
                y2r = Ring([self.sb(f"cy2{i}", [128, D], F32, st) for i in range(2)])
                for tile in range(NT):
                    b = (tile * 128) // S
                    self.load_gate(1, 1, b)
                    x_ = xc.next()
                    x_.new_gen()
                    SP.wdep(x_)
                    SP.waits([self.xres_tok.get((tile, 0)), self.xres_tok.get((tile, 1))])
                    src = self.x_in if self.x0_is_input else self.xres
                    ins = nc.sync.dma_start(out=x_.t[:], in_=src[tile * 128:(tile + 1) * 128, :])
                    x_.w["dma"] = x_.dma_sig(ins)
                    ys = []
                    for j, ring in enumerate((y1r, y2r)):
                        y_ = ring.next()
                        y_.new_gen()
                        POOL.wdep(y_)
                        ins = nc.gpsimd.indirect_dma_start(out=y_.t[:], out_offset=None, in_=Yg[:, :],
                                                           in_offset=bass.IndirectOffsetOnAxis(ap=slots_i.t[:, tile, j:j + 1], axis=0))
                        y_.w["dma"] = y_.dma_sig(ins)
                        ys.append(y_)
                    DVE.rdep(ys[0])
                    DVE.rdep(ys[1])
                    DVE.rdep(x_)
                    DVE.rdep(self.gate)
                    DVE.rdep(wts)
                    ins = nc.vector.tensor_scalar(out=ys[0].t[:], in0=ys[0].t[:], scalar1=wts.t[:, tile, 0:1], scalar2=None, op0=ALU.mult)
                    DVE.wait(DVE.sig(ins))
                    ins = nc.vector.scalar_tensor_tensor(out=ys[0].t[:], in0=ys[1].t[:], scalar=wts.t[:, tile, 1:2], in1=ys[0].t[:],
                                                         op0=ALU.mult, op1=ALU.add)
                    tok = DVE.sig(ins)
                    ys[1].r["dve"] = tok
                    DVE.wait(tok)
                    ins = nc.vector.tensor_tensor(out=ys[0].t[:], in0=ys[0].t[:], in1=self.gate.t[:], op=ALU.mult)
                    tok = DVE.sig(ins)
                    self.gate.r["dve"] = tok
                    DVE.wait(tok)
                    ins = nc.vector.tensor_tensor(out=x_.t[:], in0=x_.t[:], in1=ys[0].t[:], op=ALU.add)
                    tok = DVE.sig(ins)
                    ys[0].r["dve"] = tok
                    x_.w["dve"] = tok
                    SP.rdep(x_)
                    ins = nc.sync.dma_start(out=self.xres[tile * 128:(tile + 1) * 128, :], in_=x_.t[:])
                    tok = x_.dma_sig(ins)
                    x_.r["dma"] = tok
                    self.track_dma(tok)
                    self.new_xres_tok[(tile, 0)] = tok
                    self.new_xres_tok[(tile, 1)] = tok
                self.commit_xres()
        self.x0_is_input = False

    def phase_final(self):
        nc = self.nc
        PE, ACT, DVE, POOL, SP = self.PE, self.ACT, self.DVE, self.POOL, self.SP
        with ExitStack() as st:
            xt_ring = Ring([self.sb(f"fxt{i}", [128, D], F32, st) for i in range(3)])
            junk = self.sb("fjunk", [128, D], BF16, st)
            ss_ring = Ring([self.sb(f"fss{i}", [128, 2], F32, st) for i in range(4)])
            fg = self.fin_g
            ins = nc.sync.dma_start(out=fg.t[:], in_=self.norm_final.partition_broadcast(128))
            fg.w["dma"] = fg.dma_sig(ins)
            self.track_dma(fg.w["dma"])
            for tile in range(self.NT):
                xt = xt_ring.next()
                xt.new_gen()
                SP.wdep(xt)
                if self.x0_is_input:
                    srcap = self.x_in[tile * 128:(tile + 1) * 128, :]
                else:
                    srcap = self.xres[tile * 128:(tile + 1) * 128, :]
                    SP.waits([self.xres_tok.get((tile, 0)), self.xres_tok.get((tile, 1))])
                ins = nc.sync.dma_start(out=xt.t[:], in_=srcap)
                xt.w["dma"] = xt.dma_sig(ins)
                ss = ss_ring.next()
                ss.new_gen()
                junk.new_gen()
                ACT.wdep(ss)
                ACT.wdep(junk)
                ACT.rdep(xt)
                ins = nc.scalar.memzero(ss.t[:])
                ACT.wait(ACT.sig(ins))
                ins = nc.scalar.activation(out=junk.t[:], in_=xt.t[:], func=AF.Square, accum_out=ss.t[:, 0:1])
                tok = ACT.sig(ins)
                ss.w["act"] = tok
                junk.w["act"] = tok
                ACT.wait(tok)
                ins = nc.scalar.activation(out=ss.t[:, 1:2], in_=ss.t[:, 0:1], func=AF.Sqrt, scale=1.0 / D, bias=self.eps_col.t[:, 0:1])
                ss.w["act"] = ACT.sig(ins)
                DVE.rdep(ss)
                ins = nc.vector.reciprocal(out=ss.t[:, 1:2], in_=ss.t[:, 1:2])
                DVE.wait(DVE.sig(ins))
                DVE.rdep(fg)
                DVE.rdep(xt)
                ins = nc.vector.scalar_tensor_tensor(out=xt.t[:], in0=xt.t[:], scalar=ss.t[:, 1:2], in1=fg.t[:], op0=ALU.mult, op1=ALU.mult)
                tok = DVE.sig(ins)
                ss.r["dve"] = tok
                xt.w["dve"] = tok
                SP.rdep(xt)
                ins = nc.sync.dma_start(out=self.out[tile * 128:(tile + 1) * 128, :], in_=xt.t[:])
                tok = xt.dma_sig(ins)
                xt.r["dma"] = tok
                self.track_dma(tok)


def relayout_A(w, nchunks):
    w = np.asarray(w, dtype=np.float32)
    return np.ascontiguousarray(w.reshape(KC, 128, nchunks, 128).transpose(2, 1, 0, 3)).reshape(nchunks, 128, D)


def prepare_inputs(inp, NE=8):
    shared = {}
    shared["ada_w"] = np.ascontiguousarray(inp["ada_w"], dtype=np.float32)
    shared["ada_b"] = np.ascontiguousarray(inp["ada_b"], dtype=np.float32)
    shared["norm_mix"] = np.ascontiguousarray(inp["norm_mix"], dtype=np.float32)
    shared["norm_ffn"] = np.ascontiguousarray(inp["norm_ffn"], dtype=np.float32)
    shared["norm_final"] = np.ascontiguousarray(inp["norm_final"], dtype=np.float32)
    shared["conv_w_in"] = relayout_A(inp["conv_w_in"][0], 48)
    shared["conv_kernel"] = np.ascontiguousarray(inp["conv_kernel"][0], dtype=np.float32)
    shared["conv_w_out"] = np.ascontiguousarray(inp["conv_w_out"][0], dtype=np.float32)
    fw = np.asarray(inp["fox_w_in"][0], dtype=np.float32)
    shared["fox_qk"] = relayout_A(fw[:, 0:2 * D], 32)
    shared["fox_v"] = np.ascontiguousarray(fw[:, 2 * D:3 * D])
    shared["fox_f"] = np.ascontiguousarray(fw[:, 3 * D:3 * D + NH])
    shared["fox_b_f"] = np.ascontiguousarray(inp["fox_b_f"][0], dtype=np.float32)
    shared["fox_w_out"] = np.ascontiguousarray(inp["fox_w_out"][0], dtype=np.float32)
    dff = inp["ffn_w1"].shape[-1]
    shared["ffn_w1"] = relayout_A(inp["ffn_w1"][0], dff // 128)
    shared["ffn_w3"] = relayout_A(inp["ffn_w3"][0], dff // 128)
    shared["ffn_w2"] = np.ascontiguousarray(inp["ffn_w2"][0], dtype=np.float32)
    shared["moe_router"] = np.ascontiguousarray(inp["moe_router"][0][:, :NE], dtype=np.float32)
    dffe = inp["moe_w1"].shape[-1]
    shared["moe_w1"] = np.concatenate([relayout_A(inp["moe_w1"][0][e], dffe // 128) for e in range(NE)], axis=0)
    shared["moe_w3"] = np.concatenate([relayout_A(inp["moe_w3"][0][e], dffe // 128) for e in range(NE)], axis=0)
    shared["moe_w2"] = np.ascontiguousarray(np.asarray(inp["moe_w2"][0][:NE], dtype=np.float32).reshape(NE * dffe, D))
    return shared


def kernel(**inputs):
    x = np.asarray(inputs["x"], dtype=np.float32)
    c = np.asarray(inputs["c"], dtype=np.float32)
    ncores = 8
    bsz, S, _ = x.shape
    nseq = bsz // ncores
    shared = prepare_inputs(inputs)
    kb = KB(NSEQ=nseq, S=S)
    nc = kb.build()
    in_maps = []
    for i in range(ncores):
        m = dict(shared)
        m["x"] = np.ascontiguousarray(x[i * nseq:(i + 1) * nseq].reshape(nseq * S, D))
        m["c"] = np.ascontiguousarray(c[i * nseq:(i + 1) * nseq])
        in_maps.append(m)
    res = run_bass_kernel_spmd(nc, in_maps, core_ids=list(range(ncores)))
    out = np.concatenate([r["out"].reshape(nseq, S, D) for r in res.results], axis=0)
    return out.astype(np.float32, copy=False)
```

```python
import numpy as np
from contextlib import ExitStack

import concourse.bass as bass
import concourse.mybir as mybir
from concourse.bass_utils import run_bass_kernel_spmd

F32 = mybir.dt.float32
BF16 = mybir.dt.bfloat16
AF = mybir.ActivationFunctionType
ALU = mybir.AluOpType
AX = mybir.AxisListType

D = 2048
KC = 16
HD = 128
NH = 16
EPS = 1e-6
SEM_LIMIT = 12000
G = 512


class Buf:
    def __init__(self, kb, t, name):
        self.kb = kb
        self.t = t
        self.name = name
        self.w = {}
        self.r = {}
        self.prev = []
        self.dsem = None
        self.dcount = 0

    def new_gen(self):
        self.prev = list(self.w.values()) + list(self.r.values())
        self.w = {}
        self.r = {}

    def dma_sig(self, ins):
        if self.dsem is None:
            self.dsem = self.kb.new_sem("d_" + self.name)
        self.dcount += 16
        ins.then_inc(self.dsem, 16)
        return (self.dsem, self.dcount)


class Eng:
    def __init__(self, kb, name, eng):
        self.kb = kb
        self.name = name
        self.eng = eng
        self.sem = None
        self.count = 0
        self.nsem = 0
        self.waited = {}

    def sig(self, ins):
        if self.sem is None or self.count >= SEM_LIMIT:
            self.sem = self.kb.new_sem(f"e_{self.name}{self.nsem}")
            self.nsem += 1
            self.count = 0
        self.count += 1
        ins.then_inc(self.sem, 1)
        return (self.sem, self.count)

    def wait(self, tok):
        if tok is None:
            return
        sem, val = tok
        key = id(sem)
        if self.waited.get(key, 0) >= val:
            return
        self.eng.wait_ge(sem, val)
        self.waited[key] = val

    def waits(self, toks):
        for t in toks:
            self.wait(t)

    def wdep(self, buf):
        self.waits(buf.prev)

    def rdep(self, buf):
        self.waits(buf.w.values())


class Ring:
    def __init__(self, bufs):
        self.bufs = bufs
        self.i = 0

    def next(self):
        b = self.bufs[self.i % len(self.bufs)]
        self.i += 1
        return b


class KB:
    def __init__(self, NSEQ=2, S=2048, NE=8, DFF=5632, DFFE=7168, phases=("ada", "conv", "ffn", "attn", "moe", "final"),
                 debug=False, moe_sparse=True):
        self.moe_sparse = moe_sparse
        self.NSEQ, self.S, self.NE, self.DFF, self.DFFE = NSEQ, S, NE, DFF, DFFE
        self.T = NSEQ * S
        self.NT = self.T // 128
        self.phases = phases
        self.debug = debug
        self.nc = bass.Bass("TRN2", target_bir_lowering=False)
        self.es = ExitStack()
        self.sems = []
        self.dma_toks = []
        nc = self.nc
        self.PE = Eng(self, "pe", nc.tensor)
        self.ACT = Eng(self, "act", nc.scalar)
        self.DVE = Eng(self, "dve", nc.vector)
        self.POOL = Eng(self, "pool", nc.gpsimd)
        self.SP = Eng(self, "sp", nc.sync)
        self.engs = [self.PE, self.ACT, self.DVE, self.POOL, self.SP]

    def new_sem(self, name):
        s = self.es.enter_context(self.nc.semaphore(name))
        self.sems.append(s)
        return s

    def sb(self, name, shape, dtype, stack=None):
        self.uid = getattr(self, "uid", 0) + 1
        name = f"{name}_{self.uid}"
        t = (stack or self.es).enter_context(self.nc.sbuf_tensor(name, shape, dtype))
        return Buf(self, t, name)

    def dram(self, name, shape, dtype, kind="Internal"):
        t = self.nc.dram_tensor(name, shape, dtype, kind=kind)
        return t.ap()

    def barrier(self):
        nc = self.nc
        toks = []
        toks.append(self.DVE.sig(nc.vector.memset(self.bar_dve.t[:], 0.0)))
        toks.append(self.ACT.sig(nc.scalar.copy(out=self.bar_act.t[:], in_=self.bar_src.t[:])))
        toks.append(self.POOL.sig(nc.gpsimd.memset(self.bar_pool.t[:], 0.0)))
        if self.last_pe_tok is not None:
            toks.append(self.last_pe_tok)
        toks += self.dma_toks
        self.dma_toks = []
        for e in self.engs:
            e.waits(toks)

    def track_dma(self, tok):
        self.dma_toks.append(tok)
        if len(self.dma_toks) > 64:
            last = {}
            for s, v in self.dma_toks:
                if id(s) not in last or last[id(s)][1] < v:
                    last[id(s)] = (s, v)
            self.dma_toks = list(last.values())

    def build(self):
        nc = self.nc
        NSEQ, T, NE = self.NSEQ, self.T, self.NE
        self.last_pe_tok = None
        P = self.phases
        self.input_names = []

        def inp(name, shape, need=True):
            if not need:
                return None
            self.input_names.append(name)
            return self.dram(name, shape, F32, "ExternalInput")

        self.x_in = inp("x", [T, D])
        self.c_in = inp("c", [NSEQ, D], "ada" in P)
        self.ada_w = inp("ada_w", [2, D, 6 * D], "ada" in P)
        self.ada_b = inp("ada_b", [2, 6 * D], "ada" in P)
        self.norm_mix = inp("norm_mix", [2, D], "ada" in P)
        self.norm_ffn = inp("norm_ffn", [2, D], "ada" in P)
        self.norm_final = inp("norm_final", [D], "final" in P)
        self.conv_w_in = inp("conv_w_in", [48, 128, D], "conv" in P)
        self.conv_kernel = inp("conv_kernel", [3, D], "conv" in P)
        self.conv_w_out = inp("conv_w_out", [D, D], "conv" in P)
        self.fox_qk = inp("fox_qk", [32, 128, D], "attn" in P)
        self.fox_v = inp("fox_v", [D, D], "attn" in P)
        self.fox_f = inp("fox_f", [D, NH], "attn" in P)
        self.fox_b_f = inp("fox_b_f", [NH], "attn" in P)
        self.fox_w_out = inp("fox_w_out", [D, D], "attn" in P)
        self.ffn_w1 = inp("ffn_w1", [self.DFF // 128, 128, D], "ffn" in P)
        self.ffn_w3 = inp("ffn_w3", [self.DFF // 128, 128, D], "ffn" in P)
        self.ffn_w2 = inp("ffn_w2", [self.DFF, D], "ffn" in P)
        self.moe_router = inp("moe_router", [D, NE], "moe" in P)
        self.moe_w1 = inp("moe_w1", [NE * (self.DFFE // 128), 128, D], "moe" in P)
        self.moe_w3 = inp("moe_w3", [NE * (self.DFFE // 128), 128, D], "moe" in P)
        self.moe_w2 = inp("moe_w2", [NE * self.DFFE, D], "moe" in P)
        self.out = self.dram("out", [T, D], F32, "ExternalOutput")
        self.xres = self.dram("xres", [T, D], F32, "ExternalOutput" if self.debug else "Internal")
        self.mod_d = self.dram("mod_d", [2, NSEQ, 6 * D], F32, "ExternalOutput" if self.debug else "Internal")
        self.xres_tok = {}
        self.x0_is_input = True

        self.ident_bf = self.sb("ident_bf", [128, 128], BF16)
        self.ident_f = self.sb("ident_f", [128, 128], F32)
        self.bar_dve = self.sb("bar_dve", [128, 1], F32)
        self.bar_act = self.sb("bar_act", [128, 1], F32)
        self.bar_pool = self.sb("bar_pool", [128, 1], F32)
        self.bar_src = self.sb("bar_src", [128, 1], F32)
        self.eps_col = self.sb("eps_col", [128, 1], F32)
        self.wring = Ring([self.sb(f"w{i}", [128, D], BF16) for i in range(8)])
        self.modcols = self.sb("modcols", [128, 2 * 2 * NSEQ * 2, KC], F32)
        self.fin_g = self.sb("fin_g", [128, D], F32)
        self.banks = []
        for i in range(8):
            t = self.es.enter_context(nc.psum_tensor(f"bank{i}", [128, 512], F32))
            self.banks.append(Buf(self, t, f"bank{i}"))
        self.bank_i = 0

        for idt in (self.ident_bf, self.ident_f):
            nc.gpsimd.memset(idt.t[:], 0.0)
            ins = nc.gpsimd.affine_select(out=idt.t[:], in_=idt.t[:], pattern=[[-1, 128]],
                                          compare_op=ALU.not_equal, fill=1.0, base=0, channel_multiplier=1)
            idt.w["pool"] = self.POOL.sig(ins)
        nc.gpsimd.memset(self.eps_col.t[:], EPS)
        ins = nc.gpsimd.memset(self.bar_src.t[:], 0.0)
        self.bar_src.w["pool"] = self.POOL.sig(ins)
        self.eps_col.w["pool"] = self.bar_src.w["pool"]
        self.ACT.rdep(self.bar_src)
        self.ACT.rdep(self.eps_col)

        self.precast_layer0()
        if "ada" in self.phases:
            self.phase_ada()
            self.barrier()
        if "conv" in self.phases:
            self.phase_conv()
            self.barrier()
        if "ffn" in self.phases:
            self.phase_ffn()
            self.barrier()
        if "attn" in self.phases:
            self.phase_attn()
            self.barrier()
        if "moe" in self.phases:
            if self.moe_sparse:
                self.phase_moe_sparse()
            else:
                self.phase_moe()
            self.barrier()
        if "final" in self.phases:
            self.phase_final()
        self.barrier()
        self.es.close()
        return nc

    def next_bank(self):
        b = self.banks[self.bank_i % 8]
        self.bank_i += 1
        return b

    def wload(self, src_ap, cols=D):
        b = self.wring.next()
        b.new_gen()
        self.POOL.wdep(b)
        if isinstance(src_ap, tuple):
            table, idx_ap, idx_buf = src_ap
            self.POOL.rdep(idx_buf)
            ins = self.nc.gpsimd.indirect_dma_start(out=b.t[:, 0:cols], out_offset=None, in_=table,
                                                    in_offset=bass.IndirectOffsetOnAxis(ap=idx_ap, axis=0))
            idx_buf.r["pool_dma"] = None
        else:
            ins = self.nc.gpsimd.dma_start(out=b.t[:, 0:cols], in_=src_ap)
        b.w["dma"] = b.dma_sig(ins)
        if isinstance(src_ap, tuple):
            src_ap[2].r["wdma" + b.name] = b.w["dma"]
        self.track_dma(b.w["dma"])
        return b

    def pe_sig(self, ins):
        tok = self.PE.sig(ins)
        self.last_pe_tok = tok
        return tok

    def modcol(self, layer, sub, b):
        idx = ((layer * 2 + sub) * self.NSEQ + b) * 2
        return self.modcols.t[:, idx, :], self.modcols.t[:, idx + 1, :]

    def precast_layer0(self):
        nc = self.nc
        todo = []
        if "conv" in self.phases:
            todo += [("conv_w_in", self.conv_w_in.rearrange("c p f -> (c p) f")), ("conv_w_out", self.conv_w_out)]
        if "ffn" in self.phases:
            todo += [("ffn_w1", self.ffn_w1.rearrange("c p f -> (c p) f")), ("ffn_w3", self.ffn_w3.rearrange("c p f -> (c p) f")),
                     ("ffn_w2", self.ffn_w2)]
        self.pc_sem = None
        npc = 0
        for name, src in todo:
            rows = src.shape[0]
            dst = self.dram(name + "_bf", [rows, D], BF16)
            if self.pc_sem is None:
                self.pc_sem = self.new_sem("precast")
            for r in range(0, rows, 128):
                ins = nc.gpsimd.dma_start(out=dst[r:r + 128, :], in_=src[r:r + 128, :])
                ins.then_inc(self.pc_sem, 16)
                npc += 1
            if name in ("conv_w_in", "ffn_w1", "ffn_w3"):
                setattr(self, name, dst.rearrange("(c p) f -> c p f", p=128))
            else:
                setattr(self, name, dst)
        if npc:
            self.track_dma((self.pc_sem, 16 * npc))

    def phase_ada(self):
        nc = self.nc
        NSEQ = self.NSEQ
        PE, ACT, DVE, POOL, SP = self.PE, self.ACT, self.DVE, self.POOL, self.SP
        with ExitStack() as st:
            cT = self.sb("cT", [128, KC, NSEQ], F32, st)
            cTa = self.sb("cTa", [128, KC, NSEQ], BF16, st)
            bias = Ring([self.sb(f"adab{i}", [NSEQ, D], F32, st) for i in range(2)])
            mrow = Ring([self.sb(f"mrow{i}", [NSEQ, D], F32, st) for i in range(2)])
            cols = self.sb("adacols", [128, 3, KC], F32, st)
            with nc.allow_non_contiguous_dma(reason="tiny transposed load of conditioning vector"):
                for b in range(NSEQ):
                    ins = nc.sync.dma_start(out=cT.t[:, :, b], in_=self.c_in[b, :].rearrange("(k p) -> p k", p=128))
                    cT.w["dma"] = cT.dma_sig(ins)
            ACT.rdep(cT)
            ins = nc.scalar.activation(out=cTa.t[:], in_=cT.t[:], func=AF.Silu)
            cTa.w["act"] = ACT.sig(ins)
            mod_store_toks = []
            for layer in range(2):
                for n in range(6):
                    bt = bias.next()
                    bt.new_gen()
                    SP.wdep(bt)
                    for b in range(NSEQ):
                        ins = nc.sync.dma_start(out=bt.t[b:b + 1, :], in_=self.ada_b[layer:layer + 1, n * D:(n + 1) * D])
                        bt.w["dma"] = bt.dma_sig(ins)
                    bks = [self.next_bank() for _ in range(4)]
                    for bk in bks:
                        bk.new_gen()
                        PE.wdep(bk)
                    PE.rdep(cTa)
                    for k in range(KC):
                        wb = self.wload(self.ada_w[layer, k * 128:(k + 1) * 128, n * D:(n + 1) * D])
                        PE.rdep(wb)
                        for j in range(4):
                            ins = nc.tensor.matmul(bks[j].t[0:NSEQ, :], lhsT=cTa.t[:, k, :], rhs=wb.t[:, j * 512:(j + 1) * 512],
                                                   start=(k == 0), stop=(k == KC - 1))
                        wb.r["pe"] = self.pe_sig(ins)
                    for bk in bks:
                        bk.w["pe"] = wb.r["pe"]
                    mr = mrow.next()
                    mr.new_gen()
                    DVE.wdep(mr)
                    DVE.rdep(bt)
                    for j in range(4):
                        DVE.rdep(bks[j])
                        ins = nc.vector.tensor_tensor(out=mr.t[:, j * 512:(j + 1) * 512], in0=bks[j].t[0:NSEQ, :],
                                                      in1=bt.t[:, j * 512:(j + 1) * 512], op=ALU.add)
                        tok = DVE.sig(ins)
                        bks[j].r["dve"] = tok
                    mr.w["dve"] = tok
                    bt.r["dve"] = tok
                    SP.rdep(mr)
                    ins = nc.sync.dma_start(out=self.mod_d[layer, :, n * D:(n + 1) * D], in_=mr.t[:])
                    tok = mr.dma_sig(ins)
                    mr.r["dma"] = tok
                    mod_store_toks.append(tok)
                    self.track_dma(tok)
            SP.waits(mod_store_toks)
            with nc.allow_non_contiguous_dma(reason="tiny transposed loads of modulation vectors"):
                for layer in range(2):
                    for sub in range(2):
                        gsrc = (self.norm_mix if sub == 0 else self.norm_ffn)[layer, :]
                        for b in range(NSEQ):
                            cols.new_gen()
                            SP.wdep(cols)
                            srcs = [gsrc, self.mod_d[layer, b, (3 * sub + 1) * D:(3 * sub + 2) * D],
                                    self.mod_d[layer, b, (3 * sub) * D:(3 * sub + 1) * D]]
                            for i, s_ in enumerate(srcs):
                                ins = nc.sync.dma_start(out=cols.t[:, i, :], in_=s_.rearrange("(k p) -> p k", p=128))
                                cols.w["dma"] = cols.dma_sig(ins)
                            self.track_dma(cols.w["dma"])
                            A, B = self.modcol(layer, sub, b)
                            DVE.rdep(cols)
                            ins = nc.vector.scalar_tensor_tensor(out=A, in0=cols.t[:, 1, :], scalar=1.0, in1=cols.t[:, 0, :],
                                                                 op0=ALU.add, op1=ALU.mult)
                            DVE.sig(ins)
                            ins = nc.vector.tensor_copy(out=B, in_=cols.t[:, 2, :])
                            tok = DVE.sig(ins)
                            cols.r["dve"] = tok
                            self.modcols.w["dve"] = tok
            self.mod_ready = tok

    def make_prep_bufs(self, st):
        self.xt_ring = Ring([self.sb(f"xt{i}", [128, D], F32, st) for i in range(2)])
        self.xs_ring = Ring([self.sb(f"xs{i}", [128, D], BF16, st) for i in range(2)])
        self.junk = self.sb("junk", [128, D], BF16, st)
        self.ss_ring = Ring([self.sb(f"ss{i}", [128, 2], F32, st) for i in range(4)])

    def prep(self, tile, A, B, dstT, col0, src=None):
        nc = self.nc
        PE, ACT, DVE, POOL, SP = self.PE, self.ACT, self.DVE, self.POOL, self.SP
        xt = self.xt_ring.next()
        xt.new_gen()
        SP.wdep(xt)
        if self.x0_is_input:
            srcap = self.x_in[tile * 128:(tile + 1) * 128, :]
        else:
            srcap = self.xres[tile * 128:(tile + 1) * 128, :]
            SP.waits([self.xres_tok.get((tile, 0)), self.xres_tok.get((tile, 1))])
        ins = nc.sync.dma_start(out=xt.t[:], in_=srcap)
        xt.w["dma"] = xt.dma_sig(ins)
        self.track_dma(xt.w["dma"])
        ss = self.ss_ring.next()
        ss.new_gen()
        self.junk.new_gen()
        ACT.wdep(ss)
        ACT.wdep(self.junk)
        ACT.rdep(xt)
        ins = nc.scalar.memzero(ss.t[:])
        ACT.wait(ACT.sig(ins))
        ins = nc.scalar.activation(out=self.junk.t[:], in_=xt.t[:], func=AF.Square, accum_out=ss.t[:, 0:1])
        tok = ACT.sig(ins)
        ss.w["act"] = tok
        self.junk.w["act"] = tok
        xt.r["act"] = tok
        ACT.wait(tok)
        ins = nc.scalar.activation(out=ss.t[:, 1:2], in_=ss.t[:, 0:1], func=AF.Sqrt, scale=1.0 / D, bias=self.eps_col.t[:, 0:1])
        ss.w["act"] = ACT.sig(ins)
        DVE.rdep(ss)
        ins = nc.vector.reciprocal(out=ss.t[:, 1:2], in_=ss.t[:, 1:2])
        DVE.wait(DVE.sig(ins))
        xs = self.xs_ring.next()
        xs.new_gen()
        DVE.wdep(xs)
        DVE.rdep(xt)
        ins = nc.vector.tensor_scalar(out=xs.t[:], in0=xt.t[:], scalar1=ss.t[:, 1:2], scalar2=None, op0=ALU.mult)
        tok = DVE.sig(ins)
        xs.w["dve"] = tok
        xt.r["dve"] = tok
        ss.r["dve"] = tok
        PE.rdep(xs)
        PE.rdep(self.ident_bf)
        for j in range(4):
            bk = self.next_bank()
            bk.new_gen()
            PE.wdep(bk)
            pv = bk.t[:].bitcast(BF16)
            for i in range(4):
                k = 4 * j + i
                ins = nc.tensor.transpose(out=pv[:, i * 128:(i + 1) * 128], in_=xs.t[:, k * 128:(k + 1) * 128],
                                          identity=self.ident_bf.t[:])
            tok = self.pe_sig(ins)
            bk.w["pe"] = tok
            xs.r["pe"] = tok
            E = ACT if j % 2 == 0 else DVE
            E.rdep(bk)
            E.wdep(dstT)
            E.rdep(self.modcols)
            for i in range(4):
                k = 4 * j + i
                if E is ACT:
                    ins = nc.scalar.activation(out=dstT.t[:, k, col0:col0 + 128], in_=pv[:, i * 128:(i + 1) * 128],
                                               func=AF.Identity, scale=A[:, k:k + 1], bias=B[:, k:k + 1])
                else:
                    ins = nc.vector.tensor_scalar(out=dstT.t[:, k, col0:col0 + 128], in0=pv[:, i * 128:(i + 1) * 128],
                                                  scalar1=A[:, k:k + 1], scalar2=B[:, k:k + 1], op0=ALU.mult, op1=ALU.add)
            tok = E.sig(ins)
            bk.r[E.name] = tok
            dstT.w[E.name] = tok

    def make_resid_bufs(self, st):
        self.xr_ring = Ring([self.sb(f"xr{i}", [128, 1024], F32, st) for i in range(3)])
        self.tmp_ring = Ring([self.sb(f"tmp{i}", [128, 512], F32, st) for i in range(3)])
        self.gate = self.sb("gate", [128, D], F32, st)
        self.gate_key = None

    def load_gate(self, layer, sub, b):
        key = (layer, sub, b)
        if self.gate_key == key:
            return
        self.gate_key = key
        nc = self.nc
        self.gate.new_gen()
        self.SP.wdep(self.gate)
        self.SP.wait(self.mod_ready)
        src = self.mod_d[layer, b, (3 * sub + 2) * D:(3 * sub + 3) * D].partition_broadcast(128)
        ins = nc.sync.dma_start(out=self.gate.t[:], in_=src)
        self.gate.w["dma"] = self.gate.dma_sig(ins)
        self.track_dma(self.gate.w["dma"])

    def pass_B(self, hT, nch, w2src, ntiles, evac):
        nc = self.nc
        PE = self.PE
        assert ntiles == 4
        for half in range(2):
            bks = {}
            for t in range(ntiles):
                for n in range(2):
                    bk = self.next_bank()
                    bk.new_gen()
                    PE.wdep(bk)
                    bks[(t, n)] = bk
            PE.rdep(hT)
            for c in range(nch):
                wb = self.wload(w2src(c, half), cols=1024)
                PE.rdep(wb)
                for t in range(ntiles):
                    for n in range(2):
                        ins = nc.tensor.matmul(bks[(t, n)].t[:, :], lhsT=hT.t[:, c, t * 128:(t + 1) * 128],
                                               rhs=wb.t[:, n * 512:(n + 1) * 512], start=(c == 0), stop=(c == nch - 1))
                wb.r["pe"] = self.pe_sig(ins)
            hT.r["pe"] = wb.r["pe"]
            for bk in bks.values():
                bk.w["pe"] = wb.r["pe"]
            for t in range(ntiles):
                evac(t, half, [bks[(t, 0)], bks[(t, 1)]])

    def evac_resid(self, tile0, layer, sub, rowscale=None):
        nc = self.nc
        DVE, SP = self.DVE, self.SP

        def evac(t, half, bks2):
            tile = tile0 + t
            b = (tile * 128) // self.S
            self.load_gate(layer, sub, b)
            xr = self.xr_ring.next()
            xr.new_gen()
            SP.wdep(xr)
            if self.x0_is_input:
                srcap = self.x_in[tile * 128:(tile + 1) * 128, half * 1024:(half + 1) * 1024]
            else:
                srcap = self.xres[tile * 128:(tile + 1) * 128, half * 1024:(half + 1) * 1024]
                SP.wait(self.xres_tok.get((tile, half)))
                SP.wait(self.new_xres_tok.get((tile, half)))
            ins = nc.sync.dma_start(out=xr.t[:], in_=srcap)
            xr.w["dma"] = xr.dma_sig(ins)
            DVE.rdep(xr)
            DVE.rdep(self.gate)
            for n in range(2):
                bk = bks2[n]
                DVE.rdep(bk)
                tmp = self.tmp_ring.next()
                tmp.new_gen()
                DVE.wdep(tmp)
                gsl = self.gate.t[:, half * 1024 + n * 512: half * 1024 + (n + 1) * 512]
                if rowscale is None:
                    ins = nc.vector.tensor_tensor(out=tmp.t[:], in0=bk.t[:, :], in1=gsl, op=ALU.mult)
                else:
                    ins = nc.vector.scalar_tensor_tensor(out=tmp.t[:], in0=bk.t[:, :], scalar=rowscale(t), in1=gsl,
                                                         op0=ALU.mult, op1=ALU.mult)
                tok = DVE.sig(ins)
                bk.r["dve"] = tok
                tmp.w["dve"] = tok
                DVE.wait(tok)
                ins = nc.vector.tensor_tensor(out=xr.t[:, n * 512:(n + 1) * 512], in0=xr.t[:, n * 512:(n + 1) * 512],
                                              in1=tmp.t[:], op=ALU.add)
                tok = DVE.sig(ins)
                tmp.r["dve"] = tok
            self.gate.r["dve"] = tok
            xr.w["dve"] = tok
            SP.rdep(xr)
            ins = nc.sync.dma_start(out=self.xres[tile * 128:(tile + 1) * 128, half * 1024:(half + 1) * 1024], in_=xr.t[:])
            tok = xr.dma_sig(ins)
            xr.r["dma"] = tok
            self.track_dma(tok)
            self.new_xres_tok[(tile, half)] = tok
        return evac

    def commit_xres(self):
        self.xres_tok.update(self.new_xres_tok)
        self.new_xres_tok = {}

    def phase_conv(self):
        nc = self.nc
        PE, ACT, DVE, POOL, SP = self.PE, self.ACT, self.DVE, self.POOL, self.SP
        self.new_xres_tok = {}
        with ExitStack() as st:
            self.make_prep_bufs(st)
            self.make_resid_bufs(st)
            hnT = Ring([self.sb(f"hnT{i}", [128, KC, G], BF16, st) for i in range(2)])
            zT = Ring([self.sb(f"zT{i}", [128, KC, G], BF16, st) for i in range(2)])
            csb = Ring([self.sb(f"csb{i}", [128, G], F32, st) for i in range(2)])
            ub = Ring([self.sb(f"ub{i}", [128, G + 2], F32, st) for i in range(2)])
            cv = Ring([self.sb(f"cv{i}", [128, G], F32, st) for i in range(2)])
            halo = self.sb("halo", [128, KC, 2], F32, st)
            kcol = self.sb("kcol", [128, 3, KC], F32, st)
            with nc.allow_non_contiguous_dma(reason="tiny transposed load of conv taps"):
                for w in range(3):
                    ins = nc.sync.dma_start(out=kcol.t[:, w, :], in_=self.conv_kernel[w, :].rearrange("(k p) -> p k", p=128))
                    kcol.w["dma"] = kcol.dma_sig(ins)
            self.track_dma(kcol.w["dma"])
            ngroups = self.T // G
            for g in range(ngroups):
                tile0 = g * (G // 128)
                b = (g * G) // self.S
                first_in_seq = (g * G) % self.S == 0
                A, B = self.modcol(0, 0, b)
                h = hnT.next()
                h.new_gen()
                for t in range(G // 128):
                    self.prep(tile0 + t, A, B, h, t * 128)
                z = zT.next()
                z.new_gen()
                if first_in_seq:
                    halo.new_gen()
                    POOL.wdep(halo)
                    ins = nc.gpsimd.memset(halo.t[:], 0.0)
                    halo.w = {"pool": POOL.sig(ins)}
                PE.rdep(h)
                for j in range(KC):
                    bks = []
                    for which in range(3):
                        wb = self.wload(self.conv_w_in[which * KC + j, :, :])
                        bk = self.next_bank()
                        bk.new_gen()
                        PE.wdep(bk)
                        PE.rdep(wb)
                        for k in range(KC):
                            ins = nc.tensor.matmul(bk.t[:, 0:G], lhsT=wb.t[:, k * 128:(k + 1) * 128], rhs=h.t[:, k, :],
                                                   start=(k == 0), stop=(k == KC - 1))
                        tok = self.pe_sig(ins)
                        wb.r["pe"] = tok
                        bk.w["pe"] = tok
                        bks.append(bk)
                    h.r["pe"] = tok
                    cs = csb.next()
                    cs.new_gen()
                    ACT.wdep(cs)
                    ACT.rdep(bks[1])
                    ins = nc.scalar.copy(out=cs.t[:], in_=bks[1].t[:, 0:G])
                    tok = ACT.sig(ins)
                    cs.w["act"] = tok
                    bks[1].r["act"] = tok
                    u = ub.next()
                    u.new_gen()
                    DVE.wdep(u)
                    DVE.rdep(cs)
                    DVE.rdep(bks[2])
                    DVE.rdep(halo)
                    ins = nc.vector.tensor_copy(out=u.t[:, 0:2], in_=halo.t[:, j, :])
                    DVE.sig(ins)
                    ins = nc.vector.tensor_tensor(out=u.t[:, 2:G + 2], in0=cs.t[:], in1=bks[2].t[:, 0:G], op=ALU.mult)
                    tok = DVE.sig(ins)
                    cs.r["dve"] = tok
                    bks[2].r["dve"] = tok
                    DVE.wait(tok)
                    ins = nc.vector.tensor_copy(out=halo.t[:, j, :], in_=u.t[:, G:G + 2])
                    halo.w["dve"] = DVE.sig(ins)
                    DVE.rdep(kcol)
                    c = cv.next()
                    c.new_gen()
                    DVE.wdep(c)
                    ins = nc.vector.tensor_scalar(out=c.t[:], in0=u.t[:, 2:G + 2], scalar1=kcol.t[:, 2, j:j + 1], scalar2=None,
                                                  op0=ALU.mult)
                    DVE.wait(DVE.sig(ins))
                    ins = nc.vector.scalar_tensor_tensor(out=c.t[:], in0=u.t[:, 1:G + 1], scalar=kcol.t[:, 1, j:j + 1], in1=c.t[:],
                                                         op0=ALU.mult, op1=ALU.add)
                    DVE.wait(DVE.sig(ins))
                    ins = nc.vector.scalar_tensor_tensor(out=c.t[:], in0=u.t[:, 0:G], scalar=kcol.t[:, 0, j:j + 1], in1=c.t[:],
                                                         op0=ALU.mult, op1=ALU.add)
                    tok = DVE.sig(ins)
                    u.r["dve"] = tok
                    DVE.wait(tok)
                    DVE.rdep(bks[0])
                    DVE.wdep(z)
                    ins = nc.vector.tensor_tensor(out=z.t[:, j, :], in0=c.t[:], in1=bks[0].t[:, 0:G], op=ALU.mult)
                    tok = DVE.sig(ins)
                    bks[0].r["dve"] = tok
                    c.r["dve"] = tok
                    z.w["dve"] = tok
                self.pass_B(z, KC, lambda c_, half: self.conv_w_out[c_ * 128:(c_ + 1) * 128, half * 1024:(half + 1) * 1024],
                            G // 128, self.evac_resid(tile0, 0, 0))
            self.commit_xres()
        self.x0_is_input = False

    def swiglu_group(self, h, hT, nch, w1src, w3src, sring):
        nc = self.nc
        PE, ACT, DVE = self.PE, self.ACT, self.DVE
        PE.rdep(h)
        hT.new_gen()
        for c in range(nch):
            bks = []
            for src in (w1src, w3src):
                wb = self.wload(src(c))
                bk = self.next_bank()
                bk.new_gen()
                PE.wdep(bk)
                PE.rdep(wb)
                for k in range(KC):
                    ins = self.nc.tensor.matmul(bk.t[:, 0:G], lhsT=wb.t[:, k * 128:(k + 1) * 128], rhs=h.t[:, k, :],
                                                start=(k == 0), stop=(k == KC - 1))
                tok = self.pe_sig(ins)
                wb.r["pe"] = tok
                bk.w["pe"] = tok
                bks.append(bk)
            h.r["pe"] = tok
            s = sring.next()
            s.new_gen()
            ACT.wdep(s)
            ACT.rdep(bks[0])
            ins = nc.scalar.activation(out=s.t[:], in_=bks[0].t[:, 0:G], func=AF.Silu)
            tok = ACT.sig(ins)
            s.w["act"] = tok
            bks[0].r["act"] = tok
            DVE.rdep(s)
            DVE.rdep(bks[1])
            DVE.wdep(hT)
            ins = nc.vector.tensor_tensor(out=hT.t[:, c, :], in0=s.t[:], in1=bks[1].t[:, 0:G], op=ALU.mult)
            tok = DVE.sig(ins)
            s.r["dve"] = tok
            bks[1].r["dve"] = tok
            hT.w["dve"] = tok

    def phase_ffn(self):
        nc = self.nc
        self.new_xres_tok = {}
        nch = self.DFF // 128
        with ExitStack() as st:
            self.make_prep_bufs(st)
            self.make_resid_bufs(st)
            hnT = Ring([self.sb(f"fhnT{i}", [128, KC, G], BF16, st) for i in range(2)])
            hT = self.sb("fhT", [128, nch, G], BF16, st)
            sring = Ring([self.sb(f"fs{i}", [128, G], BF16, st) for i in range(3)])
            for g in range(self.T // G):
                tile0 = g * (G // 128)
                b = (g * G) // self.S
                A, B = self.modcol(0, 1, b)
                h = hnT.next()
                h.new_gen()
                for t in range(G // 128):
                    self.prep(tile0 + t, A, B, h, t * 128)
                self.swiglu_group(h, hT, nch, lambda c: self.ffn_w1[c, :, :], lambda c: self.ffn_w3[c, :, :], sring)
                self.pass_B(hT, nch, lambda c_, half: self.ffn_w2[c_ * 128:(c_ + 1) * 128, half * 1024:(half + 1) * 1024],
                            G // 128, self.evac_resid(tile0, 0, 1))
            self.commit_xres()
        self.x0_is_input = False

    def phase_attn(self):
        nc = self.nc
        PE, ACT, DVE, POOL, SP = self.PE, self.ACT, self.DVE, self.POOL, self.SP
        S, T, NT = self.S, self.T, self.NT
        TPS = S // 128
        scale = float(HD) ** -0.5
        self.new_xres_tok = {}
        qkT_d = self.dram("qkT_d", [32, 128, T], BF16)
        v_d = self.dram("v_d", [T, D], BF16)
        o_d = self.dram("o_d", [T, D], BF16)
        cum_d = self.dram("cum_d", [NH, T], F32)
        with ExitStack() as st:
            self.make_prep_bufs(st)
            hnT = Ring([self.sb(f"ahnT{i}", [128, KC, G], BF16, st) for i in range(2)])
            qsb = Ring([self.sb(f"qsb{i}", [128, G], BF16, st) for i in range(3)])
            vsb = Ring([self.sb(f"vsb{i}", [128, 1024], BF16, st) for i in range(3)])
            wf = self.sb("wf", [128, KC, NH], BF16, st)
            bf_t = self.sb("bf_t", [128, NH], F32, st)
            one_c = self.sb("one_c", [128, 1], F32, st)
            tri = self.sb("tri", [128, 128], F32, st)
            ones = self.sb("ones", [128, 128], F32, st)
            lf = self.sb("lf", [128, NT, NH], F32, st)
            zt = Ring([self.sb(f"zt{i}", [128, NH], F32, st) for i in range(2)])
            cumt = Ring([self.sb(f"cumt{i}", [128, NH], F32, st) for i in range(2)])
            cumr = Ring([self.sb(f"cumr{i}", [NH, 128], F32, st) for i in range(2)])
            ins = nc.gpsimd.dma_start(out=wf.t[:], in_=self.fox_f.rearrange("(k p) n -> p k n", p=128))
            wf.w["dma"] = wf.dma_sig(ins)
            ins = nc.sync.dma_start(out=bf_t.t[:], in_=self.fox_b_f.partition_broadcast(128))
            bf_t.w["dma"] = bf_t.dma_sig(ins)
            nc.gpsimd.memset(one_c.t[:], 1.0)
            nc.gpsimd.memset(ones.t[:], 1.0)
            nc.gpsimd.memset(tri.t[:], 1.0)
            ins = nc.gpsimd.affine_select(out=tri.t[:], in_=tri.t[:], pattern=[[1, 128]], compare_op=ALU.is_ge, fill=0.0,
                                          base=0, channel_multiplier=-1)
            tok = POOL.sig(ins)
            tri.w["pool"] = tok
            ones.w["pool"] = tok
            one_c.w["pool"] = tok
            for g in range(T // G):
                tile0 = g * (G // 128)
                b = (g * G) // S
                A, B = self.modcol(1, 0, b)
                h = hnT.next()
                h.new_gen()
                for t in range(G // 128):
                    self.prep(tile0 + t, A, B, h, t * 128)
                PE.rdep(h)
                for ch in range(32):
                    wb = self.wload(self.fox_qk[ch, :, :])
                    bk = self.next_bank()
                    bk.new_gen()
                    PE.wdep(bk)
                    PE.rdep(wb)
                    for k in range(KC):
                        ins = nc.tensor.matmul(bk.t[:, 0:G], lhsT=wb.t[:, k * 128:(k + 1) * 128], rhs=h.t[:, k, :],
                                               start=(k == 0), stop=(k == KC - 1))
                    tok = self.pe_sig(ins)
                    wb.r["pe"] = tok
                    bk.w["pe"] = tok
                    q = qsb.next()
                    q.new_gen()
                    E = ACT if ch % 2 == 0 else DVE
                    E.wdep(q)
                    E.rdep(bk)
                    if E is ACT:
                        ins = nc.scalar.copy(out=q.t[:], in_=bk.t[:, 0:G])
                    else:
                        ins = nc.vector.tensor_copy(out=q.t[:], in_=bk.t[:, 0:G])
                    tok = E.sig(ins)
                    q.w[E.name] = tok
                    bk.r[E.name] = tok
                    SP.rdep(q)
                    ins = nc.sync.dma_start(out=qkT_d[ch, :, g * G:(g + 1) * G], in_=q.t[:])
                    tok = q.dma_sig(ins)
                    q.r["dma"] = tok
                    self.track_dma(tok)
                for t in range(G // 128):
                    tile = tile0 + t
                    bk = self.next_bank()
                    bk.new_gen()
                    PE.wdep(bk)
                    PE.rdep(wf)
                    for k in range(KC):
                        ins = nc.tensor.matmul(bk.t[:, 0:NH], lhsT=h.t[:, k, t * 128:(t + 1) * 128], rhs=wf.t[:, k, :],
                                               start=(k == 0), stop=(k == KC - 1))
                    tok = self.pe_sig(ins)
                    bk.w["pe"] = tok
                    z = zt.next()
                    z.new_gen()
                    DVE.wdep(z)
                    DVE.rdep(bk)
                    DVE.rdep(bf_t)
                    ins = nc.vector.tensor_tensor(out=z.t[:], in0=bk.t[:, 0:NH], in1=bf_t.t[:], op=ALU.add)
                    tok = DVE.sig(ins)
                    z.w["dve"] = tok
                    bk.r["dve"] = tok
                    ACT.rdep(z)
                    ACT.rdep(one_c)
                    ins = nc.scalar.activation(out=z.t[:], in_=z.t[:], func=AF.Exp, scale=-1.0)
                    ACT.wait(ACT.sig(ins))
                    ins = nc.scalar.activation(out=z.t[:], in_=z.t[:], func=AF.Ln, bias=one_c.t[:, 0:1])
                    ACT.wait(ACT.sig(ins))
                    ACT.waits(lf.prev)
                    ins = nc.scalar.mul(out=lf.t[:, tile, :], in_=z.t[:], mul=-1.0)
                    tok = ACT.sig(ins)
                    lf.w["act"] = tok
                    z.r["act"] = tok
                h.r["pe"] = self.last_pe_tok
                for t in range(G // 128):
                    tile = tile0 + t
                    seq0 = (tile // TPS) * TPS
                    bk = self.next_bank()
                    bk.new_gen()
                    PE.wdep(bk)
                    PE.rdep(lf)
                    PE.rdep(tri)
                    prevs = list(range(seq0, tile))
                    ins = nc.tensor.matmul(bk.t[:, 0:NH], lhsT=tri.t[:], rhs=lf.t[:, tile, :], start=True, stop=(len(prevs) == 0))
                    for i_, pt in enumerate(prevs):
                        ins = nc.tensor.matmul(bk.t[:, 0:NH], lhsT=ones.t[:], rhs=lf.t[:, pt, :], start=False,
                                               stop=(i_ == len(prevs) - 1))
                    tok = self.pe_sig(ins)
                    bk.w["pe"] = tok
                    lf.r["pe"] = tok
                    ct = cumt.next()
                    ct.new_gen()
                    DVE.wdep(ct)
                    DVE.rdep(bk)
                    ins = nc.vector.tensor_scalar(out=ct.t[:], in0=bk.t[:, 0:NH], scalar1=-1.0 / scale, scalar2=None, op0=ALU.mult)
                    tok = DVE.sig(ins)
                    ct.w["dve"] = tok
                    bk.r["dve"] = tok
                    bk2 = self.next_bank()
                    bk2.new_gen()
                    PE.wdep(bk2)
                    PE.rdep(ct)
                    PE.rdep(self.ident_f)
                    ins = nc.tensor.transpose(out=bk2.t[0:NH, 0:128], in_=ct.t[:], identity=self.ident_f.t[:])
                    tok = self.pe_sig(ins)
                    bk2.w["pe"] = tok
                    ct.r["pe"] = tok
                    cr = cumr.next()
                    cr.new_gen()
                    DVE.wdep(cr)
                    DVE.rdep(bk2)
                    ins = nc.vector.tensor_copy(out=cr.t[:], in_=bk2.t[0:NH, 0:128])
                    tok = DVE.sig(ins)
                    cr.w["dve"] = tok
                    bk2.r["dve"] = tok
                    SP.rdep(cr)
                    ins = nc.sync.dma_start(out=cum_d[:, tile * 128:(tile + 1) * 128], in_=cr.t[:])
                    tok = cr.dma_sig(ins)
                    cr.r["dma"] = tok
                    self.track_dma(tok)
                def evac_v(t, half, bks2, tile0=tile0):
                    vb = vsb.next()
                    vb.new_gen()
                    for n in range(2):
                        E = ACT if n == 0 else DVE
                        E.wdep(vb)
                        E.rdep(bks2[n])
                        if E is ACT:
                            ins = nc.scalar.copy(out=vb.t[:, n * 512:(n + 1) * 512], in_=bks2[n].t[:, :])
                        else:
                            ins = nc.vector.tensor_copy(out=vb.t[:, n * 512:(n + 1) * 512], in_=bks2[n].t[:, :])
                        tok = E.sig(ins)
                        vb.w[E.name] = tok
                        bks2[n].r[E.name] = tok
                    SP.rdep(vb)
                    ins = nc.sync.dma_start(out=v_d[(tile0 + t) * 128:(tile0 + t + 1) * 128, half * 1024:(half + 1) * 1024], in_=vb.t[:])
                    tok = vb.dma_sig(ins)
                    vb.r["dma"] = tok
                    self.track_dma(tok)
                self.pass_B(h, KC, lambda c_, half: self.fox_v[c_ * 128:(c_ + 1) * 128, half * 1024:(half + 1) * 1024],
                            G // 128, evac_v)
        self.barrier()
        with ExitStack() as st:
            qT = Ring([self.sb(f"qT{i}", [128, S], BF16, st) for i in range(2)])
            kT = Ring([self.sb(f"kT{i}", [128, S], BF16, st) for i in range(2)])
            vt = Ring([self.sb(f"vt{i}", [128, TPS, HD], BF16, st) for i in range(2)])
            bias = Ring([self.sb(f"abias{i}", [128, S], F32, st) for i in range(2)])
            Sb = Ring([self.sb(f"Sb{i}", [128, S], F32, st) for i in range(2)])
            Pb = Ring([self.sb(f"Pb{i}", [128, S], BF16, st) for i in range(2)])
            PT = Ring([self.sb(f"PT{i}", [128, TPS, 128], BF16, st) for i in range(2)])
            Oh = Ring([self.sb(f"Oh{i}", [128, TPS, HD], BF16, st) for i in range(2)])
            st_ring = Ring([self.sb(f"ast{i}", [128, 4], F32, st) for i in range(4)])
            neg_reg = nc.gpsimd.to_reg(-1.0e30)
            steps = [(seq, hh, i) for seq in range(self.NSEQ) for hh in range(NH) for i in range(TPS)]
            head_state = {}
            step_state = {}

            def stage_a(seq, hh, i):
                if i == 0:
                    q, kk, v, bs = qT.next(), kT.next(), vt.next(), bias.next()
                    for bf_, src in ((q, qkT_d[hh, :, seq * S:(seq + 1) * S]), (kk, qkT_d[16 + hh, :, seq * S:(seq + 1) * S]),
                                     (v, v_d[seq * S:(seq + 1) * S, hh * HD:(hh + 1) * HD].rearrange("(j p) d -> p j d", p=128)),
                                     (bs, cum_d[hh, seq * S:(seq + 1) * S].partition_broadcast(128))):
                        bf_.new_gen()
                        SP.wdep(bf_)
                        ins = nc.sync.dma_start(out=bf_.t[:], in_=src)
                        bf_.w["dma"] = bf_.dma_sig(ins)
                        self.track_dma(bf_.w["dma"])
                    oh = Oh.next()
                    oh.new_gen()
                    head_state[(seq, hh)] = (q, kk, v, bs, oh)
                q, kk, v, bs, oh = head_state[(seq, hh)]
                nk = (i + 1) * 128
                nkb = (nk + 511) // 512
                sb_ = Sb.next()
                sb_.new_gen()
                PE.rdep(q)
                PE.rdep(kk)
                DVE.rdep(bs)
                for kb in range(nkb):
                    w = min(512, nk - kb * 512)
                    bk = self.next_bank()
                    bk.new_gen()
                    PE.wdep(bk)
                    ins = nc.tensor.matmul(bk.t[:, 0:w], lhsT=q.t[:, i * 128:(i + 1) * 128], rhs=kk.t[:, kb * 512:kb * 512 + w],
                                           start=True, stop=True)
                    tok = self.pe_sig(ins)
                    bk.w["pe"] = tok
                    DVE.rdep(bk)
                    DVE.wdep(sb_)
                    ins = nc.vector.tensor_tensor(out=sb_.t[:, kb * 512:kb * 512 + w], in0=bk.t[:, 0:w],
                                                  in1=bs.t[:, kb * 512:kb * 512 + w], op=ALU.add)
                    tok = DVE.sig(ins)
                    bk.r["dve"] = tok
                    sb_.w["dve"] = tok
                q.r["pe"] = self.last_pe_tok
                kk.r["pe"] = self.last_pe_tok
                bs.r["dve"] = tok
                POOL.rdep(sb_)
                ins = nc.gpsimd.affine_select(out=sb_.t[:, i * 128:(i + 1) * 128], in_=sb_.t[:, i * 128:(i + 1) * 128],
                                              pattern=[[-1, 128]], compare_op=ALU.is_ge, fill=neg_reg, base=0, channel_multiplier=1)
                sb_.w["pool"] = POOL.sig(ins)
                stt = st_ring.next()
                stt.new_gen()
                DVE.wdep(stt)
                DVE.rdep(sb_)
                ins = nc.vector.reduce_max(out=stt.t[:, 0:1], in_=sb_.t[:, 0:nk], axis=AX.X)
                DVE.wait(DVE.sig(ins))
                ins = nc.vector.tensor_scalar(out=stt.t[:, 1:2], in0=stt.t[:, 0:1], scalar1=-scale, scalar2=None, op0=ALU.mult)
                stt.w["dve"] = DVE.sig(ins)
                pb = Pb.next()
                pb.new_gen()
                ACT.wdep(pb)
                ACT.rdep(stt)
                ACT.rdep(sb_)
                ins = nc.scalar.memzero(stt.t[:, 2:3])
                ACT.wait(ACT.sig(ins))
                ins = nc.scalar.activation(out=pb.t[:, 0:nk], in_=sb_.t[:, 0:nk], func=AF.Exp, scale=scale, bias=stt.t[:, 1:2],
                                           accum_out=stt.t[:, 2:3])
                tok = ACT.sig(ins)
                pb.w["act"] = tok
                sb_.r["act"] = tok
                stt.w["act"] = tok
                step_state[(seq, hh, i)] = (pb, stt)

            def stage_b(seq, hh, i):
                q, kk, v, bs, oh = head_state[(seq, hh)]
                pb, stt = step_state.pop((seq, hh, i))
                pt = PT.next()
                pt.new_gen()
                PE.rdep(pb)
                PE.rdep(self.ident_bf)
                nkt = i + 1
                for j0 in range(0, nkt, 8):
                    bk = self.next_bank()
                    bk.new_gen()
                    PE.wdep(bk)
                    pv = bk.t[:].bitcast(BF16)
                    cnt = min(8, nkt - j0)
                    for jj in range(cnt):
                        kt = j0 + jj
                        ins = nc.tensor.transpose(out=pv[:, jj * 128:(jj + 1) * 128], in_=pb.t[:, kt * 128:(kt + 1) * 128],
                                                  identity=self.ident_bf.t[:])
                    tok = self.pe_sig(ins)
                    bk.w["pe"] = tok
                    E = ACT if (j0 // 8) % 2 == 0 else DVE
                    E.rdep(bk)
                    E.wdep(pt)
                    if E is ACT:
                        ins = nc.scalar.copy(out=pt.t[:, j0:j0 + cnt, :], in_=pv[:, 0:cnt * 128].rearrange("p (j q) -> p j q", q=128))
                    else:
                        ins = nc.vector.tensor_copy(out=pt.t[:, j0:j0 + cnt, :], in_=pv[:, 0:cnt * 128].rearrange("p (j q) -> p j q", q=128))
                    tok = E.sig(ins)
                    bk.r[E.name] = tok
                    pt.w[E.name] = tok
                pb.r["pe"] = self.last_pe_tok
                bk = self.next_bank()
                bk.new_gen()
                PE.wdep(bk)
                PE.rdep(pt)
                PE.rdep(v)
                for kt in range(nkt):
                    ins = nc.tensor.matmul(bk.t[:, 0:HD], lhsT=pt.t[:, kt, :], rhs=v.t[:, kt, :], start=(kt == 0), stop=(kt == nkt - 1))
                tok = self.pe_sig(ins)
                bk.w["pe"] = tok
                pt.r["pe"] = tok
                v.r["pe"] = tok
                DVE.rdep(stt)
                ins = nc.vector.reciprocal(out=stt.t[:, 3:4], in_=stt.t[:, 2:3])
                stt.w["dve"] = DVE.sig(ins)
                ACT.rdep(stt)
                ACT.rdep(bk)
                ACT.wdep(oh)
                ins = nc.scalar.activation(out=oh.t[:, i, :], in_=bk.t[:, 0:HD], func=AF.Copy, scale=stt.t[:, 3:4])
                tok = ACT.sig(ins)
                bk.r["act"] = tok
                oh.w["act"] = tok
                stt.r["act"] = tok
                if i == TPS - 1:
                    SP.rdep(oh)
                    ins = nc.sync.dma_start(out=o_d[seq * S:(seq + 1) * S, hh * HD:(hh + 1) * HD].rearrange("(j p) d -> p j d", p=128),
                                            in_=oh.t[:])
                    tok = oh.dma_sig(ins)
                    oh.r["dma"] = tok
                    self.track_dma(tok)

            stage_a(*steps[0])
            for n in range(len(steps)):
                if n + 1 < len(steps):
                    stage_a(*steps[n + 1])
                stage_b(*steps[n])
        self.barrier()
        with ExitStack() as st:
            self.make_resid_bufs(st)
            ot_ring = Ring([self.sb(f"otok{i}", [128, D], BF16, st) for i in range(2)])
            oT = Ring([self.sb(f"oT{i}", [128, KC, G], BF16, st) for i in range(2)])
            for g in range(T // G):
                tile0 = g * (G // 128)
                o_T = oT.next()
                o_T.new_gen()
                for t in range(G // 128):
                    ot = ot_ring.next()
                    ot.new_gen()
                    SP.wdep(ot)
                    ins = nc.sync.dma_start(out=ot.t[:], in_=o_d[(tile0 + t) * 128:(tile0 + t + 1) * 128, :])
                    ot.w["dma"] = ot.dma_sig(ins)
                    self.track_dma(ot.w["dma"])
                    PE.rdep(ot)
                    for j in range(2):
                        bk = self.next_bank()
                        bk.new_gen()
                        PE.wdep(bk)
                        pv = bk.t[:].bitcast(BF16)
                        for i in range(8):
                            k = 8 * j + i
                            ins = nc.tensor.transpose(out=pv[:, i * 128:(i + 1) * 128], in_=ot.t[:, k * 128:(k + 1) * 128],
                                                      identity=self.ident_bf.t[:])
                        tok = self.pe_sig(ins)
                        bk.w["pe"] = tok
                        ot.r["pe"] = tok
                        E = ACT if j == 0 else DVE
                        E.rdep(bk)
                        E.wdep(o_T)
                        for i in range(8):
                            k = 8 * j + i
                            if E is ACT:
                                ins = nc.scalar.copy(out=o_T.t[:, k, t * 128:(t + 1) * 128], in_=pv[:, i * 128:(i + 1) * 128])
                            else:
                                ins = nc.vector.tensor_copy(out=o_T.t[:, k, t * 128:(t + 1) * 128], in_=pv[:, i * 128:(i + 1) * 128])
                        tok = E.sig(ins)
                        bk.r[E.name] = tok
                        o_T.w[E.name] = tok
                self.pass_B(o_T, KC, lambda c_, half: self.fox_w_out[c_ * 128:(c_ + 1) * 128, half * 1024:(half + 1) * 1024],
                            G // 128, self.evac_resid(tile0, 1, 0))
            self.commit_xres()
        self.x0_is_input = False

    def phase_moe(self):
        nc = self.nc
        PE, ACT, DVE, POOL, SP = self.PE, self.ACT, self.DVE, self.POOL, self.SP
        NE, T, NT = self.NE, self.T, self.NT
        nch = self.DFFE // 128
        self.new_xres_tok = {}
        with ExitStack() as st:
            self.make_prep_bufs(st)
            self.make_resid_bufs(st)
            hnT = Ring([self.sb(f"mhnT{i}", [128, KC, G], BF16, st) for i in range(1)])
            hT = self.sb("mhT", [128, nch, G], BF16, st)
            sring = Ring([self.sb(f"ms{i}", [128, G], BF16, st) for i in range(3)])
            wr = self.sb("wr", [128, KC, NE], BF16, st)
            gates = self.sb("gates", [128, NT, NE], F32, st)
            lg = Ring([self.sb(f"lg{i}", [128, 4, NE], F32, st) for i in range(2)])
            sm = Ring([self.sb(f"sm{i}", [128, 8], F32, st) for i in range(2)])
            ins = nc.gpsimd.dma_start(out=wr.t[:], in_=self.moe_router.rearrange("(k p) n -> p k n", p=128))
            wr.w["dma"] = wr.dma_sig(ins)
            for g in range(T // G):
                tile0 = g * (G // 128)
                b = (g * G) // self.S
                A, B = self.modcol(1, 1, b)
                h = hnT.next()
                h.new_gen()
                for t in range(G // 128):
                    self.prep(tile0 + t, A, B, h, t * 128)
                PE.rdep(h)
                PE.rdep(wr)
                for t in range(G // 128):
                    tile = tile0 + t
                    bk = self.next_bank()
                    bk.new_gen()
                    PE.wdep(bk)
                    for k in range(KC):
                        ins = nc.tensor.matmul(bk.t[:, 0:NE], lhsT=h.t[:, k, t * 128:(t + 1) * 128], rhs=wr.t[:, k, :],
                                               start=(k == 0), stop=(k == KC - 1))
                    tok = self.pe_sig(ins)
                    bk.w["pe"] = tok
                    L = lg.next()
                    L.new_gen()
                    s_ = sm.next()
                    s_.new_gen()
                    DVE.wdep(L)
                    DVE.wdep(s_)
                    DVE.rdep(bk)
                    ins = nc.vector.tensor_copy(out=L.t[:, 0, :], in_=bk.t[:, 0:NE])
                    tok = DVE.sig(ins)
                    bk.r["dve"] = tok
                    DVE.wait(tok)
                    ins = nc.vector.reduce_max(out=s_.t[:, 0:1], in_=L.t[:, 0, :], axis=AX.X)
                    DVE.wait(DVE.sig(ins))
                    ins = nc.vector.tensor_scalar(out=L.t[:, 1, :], in0=L.t[:, 0, :], scalar1=s_.t[:, 0:1], scalar2=None, op0=ALU.is_equal)
                    DVE.wait(DVE.sig(ins))
                    ins = nc.vector.scalar_tensor_tensor(out=L.t[:, 2, :], in0=L.t[:, 1, :], scalar=-1.0e30, in1=L.t[:, 0, :],
                                                         op0=ALU.mult, op1=ALU.add)
                    DVE.wait(DVE.sig(ins))
                    ins = nc.vector.reduce_max(out=s_.t[:, 1:2], in_=L.t[:, 2, :], axis=AX.X)
                    DVE.wait(DVE.sig(ins))
                    ins = nc.vector.tensor_scalar(out=L.t[:, 3, :], in0=L.t[:, 2, :], scalar1=s_.t[:, 1:2], scalar2=None, op0=ALU.is_equal)
                    DVE.sig(ins)
                    ins = nc.vector.tensor_tensor(out=s_.t[:, 2:3], in0=s_.t[:, 1:2], in1=s_.t[:, 0:1], op=ALU.subtract)
                    tok = DVE.sig(ins)
                    s_.w["dve"] = tok
                    ACT.rdep(s_)
                    ins = nc.scalar.activation(out=s_.t[:, 3:4], in_=s_.t[:, 2:3], func=AF.Sigmoid, scale=-1.0)
                    ACT.sig(ins)
                    ins = nc.scalar.activation(out=s_.t[:, 4:5], in_=s_.t[:, 2:3], func=AF.Sigmoid, scale=1.0)
                    tok = ACT.sig(ins)
                    s_.w["act"] = tok
                    DVE.rdep(s_)
                    DVE.waits(gates.prev)
                    ins = nc.vector.tensor_scalar(out=gates.t[:, tile, :], in0=L.t[:, 1, :], scalar1=s_.t[:, 3:4], scalar2=None, op0=ALU.mult)
                    DVE.wait(DVE.sig(ins))
                    ins = nc.vector.scalar_tensor_tensor(out=gates.t[:, tile, :], in0=L.t[:, 3, :], scalar=s_.t[:, 4:5], in1=gates.t[:, tile, :],
                                                         op0=ALU.mult, op1=ALU.add)
                    tok = DVE.sig(ins)
                    gates.w["dve"] = tok
                    L.r["dve"] = tok
                    s_.r["dve"] = tok
                for e in range(NE):
                    self.swiglu_group(h, hT, nch, lambda c, e=e: self.moe_w1[e * nch + c, :, :], lambda c, e=e: self.moe_w3[e * nch + c, :, :], sring)
                    self.DVE.rdep(gates)
                    self.pass_B(hT, nch,
                                lambda c_, half, e=e: self.moe_w2[e * self.DFFE + c_ * 128: e * self.DFFE + (c_ + 1) * 128, half * 1024:(half + 1) * 1024],
                                G // 128, self.evac_resid(tile0, 1, 1, rowscale=lambda t, e=e, tile0=tile0: gates.t[:, tile0 + t, e:e + 1]))
                    self.commit_xres()
                    self.x0_is_input = False
            self.commit_xres()

    def phase_moe_sparse(self):
        nc = self.nc
        PE, ACT, DVE, POOL, SP = self.PE, self.ACT, self.DVE, self.POOL, self.SP
        NE, T, NT, S = self.NE, self.T, self.NT, self.S
        I32 = mybir.dt.int32
        nch = self.DFFE // 128
        NG = (2 * T) // G + NE - 1
        NSLOT = NG * G
        Xg = self.dram("Xg", [NSLOT, D], BF16)
        Yg = self.dram("Yg", [NSLOT, D], F32)
        hn_d = self.dram("hn_d", [T, D], BF16)
        w1tab = self.moe_w1.rearrange("c p f -> (c p) f")
        w3tab = self.moe_w3.rearrange("c p f -> (c p) f")
        w2tab = self.moe_w2.rearrange("r (h c) -> (r h) c", h=2)
        self.new_xres_tok = {}
        with ExitStack() as ph:
            a_bf = self.sb("a_bf", [128, NT, NE], BF16, ph)
            eq1f = self.sb("eq1f", [128, NT, NE], F32, ph)
            eq2f = self.sb("eq2f", [128, NT, NE], F32, ph)
            wts = self.sb("wts", [128, NT, 2], F32, ph)
            cnt = self.sb("cnt", [128, NT, NE], F32, ph)
            slots_f = self.sb("slots_f", [128, NT, 2], F32, ph)
            slots_i = self.sb("slots_i", [128, NT, 2], I32, ph)
            triS = self.sb("triS", [128, 128], BF16, ph)
            ones_bf = self.sb("ones_bf", [128, 128], BF16, ph)
            ntot = self.sb("ntot", [128, NE], F32, ph)
            padded = self.sb("padded", [128, NE], F32, ph)
            base = self.sb("base", [128, NE], F32, ph)
            endt = self.sb("endt", [128, NE], F32, ph)
            etab = self.sb("etab", [128, NG], F32, ph)
            iota_i = self.sb("iota_i", [128, nch], I32, ph)
            iota_f = self.sb("iota_f", [128, nch], F32, ph)
            wr = self.sb("wr", [128, KC, NE], BF16, ph)
            ins = nc.gpsimd.dma_start(out=wr.t[:], in_=self.moe_router.rearrange("(k p) n -> p k n", p=128))
            wr.w["dma"] = wr.dma_sig(ins)
            nc.gpsimd.memset(ones_bf.t[:], 1.0)
            nc.gpsimd.memset(triS.t[:], 1.0)
            nc.gpsimd.affine_select(out=triS.t[:], in_=triS.t[:], pattern=[[1, 128]], compare_op=ALU.is_ge, fill=0.0,
                                    base=-1, channel_multiplier=-1)
            ins = nc.gpsimd.iota(iota_i.t[:], pattern=[[128, nch]], base=0, channel_multiplier=1)
            tok = POOL.sig(ins)
            triS.w["pool"] = tok
            ones_bf.w["pool"] = tok
            iota_i.w["pool"] = tok
            DVE.rdep(iota_i)
            ins = nc.vector.tensor_copy(out=iota_f.t[:], in_=iota_i.t[:])
            iota_f.w["dve"] = DVE.sig(ins)
            with ExitStack() as st:
                xt_ring = Ring([self.sb(f"rxt{i}", [128, D], F32, st) for i in range(2)])
                hn_ring = Ring([self.sb(f"rhn{i}", [128, D], BF16, st) for i in range(2)])
                junk = self.sb("rjunk", [128, D], BF16, st)
                ss_ring = Ring([self.sb(f"rss{i}", [128, 2], F32, st) for i in range(4)])
                Arow = self.sb("Arow", [128, D], F32, st)
                Brow = self.sb("Brow", [128, D], F32, st)
                Trow = self.sb("Trow", [128, D], F32, st)
                hTr = Ring([self.sb(f"hTr{i}", [128, KC, 128], BF16, st) for i in range(2)])
                lg = Ring([self.sb(f"slg{i}", [128, 4, NE], F32, st) for i in range(2)])
                sm = Ring([self.sb(f"ssm{i}", [128, 8], F32, st) for i in range(2)])
                hn_tok = {}
                cur_b = None
                for tile in range(NT):
                    b = (tile * 128) // S
                    if b != cur_b:
                        cur_b = b
                        for bf_ in (Arow, Brow, Trow):
                            bf_.new_gen()
                            SP.wdep(bf_)
                        SP.wait(self.mod_ready)
                        ins = nc.sync.dma_start(out=Trow.t[:], in_=self.norm_ffn[1, :].partition_broadcast(128))
                        Trow.w["dma"] = Trow.dma_sig(ins)
                        ins = nc.sync.dma_start(out=Arow.t[:], in_=self.mod_d[1, b, 4 * D:5 * D].partition_broadcast(128))
                        Arow.w["dma"] = Arow.dma_sig(ins)
                        ins = nc.sync.dma_start(out=Brow.t[:], in_=self.mod_d[1, b, 3 * D:4 * D].partition_broadcast(128))
                        Brow.w["dma"] = Brow.dma_sig(ins)
                        DVE.rdep(Arow)
                        DVE.rdep(Trow)
                        DVE.rdep(Brow)
                        ins = nc.vector.scalar_tensor_tensor(out=Arow.t[:], in0=Arow.t[:], scalar=1.0, in1=Trow.t[:], op0=ALU.add, op1=ALU.mult)
                        tok = DVE.sig(ins)
                        Arow.w["dve"] = tok
                        Trow.r["dve"] = tok
                        DVE.wait(tok)
                    xt = xt_ring.next()
                    xt.new_gen()
                    SP.wdep(xt)
                    SP.waits([self.xres_tok.get((tile, 0)), self.xres_tok.get((tile, 1))])
                    src = self.x_in if self.x0_is_input else self.xres
                    ins = nc.sync.dma_start(out=xt.t[:], in_=src[tile * 128:(tile + 1) * 128, :])
                    xt.w["dma"] = xt.dma_sig(ins)
                    ss = ss_ring.next()
                    ss.new_gen()
                    junk.new_gen()
                    ACT.wdep(ss)
                    ACT.wdep(junk)
                    ACT.rdep(xt)
                    ins = nc.scalar.memzero(ss.t[:])
                    ACT.wait(ACT.sig(ins))
                    ins = nc.scalar.activation(out=junk.t[:], in_=xt.t[:], func=AF.Square, accum_out=ss.t[:, 0:1])
                    tok = ACT.sig(ins)
                    junk.w["act"] = tok
                    ACT.wait(tok)
                    ins = nc.scalar.activation(out=ss.t[:, 1:2], in_=ss.t[:, 0:1], func=AF.Sqrt, scale=1.0 / D, bias=self.eps_col.t[:, 0:1])
                    ss.w["act"] = ACT.sig(ins)
                    DVE.rdep(ss)
                    ins = nc.vector.reciprocal(out=ss.t[:, 1:2], in_=ss.t[:, 1:2])
                    DVE.wait(DVE.sig(ins))
                    DVE.rdep(xt)
                    ins = nc.vector.scalar_tensor_tensor(out=xt.t[:], in0=xt.t[:], scalar=ss.t[:, 1:2], in1=Arow.t[:], op0=ALU.mult, op1=ALU.mult)
                    tok = DVE.sig(ins)
                    ss.r["dve"] = tok
                    DVE.wait(tok)
                    hn = hn_ring.next()
                    hn.new_gen()
                    DVE.wdep(hn)
                    ins = nc.vector.tensor_tensor(out=hn.t[:], in0=xt.t[:], in1=Brow.t[:], op=ALU.add)
                    tok = DVE.sig(ins)
                    hn.w["dve"] = tok
                    xt.r["dve"] = tok
                    Arow.r["dve"] = tok
                    Brow.r["dve"] = tok
                    SP.rdep(hn)
                    ins = nc.sync.dma_start(out=hn_d[tile * 128:(tile + 1) * 128, :], in_=hn.t[:])
                    tok = hn.dma_sig(ins)
                    hn.r["dma"] = tok
                    hn_tok[tile] = tok
                    self.track_dma(tok)
                    hr = hTr.next()
                    hr.new_gen()
                    PE.rdep(hn)
                    PE.rdep(self.ident_bf)
                    for j in range(2):
                        bk = self.next_bank()
                        bk.new_gen()
                        PE.wdep(bk)
                        pv = bk.t[:].bitcast(BF16)
                        for i in range(8):
                            k = 8 * j + i
                            ins = nc.tensor.transpose(out=pv[:, i * 128:(i + 1) * 128], in_=hn.t[:, k * 128:(k + 1) * 128],
                                                      identity=self.ident_bf.t[:])
                        tok = self.pe_sig(ins)
                        bk.w["pe"] = tok
                        hn.r["pe"] = tok
                        E = ACT if j == 0 else DVE
                        E.rdep(bk)
                        E.wdep(hr)
                        if E is ACT:
                            ins = nc.scalar.copy(out=hr.t[:, 8 * j:8 * j + 8, :], in_=pv.rearrange("p (j q) -> p j q", q=128))
                        else:
                            ins = nc.vector.tensor_copy(out=hr.t[:, 8 * j:8 * j + 8, :], in_=pv.rearrange("p (j q) -> p j q", q=128))
                        tok = E.sig(ins)
                        bk.r[E.name] = tok
                        hr.w[E.name] = tok
                    bk = self.next_bank()
                    bk.new_gen()
                    PE.wdep(bk)
                    PE.rdep(hr)
                    PE.rdep(wr)
                    for k in range(KC):
                        ins = nc.tensor.matmul(bk.t[:, 0:NE], lhsT=hr.t[:, k, :], rhs=wr.t[:, k, :], start=(k == 0), stop=(k == KC - 1))
                    tok = self.pe_sig(ins)
                    bk.w["pe"] = tok
                    hr.r["pe"] = tok
                    L = lg.next()
                    L.new_gen()
                    s_ = sm.next()
                    s_.new_gen()
                    DVE.wdep(L)
                    DVE.wdep(s_)
                    DVE.rdep(bk)
                    ins = nc.vector.tensor_copy(out=L.t[:, 0, :], in_=bk.t[:, 0:NE])
                    tok = DVE.sig(ins)
                    bk.r["dve"] = tok
                    DVE.wait(tok)
                    ins = nc.vector.reduce_max(out=s_.t[:, 0:1], in_=L.t[:, 0, :], axis=AX.X)
                    DVE.wait(DVE.sig(ins))
                    ins = nc.vector.tensor_scalar(out=eq1f.t[:, tile, :], in0=L.t[:, 0, :], scalar1=s_.t[:, 0:1], scalar2=None, op0=ALU.is_equal)
                    DVE.wait(DVE.sig(ins))
                    ins = nc.vector.scalar_tensor_tensor(out=L.t[:, 2, :], in0=eq1f.t[:, tile, :], scalar=-1.0e30, in1=L.t[:, 0, :],
                                                         op0=ALU.mult, op1=ALU.add)
                    DVE.wait(DVE.sig(ins))
                    ins = nc.vector.reduce_max(out=s_.t[:, 1:2], in_=L.t[:, 2, :], axis=AX.X)
                    DVE.wait(DVE.sig(ins))
                    ins = nc.vector.tensor_scalar(out=eq2f.t[:, tile, :], in0=L.t[:, 2, :], scalar1=s_.t[:, 1:2], scalar2=None, op0=ALU.is_equal)
                    DVE.wait(DVE.sig(ins))
                    ins = nc.vector.tensor_tensor(out=a_bf.t[:, tile, :], in0=eq1f.t[:, tile, :], in1=eq2f.t[:, tile, :], op=ALU.add)
                    a_bf.w["dve"] = DVE.sig(ins)
                    ins = nc.vector.tensor_tensor(out=s_.t[:, 2:3], in0=s_.t[:, 1:2], in1=s_.t[:, 0:1], op=ALU.subtract)
                    tok = DVE.sig(ins)
                    s_.w["dve"] = tok
                    ACT.rdep(s_)
                    ins = nc.scalar.activation(out=wts.t[:, tile, 0:1], in_=s_.t[:, 2:3], func=AF.Sigmoid, scale=-1.0)
                    ACT.sig(ins)
                    ins = nc.scalar.activation(out=wts.t[:, tile, 1:2], in_=s_.t[:, 2:3], func=AF.Sigmoid, scale=1.0)
                    tok = ACT.sig(ins)
                    wts.w["act"] = tok
                    s_.r["act"] = tok
                    L.r["dve"] = a_bf.w["dve"]
                eq1f.w["dve"] = a_bf.w["dve"]
                eq2f.w["dve"] = a_bf.w["dve"]
                PE.rdep(a_bf)
                PE.rdep(triS)
                PE.rdep(ones_bf)
                for tile in range(NT):
                    bk = self.next_bank()
                    bk.new_gen()
                    PE.wdep(bk)
                    ins = nc.tensor.matmul(bk.t[:, 0:NE], lhsT=triS.t[:], rhs=a_bf.t[:, tile, :], start=True, stop=(tile == 0))
                    for pt in range(tile):
                        ins = nc.tensor.matmul(bk.t[:, 0:NE], lhsT=ones_bf.t[:], rhs=a_bf.t[:, pt, :], start=False, stop=(pt == tile - 1))
                    tok = self.pe_sig(ins)
                    bk.w["pe"] = tok
                    DVE.rdep(bk)
                    ins = nc.vector.tensor_copy(out=cnt.t[:, tile, :], in_=bk.t[:, 0:NE])
                    tok = DVE.sig(ins)
                    bk.r["dve"] = tok
                    cnt.w["dve"] = tok
                bk = self.next_bank()
                bk.new_gen()
                PE.wdep(bk)
                for tile in range(NT):
                    ins = nc.tensor.matmul(bk.t[:, 0:NE], lhsT=ones_bf.t[:], rhs=a_bf.t[:, tile, :], start=(tile == 0), stop=(tile == NT - 1))
                tok = self.pe_sig(ins)
                bk.w["pe"] = tok
                a_bf.r["pe"] = tok
                DVE.rdep(bk)
                ins = nc.vector.tensor_copy(out=ntot.t[:], in_=bk.t[:, 0:NE])
                tok = DVE.sig(ins)
                bk.r["dve"] = tok
                DVE.wait(tok)
                ins = nc.vector.memset(padded.t[:], 0.0)
                DVE.wait(DVE.sig(ins))
                for m in range(T // G):
                    ins = nc.vector.scalar_tensor_tensor(out=padded.t[:], in0=ntot.t[:], scalar=float(G * m), in1=padded.t[:],
                                                         op0=ALU.is_gt, op1=ALU.add)
                    DVE.wait(DVE.sig(ins))
                ins = nc.vector.tensor_scalar(out=padded.t[:], in0=padded.t[:], scalar1=float(G), scalar2=None, op0=ALU.mult)
                DVE.wait(DVE.sig(ins))
                ins = nc.vector.memset(base.t[:], 0.0)
                DVE.wait(DVE.sig(ins))
                for e in range(1, NE):
                    ins = nc.vector.tensor_tensor(out=base.t[:, e:e + 1], in0=base.t[:, e - 1:e], in1=padded.t[:, e - 1:e], op=ALU.add)
                    DVE.wait(DVE.sig(ins))
                ins = nc.vector.tensor_tensor(out=endt.t[:], in0=base.t[:], in1=padded.t[:], op=ALU.add)
                DVE.wait(DVE.sig(ins))
                tmp8 = lg.next()
                for gi in range(NG):
                    ins = nc.vector.tensor_scalar(out=tmp8.t[:, 0, :], in0=endt.t[:], scalar1=float(gi * G), scalar2=None, op0=ALU.is_le)
                    DVE.wait(DVE.sig(ins))
                    ins = nc.vector.reduce_sum(out=etab.t[:, gi:gi + 1], in_=tmp8.t[:, 0, :], axis=AX.X)
                    DVE.wait(DVE.sig(ins))
                ins = nc.vector.tensor_scalar(out=etab.t[:], in0=etab.t[:], scalar1=float(NE - 1), scalar2=None, op0=ALU.min)
                etab.w["dve"] = DVE.sig(ins)
                DVE.wait(etab.w["dve"])
                for tile in range(NT):
                    ins = nc.vector.tensor_tensor(out=tmp8.t[:, 1, :], in0=cnt.t[:, tile, :], in1=base.t[:], op=ALU.add)
                    DVE.wait(DVE.sig(ins))
                    ins = nc.vector.tensor_tensor(out=tmp8.t[:, 2, :], in0=tmp8.t[:, 1, :], in1=eq1f.t[:, tile, :], op=ALU.mult)
                    DVE.sig(ins)
                    ins = nc.vector.tensor_tensor(out=tmp8.t[:, 3, :], in0=tmp8.t[:, 1, :], in1=eq2f.t[:, tile, :], op=ALU.mult)
                    DVE.wait(DVE.sig(ins))
                    ins = nc.vector.reduce_sum(out=slots_f.t[:, tile, 0:1], in_=tmp8.t[:, 2, :], axis=AX.X)
                    DVE.sig(ins)
                    ins = nc.vector.reduce_sum(out=slots_f.t[:, tile, 1:2], in_=tmp8.t[:, 3, :], axis=AX.X)
                    DVE.wait(DVE.sig(ins))
                ins = nc.vector.tensor_copy(out=slots_i.t[:], in_=slots_f.t[:])
                slots_i.w["dve"] = DVE.sig(ins)
                POOL.rdep(slots_i)
                for tile in range(NT):
                    hn = hn_ring.next()
                    hn.new_gen()
                    SP.wdep(hn)
                    SP.wait(hn_tok[tile])
                    ins = nc.sync.dma_start(out=hn.t[:], in_=hn_d[tile * 128:(tile + 1) * 128, :])
                    hn.w["dma"] = hn.dma_sig(ins)
                    POOL.rdep(hn)
                    for j in range(2):
                        ins = nc.gpsimd.indirect_dma_start(out=Xg[:, :], out_offset=bass.IndirectOffsetOnAxis(ap=slots_i.t[:, tile, j:j + 1], axis=0),
                                                           in_=hn.t[:], in_offset=None)
                        tok = hn.dma_sig(ins)
                        hn.r["sc"] = tok
                        self.track_dma(tok)
            self.barrier()
            with ExitStack() as st:
                xg_ring = Ring([self.sb(f"xg{i}", [128, D], BF16, st) for i in range(2)])
                xT = Ring([self.sb(f"gxT{i}", [128, KC, G], BF16, st) for i in range(2)])
                hT = self.sb("ghT", [128, nch, G], BF16, st)
                sring = Ring([self.sb(f"gs{i}", [128, G], BF16, st) for i in range(3)])
                ysb = Ring([self.sb(f"ysb{i}", [128, 1024], F32, st) for i in range(3)])
                idxs = Ring([self.sb(f"idx{i}", [128, 3, nch], I32, st) for i in range(2)])
                idxf = self.sb("idxf", [128, nch], F32, st)
                ecol = self.sb("ecol", [128, 1], F32, st)
                for gi in range(NG):
                    ix = idxs.next()
                    ix.new_gen()
                    DVE.wdep(ix)
                    DVE.rdep(etab)
                    DVE.rdep(iota_f)
                    ins = nc.vector.tensor_scalar(out=ecol.t[:], in0=etab.t[:, gi:gi + 1], scalar1=float(self.DFFE), scalar2=None, op0=ALU.mult)
                    DVE.wait(DVE.sig(ins))
                    ins = nc.vector.tensor_scalar(out=idxf.t[:], in0=iota_f.t[:], scalar1=ecol.t[:, 0:1], scalar2=None, op0=ALU.add)
                    DVE.wait(DVE.sig(ins))
                    ins = nc.vector.tensor_copy(out=ix.t[:, 0, :], in_=idxf.t[:])
                    DVE.sig(ins)
                    ins = nc.vector.tensor_scalar(out=ix.t[:, 1, :], in0=idxf.t[:], scalar1=2.0, scalar2=None, op0=ALU.mult)
                    DVE.sig(ins)
                    ins = nc.vector.tensor_scalar(out=ix.t[:, 2, :], in0=idxf.t[:], scalar1=2.0, scalar2=1.0, op0=ALU.mult, op1=ALU.add)
                    tok = DVE.sig(ins)
                    ix.w["dve"] = tok
                    DVE.wait(tok)
                    x_T = xT.next()
                    x_T.new_gen()
                    for t in range(G // 128):
                        xg = xg_ring.next()
                        xg.new_gen()
                        SP.wdep(xg)
                        ins = nc.sync.dma_start(out=xg.t[:], in_=Xg[gi * G + t * 128: gi * G + (t + 1) * 128, :])
                        xg.w["dma"] = xg.dma_sig(ins)
                        PE.rdep(xg)
                        for j in range(2):
                            bk = self.next_bank()
                            bk.new_gen()
                            PE.wdep(bk)
                            pv = bk.t[:].bitcast(BF16)
                            for i in range(8):
                                k = 8 * j + i
                                ins = nc.tensor.transpose(out=pv[:, i * 128:(i + 1) * 128], in_=xg.t[:, k * 128:(k + 1) * 128],
                                                          identity=self.ident_bf.t[:])
                            tok = self.pe_sig(ins)
                            bk.w["pe"] = tok
                            xg.r["pe"] = tok
                            E = ACT if j == 0 else DVE
                            E.rdep(bk)
                            E.wdep(x_T)
                            if E is ACT:
                                ins = nc.scalar.copy(out=x_T.t[:, 8 * j:8 * j + 8, t * 128:(t + 1) * 128], in_=pv.rearrange("p (j q) -> p j q", q=128))
                            else:
                                ins = nc.vector.tensor_copy(out=x_T.t[:, 8 * j:8 * j + 8, t * 128:(t + 1) * 128], in_=pv.rearrange("p (j q) -> p j q", q=128))
                            tok = E.sig(ins)
                            bk.r[E.name] = tok
                            x_T.w[E.name] = tok
                    self.swiglu_group(x_T, hT, nch, lambda c, ix=ix: (w1tab, ix.t[:, 0, c:c + 1], ix),
                                      lambda c, ix=ix: (w3tab, ix.t[:, 0, c:c + 1], ix), sring)

                    def evac_y(t, half, bks2, gi=gi):
                        yb = ysb.next()
                        yb.new_gen()
                        for n in range(2):
                            E = ACT if n == 0 else DVE
                            E.wdep(yb)
                            E.rdep(bks2[n])
                            if E is ACT:
                                ins = nc.scalar.copy(out=yb.t[:, n * 512:(n + 1) * 512], in_=bks2[n].t[:, :])
                            else:
                                ins = nc.vector.tensor_copy(out=yb.t[:, n * 512:(n + 1) * 512], in_=bks2[n].t[:, :])
                            tok = E.sig(ins)
                            yb.w[E.name] = tok
                            bks2[n].r[E.name] = tok
                        SP.rdep(yb)
                        ins = nc.sync.dma_start(out=Yg[gi * G + t * 128: gi * G + (t + 1) * 128, half * 1024:(half + 1) * 1024], in_=yb.t[:])
                        tok = yb.dma_sig(ins)
                        yb.r["dma"] = tok
                        self.track_dma(tok)
                    self.pass_B(hT, nch, lambda c_, half, ix=ix: (w2tab, ix.t[:, 1 + half, c_:c_ + 1], ix), G // 128, evac_y)
            self.barrier()
            with ExitStack() as st:
                self.make_resid_bufs(st)
                xc = Ring([self.sb(f"cx{i}", [128, D], F32, st) for i in range(2)])
                y1r = Ring([self.sb(f"cy1{i}", [128, D], F32, st) for i in range(2)])
                y2r = Ring([self.sb(f"cy2{i}", [128, D], F32, st) for i in range(2)])
                for tile in range(NT):
                    b = (tile * 128) // S
                    self.load_gate(1, 1, b)
                    x_ = xc.next()
                    x_.new_gen()
                    SP.wdep(x_)
                    SP.waits([self.xres_tok.get((tile, 0)), self.xres_tok.get((tile, 1))])
                    src = self.x_in if self.x0_is_input else self.xres
                    ins = nc.sync.dma_start(out=x_.t[:], in_=src[tile * 128:(tile + 1) * 128, :])
                    x_.w["dma"] = x_.dma_sig(ins)
                    ys = []
                    for j, ring in enumerate((y1r, y2r)):
                        y_ = ring.next()
                        y_.new_gen()
                        POOL.wdep(y_)
                        ins = nc.gpsimd.indirect_dma_start(out=y_.t[:], out_offset=None, in_=Yg[:, :],
                                                           in_offset=bass.IndirectOffsetOnAxis(ap=slots_i.t[:, tile, j:j + 1], axis=0))
                        y_.w["dma"] = y_.dma_sig(ins)
                        ys.append(y_)
                    DVE.rdep(ys[0])
                    DVE.rdep(ys[1])
                    DVE.rdep(x_)
                    DVE.rdep(self.gate)
                    DVE.rdep(wts)
                    ins = nc.vector.tensor_scalar(out=ys[0].t[:], in0=ys[0].t[:], scalar1=wts.t[:, tile, 0:1], scalar2=None, op0=ALU.mult)
                    DVE.wait(DVE.sig(ins))
                    ins = nc.vector.scalar_tensor_tensor(out=ys[0].t[:], in0=ys[1].t[:], scalar=wts.t[:, tile, 1:2], in1=ys[0].t[:],
                                                         op0=ALU.mult, op1=ALU.add)
                    tok = DVE.sig(ins)
                    ys[1].r["dve"] = tok
                    DVE.wait(tok)
                    ins = nc.vector.tensor_tensor(out=ys[0].t[:], in0=ys[0].t[:], in1=self.gate.t[:], op=ALU.mult)
                    tok = DVE.sig(ins)
                    self.gate.r["dve"] = tok
                    DVE.wait(tok)
                    ins = nc.vector.tensor_tensor(out=x_.t[:], in0=x_.t[:], in1=ys[0].t[:], op=ALU.add)
                    tok = DVE.sig(ins)
                    ys[0].r["dve"] = tok
                    x_.w["dve"] = tok
                    SP.rdep(x_)
                    ins = nc.sync.dma_start(out=self.xres[tile * 128:(tile + 1) * 128, :], in_=x_.t[:])
                    tok = x_.dma_sig(ins)
                    x_.r["dma"] = tok
                    self.track_dma(tok)
                    self.new_xres_tok[(tile, 0)] = tok
                    self.new_xres_tok[(tile, 1)] = tok
                self.commit_xres()
        self.x0_is_input = False

    def phase_final(self):
        nc = self.nc
        PE, ACT, DVE, POOL, SP = self.PE, self.ACT, self.DVE, self.POOL, self.SP
        with ExitStack() as st:
            xt_ring = Ring([self.sb(f"fxt{i}", [128, D], F32, st) for i in range(3)])
            junk = self.sb("fjunk", [128, D], BF16, st)
            ss_ring = Ring([self.sb(f"fss{i}", [128, 2], F32, st) for i in range(4)])
            fg = self.fin_g
            ins = nc.sync.dma_start(out=fg.t[:], in_=self.norm_final.partition_broadcast(128))
            fg.w["dma"] = fg.dma_sig(ins)
            self.track_dma(fg.w["dma"])
            for tile in range(self.NT):
                xt = xt_ring.next()
                xt.new_gen()
                SP.wdep(xt)
                if self.x0_is_input:
                    srcap = self.x_in[tile * 128:(tile + 1) * 128, :]
                else:
                    srcap = self.xres[tile * 128:(tile + 1) * 128, :]
                    SP.waits([self.xres_tok.get((tile, 0)), self.xres_tok.get((tile, 1))])
                ins = nc.sync.dma_start(out=xt.t[:], in_=srcap)
                xt.w["dma"] = xt.dma_sig(ins)
                ss = ss_ring.next()
                ss.new_gen()
                junk.new_gen()
                ACT.wdep(ss)
                ACT.wdep(junk)
                ACT.rdep(xt)
                ins = nc.scalar.memzero(ss.t[:])
                ACT.wait(ACT.sig(ins))
                ins = nc.scalar.activation(out=junk.t[:], in_=xt.t[:], func=AF.Square, accum_out=ss.t[:, 0:1])
                tok = ACT.sig(ins)
                ss.w["act"] = tok
                junk.w["act"] = tok
                ACT.wait(tok)
                ins = nc.scalar.activation(out=ss.t[:, 1:2], in_=ss.t[:, 0:1], func=AF.Sqrt, scale=1.0 / D, bias=self.eps_col.t[:, 0:1])
                ss.w["act"] = ACT.sig(ins)
                DVE.rdep(ss)
                ins = nc.vector.reciprocal(out=ss.t[:, 1:2], in_=ss.t[:, 1:2])
                DVE.wait(DVE.sig(ins))
                DVE.rdep(fg)
                DVE.rdep(xt)
                ins = nc.vector.scalar_tensor_tensor(out=xt.t[:], in0=xt.t[:], scalar=ss.t[:, 1:2], in1=fg.t[:], op0=ALU.mult, op1=ALU.mult)
                tok = DVE.sig(ins)
                ss.r["dve"] = tok
                xt.w["dve"] = tok
                SP.rdep(xt)
                ins = nc.sync.dma_start(out=self.out[tile * 128:(tile + 1) * 128, :], in_=xt.t[:])
                tok = xt.dma_sig(ins)
                xt.r["dma"] = tok
                self.track_dma(tok)


def relayout_A(w, nchunks):
    w = np.asarray(w, dtype=np.float32)
    return np.ascontiguousarray(w.reshape(KC, 128, nchunks, 128).transpose(2, 1, 0, 3)).reshape(nchunks, 128, D)


def prepare_inputs(inp, NE=8):
    shared = {}
    shared["ada_w"] = np.ascontiguousarray(inp["ada_w"], dtype=np.float32)
    shared["ada_b"] = np.ascontiguousarray(inp["ada_b"], dtype=np.float32)
    shared["norm_mix"] = np.ascontiguousarray(inp["norm_mix"], dtype=np.float32)
    shared["norm_ffn"] = np.ascontiguousarray(inp["norm_ffn"], dtype=np.float32)
    shared["norm_final"] = np.ascontiguousarray(inp["norm_final"], dtype=np.float32)
    shared["conv_w_in"] = relayout_A(inp["conv_w_in"][0], 48)
    shared["conv_kernel"] = np.ascontiguousarray(inp["conv_kernel"][0], dtype=np.float32)
    shared["conv_w_out"] = np.ascontiguousarray(inp["conv_w_out"][0], dtype=np.float32)
    fw = np.asarray(inp["fox_w_in"][0], dtype=np.float32)
    shared["fox_qk"] = relayout_A(fw[:, 0:2 * D], 32)
    shared["fox_v"] = np.ascontiguousarray(fw[:, 2 * D:3 * D])
    shared["fox_f"] = np.ascontiguousarray(fw[:, 3 * D:3 * D + NH])
    shared["fox_b_f"] = np.ascontiguousarray(inp["fox_b_f"][0], dtype=np.float32)
    shared["fox_w_out"] = np.ascontiguousarray(inp["fox_w_out"][0], dtype=np.float32)
    dff = inp["ffn_w1"].shape[-1]
    shared["ffn_w1"] = relayout_A(inp["ffn_w1"][0], dff // 128)
    shared["ffn_w3"] = relayout_A(inp["ffn_w3"][0], dff // 128)
    shared["ffn_w2"] = np.ascontiguousarray(inp["ffn_w2"][0], dtype=np.float32)
    shared["moe_router"] = np.ascontiguousarray(inp["moe_router"][0][:, :NE], dtype=np.float32)
    dffe = inp["moe_w1"].shape[-1]
    shared["moe_w1"] = np.concatenate([relayout_A(inp["moe_w1"][0][e], dffe // 128) for e in range(NE)], axis=0)
    shared["moe_w3"] = np.concatenate([relayout_A(inp["moe_w3"][0][e], dffe // 128) for e in range(NE)], axis=0)
    shared["moe_w2"] = np.ascontiguousarray(np.asarray(inp["moe_w2"][0][:NE], dtype=np.float32).reshape(NE * dffe, D))
    return shared


def kernel(**inputs):
    x = np.asarray(inputs["x"], dtype=np.float32)
    c = np.asarray(inputs["c"], dtype=np.float32)
    ncores = 8
    bsz, S, _ = x.shape
    nseq = bsz // ncores
    shared = prepare_inputs(inputs)
    kb = KB(NSEQ=nseq, S=S)
    nc = kb.build()
    in_maps = []
    for i in range(ncores):
        m = dict(shared)
        m["x"] = np.ascontiguousarray(x[i * nseq:(i + 1) * nseq].reshape(nseq * S, D))
        m["c"] = np.ascontiguousarray(c[i * nseq:(i + 1) * nseq])
        in_maps.append(m)
    res = run_bass_kernel_spmd(nc, in_maps, core_ids=list(range(ncores)))
    out = np.concatenate([r["out"].reshape(nseq, S, D) for r in res.results], axis=0)
    return out.astype(np.float32, copy=False)
```

```python
import numpy as np
from contextlib import ExitStack

import concourse.bass as bass
import concourse.mybir as mybir
from concourse.bass_utils import run_bass_kernel_spmd

F32 = mybir.dt.float32
BF16 = mybir.dt.bfloat16
AF = mybir.ActivationFunctionType
ALU = mybir.AluOpType
AX = mybir.AxisListType

D = 2048
KC = 16
HD = 128
NH = 16
EPS = 1e-6
SEM_LIMIT = 12000
G = 512


class Buf:
    def __init__(self, kb, t, name):
        self.kb = kb
        self.t = t
        self.name = name
        self.w = {}
        self.r = {}
        self.prev = []
        self.dsem = None
        self.dcount = 0

    def new_gen(self):
        self.prev = list(self.w.values()) + list(self.r.values())
        self.w = {}
        self.r = {}

    def dma_sig(self, ins):
        if self.dsem is None:
            self.dsem = self.kb.new_sem("d_" + self.name)
        self.dcount += 16
        ins.then_inc(self.dsem, 16)
        return (self.dsem, self.dcount)


class Eng:
    def __init__(self, kb, name, eng):
        self.kb = kb
        self.name = name
        self.eng = eng
        self.sem = None
        self.count = 0
        self.nsem = 0
        self.waited = {}

    def sig(self, ins):
        if self.sem is None or self.count >= SEM_LIMIT:
            self.sem = self.kb.new_sem(f"e_{self.name}{self.nsem}")
            self.nsem += 1
            self.count = 0
        self.count += 1
        ins.then_inc(self.sem, 1)
        return (self.sem, self.count)

    def wait(self, tok):
        if tok is None:
            return
        sem, val = tok
        key = id(sem)
        if self.waited.get(key, 0) >= val:
            return
        self.eng.wait_ge(sem, val)
        self.waited[key] = val

    def waits(self, toks):
        for t in toks:
            self.wait(t)

    def wdep(self, buf):
        self.waits(buf.prev)

    def rdep(self, buf):
        self.waits(buf.w.values())


class Ring:
    def __init__(self, bufs):
        self.bufs = bufs
        self.i = 0

    def next(self):
        b = self.bufs[self.i % len(self.bufs)]
        self.i += 1
        return b


class KB:
    def __init__(self, NSEQ=2, S=2048, NE=8, DFF=5632, DFFE=7168, phases=("ada", "conv", "ffn", "attn", "moe", "final"),
                 debug=False, moe_sparse=True):
        self.moe_sparse = moe_sparse
        self.NSEQ, self.S, self.NE, self.DFF, self.DFFE = NSEQ, S, NE, DFF, DFFE
        self.T = NSEQ * S
        self.NT = self.T // 128
        self.phases = phases
        self.debug = debug
        self.nc = bass.Bass("TRN2", target_bir_lowering=False)
        self.es = ExitStack()
        self.sems = []
        self.dma_toks = []
        nc = self.nc
        self.PE = Eng(self, "pe", nc.tensor)
        self.ACT = Eng(self, "act", nc.scalar)
        self.DVE = Eng(self, "dve", nc.vector)
        self.POOL = Eng(self, "pool", nc.gpsimd)
        self.SP = Eng(self, "sp", nc.sync)
        self.engs = [self.PE, self.ACT, self.DVE, self.POOL, self.SP]

    def new_sem(self, name):
        s = self.es.enter_context(self.nc.semaphore(name))
        self.sems.append(s)
        return s

    def sb(self, name, shape, dtype, stack=None):
        self.uid = getattr(self, "uid", 0) + 1
        name = f"{name}_{self.uid}"
        t = (stack or self.es).enter_context(self.nc.sbuf_tensor(name, shape, dtype))
        return Buf(self, t, name)

    def dram(self, name, shape, dtype, kind="Internal"):
        t = self.nc.dram_tensor(name, shape, dtype, kind=kind)
        return t.ap()

    def barrier(self):
        nc = self.nc
        toks = []
        toks.append(self.DVE.sig(nc.vector.memset(self.bar_dve.t[:], 0.0)))
        toks.append(self.ACT.sig(nc.scalar.copy(out=self.bar_act.t[:], in_=self.bar_src.t[:])))
        toks.append(self.POOL.sig(nc.gpsimd.memset(self.bar_pool.t[:], 0.0)))
        if self.last_pe_tok is not None:
            toks.append(self.last_pe_tok)
        toks += self.dma_toks
        self.dma_toks = []
        for e in self.engs:
            e.waits(toks)

    def track_dma(self, tok):
        self.dma_toks.append(tok)
        if len(self.dma_toks) > 64:
            last = {}
            for s, v in self.dma_toks:
                if id(s) not in last or last[id(s)][1] < v:
                    last[id(s)] = (s, v)
            self.dma_toks = list(last.values())

    def build(self):
        nc = self.nc
        NSEQ, T, NE = self.NSEQ, self.T, self.NE
        self.last_pe_tok = None
        P = self.phases
        self.input_names = []

        def inp(name, shape, need=True):
            if not need:
                return None
            self.input_names.append(name)
            return self.dram(name, shape, F32, "ExternalInput")

        self.x_in = inp("x", [T, D])
        self.c_in = inp("c", [NSEQ, D], "ada" in P)
        self.ada_w = inp("ada_w", [2, D, 6 * D], "ada" in P)
        self.ada_b = inp("ada_b", [2, 6 * D], "ada" in P)
        self.norm_mix = inp("norm_mix", [2, D], "ada" in P)
        self.norm_ffn = inp("norm_ffn", [2, D], "ada" in P)
        self.norm_final = inp("norm_final", [D], "final" in P)
        self.conv_w_in = inp("conv_w_in", [48, 128, D], "conv" in P)
        self.conv_kernel = inp("conv_kernel", [3, D], "conv" in P)
        self.conv_w_out = inp("conv_w_out", [D, D], "conv" in P)
        self.fox_qk = inp("fox_qk", [32, 128, D], "attn" in P)
        self.fox_v = inp("fox_v", [D, D], "attn" in P)
        self.fox_f = inp("fox_f", [D, NH], "attn" in P)
        self.fox_b_f = inp("fox_b_f", [NH], "attn" in P)
        self.fox_w_out = inp("fox_w_out", [D, D], "attn" in P)
        self.ffn_w1 = inp("ffn_w1", [self.DFF // 128, 128, D], "ffn" in P)
        self.ffn_w3 = inp("ffn_w3", [self.DFF // 128, 128, D], "ffn" in P)
        self.ffn_w2 = inp("ffn_w2", [self.DFF, D], "ffn" in P)
        self.moe_router = inp("moe_router", [D, NE], "moe" in P)
        self.moe_w1 = inp("moe_w1", [NE * (self.DFFE // 128), 128, D], "moe" in P)
        self.moe_w3 = inp("moe_w3", [NE * (self.DFFE // 128), 128, D], "moe" in P)
        self.moe_w2 = inp("moe_w2", [NE * self.DFFE, D], "moe" in P)
        self.out = self.dram("out", [T, D], F32, "ExternalOutput")
        self.xres = self.dram("xres", [T, D], F32, "ExternalOutput" if self.debug else "Internal")
        self.mod_d = self.dram("mod_d", [2, NSEQ, 6 * D], F32, "ExternalOutput" if self.debug else "Internal")
        self.xres_tok = {}
        self.x0_is_input = True

        self.ident_bf = self.sb("ident_bf", [128, 128], BF16)
        self.ident_f = self.sb("ident_f", [128, 128], F32)
        self.bar_dve = self.sb("bar_dve", [128, 1], F32)
        self.bar_act = self.sb("bar_act", [128, 1], F32)
        self.bar_pool = self.sb("bar_pool", [128, 1], F32)
        self.bar_src = self.sb("bar_src", [128, 1], F32)
        self.eps_col = self.sb("eps_col", [128, 1], F32)
        self.wring = Ring([self.sb(f"w{i}", [128, D], BF16) for i in range(8)])
        self.modcols = self.sb("modcols", [128, 2 * 2 * NSEQ * 2, KC], F32)
        self.fin_g = self.sb("fin_g", [128, D], F32)
        self.banks = []
        for i in range(8):
            t = self.es.enter_context(nc.psum_tensor(f"bank{i}", [128, 512], F32))
            self.banks.append(Buf(self, t, f"bank{i}"))
        self.bank_i = 0

        for idt in (self.ident_bf, self.ident_f):
            nc.gpsimd.memset(idt.t[:], 0.0)
            ins = nc.gpsimd.affine_select(out=idt.t[:], in_=idt.t[:], pattern=[[-1, 128]],
                                          compare_op=ALU.not_equal, fill=1.0, base=0, channel_multiplier=1)
            idt.w["pool"] = self.POOL.sig(ins)
        nc.gpsimd.memset(self.eps_col.t[:], EPS)
        ins = nc.gpsimd.memset(self.bar_src.t[:], 0.0)
        self.bar_src.w["pool"] = self.POOL.sig(ins)
        self.eps_col.w["pool"] = self.bar_src.w["pool"]
        self.ACT.rdep(self.bar_src)
        self.ACT.rdep(self.eps_col)

        self.precast_layer0()
        if "ada" in self.phases:
            self.phase_ada()
            self.barrier()
        if "conv" in self.phases:
            self.phase_conv()
            self.barrier()
        if "ffn" in self.phases:
            self.phase_ffn()
            self.barrier()
        if "attn" in self.phases:
            self.phase_attn()
            self.barrier()
        if "moe" in self.phases:
            if self.moe_sparse:
                self.phase_moe_sparse()
            else:
                self.phase_moe()
            self.barrier()
        if "final" in self.phases and not getattr(self, "final_done", False):
            self.phase_final()
        self.barrier()
        self.es.close()
        return nc

    def next_bank(self):
        b = self.banks[self.bank_i % 8]
        self.bank_i += 1
        return b

    def wload(self, src_ap, cols=D):
        b = self.wring.next()
        b.new_gen()
        self.POOL.wdep(b)
        if isinstance(src_ap, tuple):
            table, idx_ap, idx_buf = src_ap
            self.POOL.rdep(idx_buf)
            ins = self.nc.gpsimd.indirect_dma_start(out=b.t[:, 0:cols], out_offset=None, in_=table,
                                                    in_offset=bass.IndirectOffsetOnAxis(ap=idx_ap, axis=0))
            idx_buf.r["pool_dma"] = None
        else:
            ins = self.nc.gpsimd.dma_start(out=b.t[:, 0:cols], in_=src_ap)
        b.w["dma"] = b.dma_sig(ins)
        if isinstance(src_ap, tuple):
            src_ap[2].r["wdma" + b.name] = b.w["dma"]
        self.track_dma(b.w["dma"])
        return b

    def pe_sig(self, ins):
        tok = self.PE.sig(ins)
        self.last_pe_tok = tok
        return tok

    def modcol(self, layer, sub, b):
        idx = ((layer * 2 + sub) * self.NSEQ + b) * 2
        return self.modcols.t[:, idx, :], self.modcols.t[:, idx + 1, :]

    def precast_layer0(self):
        nc = self.nc
        todo = []
        if "conv" in self.phases:
            todo += [("conv_w_in", self.conv_w_in.rearrange("c p f -> (c p) f")), ("conv_w_out", self.conv_w_out)]
        if "ffn" in self.phases:
            todo += [("ffn_w1", self.ffn_w1.rearrange("c p f -> (c p) f")), ("ffn_w3", self.ffn_w3.rearrange("c p f -> (c p) f")),
                     ("ffn_w2", self.ffn_w2)]
        self.pc_sem = None
        npc = 0
        for name, src in todo:
            rows = src.shape[0]
            dst = self.dram(name + "_bf", [rows, D], BF16)
            if self.pc_sem is None:
                self.pc_sem = self.new_sem("precast")
            for r in range(0, rows, 128):
                ins = nc.gpsimd.dma_start(out=dst[r:r + 128, :], in_=src[r:r + 128, :])
                ins.then_inc(self.pc_sem, 16)
                npc += 1
            if name in ("conv_w_in", "ffn_w1", "ffn_w3"):
                setattr(self, name, dst.rearrange("(c p) f -> c p f", p=128))
            else:
                setattr(self, name, dst)
        if npc:
            self.track_dma((self.pc_sem, 16 * npc))

    def phase_ada(self):
        nc = self.nc
        NSEQ = self.NSEQ
        PE, ACT, DVE, POOL, SP = self.PE, self.ACT, self.DVE, self.POOL, self.SP
        with ExitStack() as st:
            cT = self.sb("cT", [128, KC, NSEQ], F32, st)
            cTa = self.sb("cTa", [128, KC, NSEQ], BF16, st)
            bias = Ring([self.sb(f"adab{i}", [NSEQ, D], F32, st) for i in range(2)])
            mrow = Ring([self.sb(f"mrow{i}", [NSEQ, D], F32, st) for i in range(2)])
            cols = self.sb("adacols", [128, 3, KC], F32, st)
            with nc.allow_non_contiguous_dma(reason="tiny transposed load of conditioning vector"):
                for b in range(NSEQ):
                    ins = nc.sync.dma_start(out=cT.t[:, :, b], in_=self.c_in[b, :].rearrange("(k p) -> p k", p=128))
                    cT.w["dma"] = cT.dma_sig(ins)
            ACT.rdep(cT)
            ins = nc.scalar.activation(out=cTa.t[:], in_=cT.t[:], func=AF.Silu)
            cTa.w["act"] = ACT.sig(ins)
            mod_store_toks = []
            for layer in range(2):
                for n in range(6):
                    bt = bias.next()
                    bt.new_gen()
                    SP.wdep(bt)
                    for b in range(NSEQ):
                        ins = nc.sync.dma_start(out=bt.t[b:b + 1, :], in_=self.ada_b[layer:layer + 1, n * D:(n + 1) * D])
                        bt.w["dma"] = bt.dma_sig(ins)
                    bks = [self.next_bank() for _ in range(4)]
                    for bk in bks:
                        bk.new_gen()
                        PE.wdep(bk)
                    PE.rdep(cTa)
                    for k in range(KC):
                        wb = self.wload(self.ada_w[layer, k * 128:(k + 1) * 128, n * D:(n + 1) * D])
                        PE.rdep(wb)
                        for j in range(4):
                            ins = nc.tensor.matmul(bks[j].t[0:NSEQ, :], lhsT=cTa.t[:, k, :], rhs=wb.t[:, j * 512:(j + 1) * 512],
                                                   start=(k == 0), stop=(k == KC - 1))
                        wb.r["pe"] = self.pe_sig(ins)
                    for bk in bks:
                        bk.w["pe"] = wb.r["pe"]
                    mr = mrow.next()
                    mr.new_gen()
                    DVE.wdep(mr)
                    DVE.rdep(bt)
                    for j in range(4):
                        DVE.rdep(bks[j])
                        ins = nc.vector.tensor_tensor(out=mr.t[:, j * 512:(j + 1) * 512], in0=bks[j].t[0:NSEQ, :],
                                                      in1=bt.t[:, j * 512:(j + 1) * 512], op=ALU.add)
                        tok = DVE.sig(ins)
                        bks[j].r["dve"] = tok
                    mr.w["dve"] = tok
                    bt.r["dve"] = tok
                    SP.rdep(mr)
                    ins = nc.sync.dma_start(out=self.mod_d[layer, :, n * D:(n + 1) * D], in_=mr.t[:])
                    tok = mr.dma_sig(ins)
                    mr.r["dma"] = tok
                    mod_store_toks.append(tok)
                    self.track_dma(tok)
            SP.waits(mod_store_toks)
            with nc.allow_non_contiguous_dma(reason="tiny transposed loads of modulation vectors"):
                for layer in range(2):
                    for sub in range(2):
                        gsrc = (self.norm_mix if sub == 0 else self.norm_ffn)[layer, :]
                        for b in range(NSEQ):
                            cols.new_gen()
                            SP.wdep(cols)
                            srcs = [gsrc, self.mod_d[layer, b, (3 * sub + 1) * D:(3 * sub + 2) * D],
                                    self.mod_d[layer, b, (3 * sub) * D:(3 * sub + 1) * D]]
                            for i, s_ in enumerate(srcs):
                                ins = nc.sync.dma_start(out=cols.t[:, i, :], in_=s_.rearrange("(k p) -> p k", p=128))
                                cols.w["dma"] = cols.dma_sig(ins)
                            self.track_dma(cols.w["dma"])
                            A, B = self.modcol(layer, sub, b)
                            DVE.rdep(cols)
                            ins = nc.vector.scalar_tensor_tensor(out=A, in0=cols.t[:, 1, :], scalar=1.0, in1=cols.t[:, 0, :],
                                                                 op0=ALU.add, op1=ALU.mult)
                            DVE.sig(ins)
                            ins = nc.vector.tensor_copy(out=B, in_=cols.t[:, 2, :])
                            tok = DVE.sig(ins)
                            cols.r["dve"] = tok
                            self.modcols.w["dve"] = tok
            self.mod_ready = tok

    def make_prep_bufs(self, st):
        self.xt_ring = Ring([self.sb(f"xt{i}", [128, D], F32, st) for i in range(2)])
        self.xs_ring = Ring([self.sb(f"xs{i}", [128, D], BF16, st) for i in range(2)])
        self.junk = self.sb("junk", [128, D], BF16, st)
        self.ss_ring = Ring([self.sb(f"ss{i}", [128, 2], F32, st) for i in range(4)])

    def prep(self, tile, A, B, dstT, col0, src=None):
        nc = self.nc
        PE, ACT, DVE, POOL, SP = self.PE, self.ACT, self.DVE, self.POOL, self.SP
        xt = self.xt_ring.next()
        xt.new_gen()
        SP.wdep(xt)
        if self.x0_is_input:
            srcap = self.x_in[tile * 128:(tile + 1) * 128, :]
        else:
            srcap = self.xres[tile * 128:(tile + 1) * 128, :]
            SP.waits([self.xres_tok.get((tile, 0)), self.xres_tok.get((tile, 1))])
        ins = nc.sync.dma_start(out=xt.t[:], in_=srcap)
        xt.w["dma"] = xt.dma_sig(ins)
        self.track_dma(xt.w["dma"])
        ss = self.ss_ring.next()
        ss.new_gen()
        self.junk.new_gen()
        ACT.wdep(ss)
        ACT.wdep(self.junk)
        ACT.rdep(xt)
        ins = nc.scalar.memzero(ss.t[:])
        ACT.wait(ACT.sig(ins))
        ins = nc.scalar.activation(out=self.junk.t[:], in_=xt.t[:], func=AF.Square, accum_out=ss.t[:, 0:1])
        tok = ACT.sig(ins)
        ss.w["act"] = tok
        self.junk.w["act"] = tok
        xt.r["act"] = tok
        ACT.wait(tok)
        ins = nc.scalar.activation(out=ss.t[:, 1:2], in_=ss.t[:, 0:1], func=AF.Sqrt, scale=1.0 / D, bias=self.eps_col.t[:, 0:1])
        ss.w["act"] = ACT.sig(ins)
        DVE.rdep(ss)
        ins = nc.vector.reciprocal(out=ss.t[:, 1:2], in_=ss.t[:, 1:2])
        DVE.wait(DVE.sig(ins))
        xs = self.xs_ring.next()
        xs.new_gen()
        DVE.wdep(xs)
        DVE.rdep(xt)
        ins = nc.vector.tensor_scalar(out=xs.t[:], in0=xt.t[:], scalar1=ss.t[:, 1:2], scalar2=None, op0=ALU.mult)
        tok = DVE.sig(ins)
        xs.w["dve"] = tok
        xt.r["dve"] = tok
        ss.r["dve"] = tok
        PE.rdep(xs)
        PE.rdep(self.ident_bf)
        for j in range(4):
            bk = self.next_bank()
            bk.new_gen()
            PE.wdep(bk)
            pv = bk.t[:].bitcast(BF16)
            for i in range(4):
                k = 4 * j + i
                ins = nc.tensor.transpose(out=pv[:, i * 128:(i + 1) * 128], in_=xs.t[:, k * 128:(k + 1) * 128],
                                          identity=self.ident_bf.t[:])
            tok = self.pe_sig(ins)
            bk.w["pe"] = tok
            xs.r["pe"] = tok
            E = ACT if j % 2 == 0 else DVE
            E.rdep(bk)
            E.wdep(dstT)
            E.rdep(self.modcols)
            for i in range(4):
                k = 4 * j + i
                if E is ACT:
                    ins = nc.scalar.activation(out=dstT.t[:, k, col0:col0 + 128], in_=pv[:, i * 128:(i + 1) * 128],
                                               func=AF.Identity, scale=A[:, k:k + 1], bias=B[:, k:k + 1])
                else:
                    ins = nc.vector.tensor_scalar(out=dstT.t[:, k, col0:col0 + 128], in0=pv[:, i * 128:(i + 1) * 128],
                                                  scalar1=A[:, k:k + 1], scalar2=B[:, k:k + 1], op0=ALU.mult, op1=ALU.add)
            tok = E.sig(ins)
            bk.r[E.name] = tok
            dstT.w[E.name] = tok

    def make_resid_bufs(self, st):
        self.xr_ring = Ring([self.sb(f"xr{i}", [128, 1024], F32, st) for i in range(3)])
        self.tmp_ring = Ring([self.sb(f"tmp{i}", [128, 512], F32, st) for i in range(3)])
        self.gate = self.sb("gate", [128, D], F32, st)
        self.gate_key = None

    def load_gate(self, layer, sub, b):
        key = (layer, sub, b)
        if self.gate_key == key:
            return
        self.gate_key = key
        nc = self.nc
        self.gate.new_gen()
        self.SP.wdep(self.gate)
        self.SP.wait(self.mod_ready)
        src = self.mod_d[layer, b, (3 * sub + 2) * D:(3 * sub + 3) * D].partition_broadcast(128)
        ins = nc.sync.dma_start(out=self.gate.t[:], in_=src)
        self.gate.w["dma"] = self.gate.dma_sig(ins)
        self.track_dma(self.gate.w["dma"])

    def pass_B(self, hT, nch, w2src, ntiles, evac):
        nc = self.nc
        PE = self.PE
        assert ntiles == 4
        for half in range(2):
            bks = {}
            for t in range(ntiles):
                for n in range(2):
                    bk = self.next_bank()
                    bk.new_gen()
                    PE.wdep(bk)
                    bks[(t, n)] = bk
            PE.rdep(hT)
            for c in range(nch):
                wb = self.wload(w2src(c, half), cols=1024)
                PE.rdep(wb)
                for t in range(ntiles):
                    for n in range(2):
                        ins = nc.tensor.matmul(bks[(t, n)].t[:, :], lhsT=hT.t[:, c, t * 128:(t + 1) * 128],
                                               rhs=wb.t[:, n * 512:(n + 1) * 512], start=(c == 0), stop=(c == nch - 1))
                wb.r["pe"] = self.pe_sig(ins)
            hT.r["pe"] = wb.r["pe"]
            for bk in bks.values():
                bk.w["pe"] = wb.r["pe"]
            for t in range(ntiles):
                evac(t, half, [bks[(t, 0)], bks[(t, 1)]])

    def evac_resid(self, tile0, layer, sub, rowscale=None):
        nc = self.nc
        DVE, SP = self.DVE, self.SP

        def evac(t, half, bks2):
            tile = tile0 + t
            b = (tile * 128) // self.S
            self.load_gate(layer, sub, b)
            xr = self.xr_ring.next()
            xr.new_gen()
            SP.wdep(xr)
            if self.x0_is_input:
                srcap = self.x_in[tile * 128:(tile + 1) * 128, half * 1024:(half + 1) * 1024]
            else:
                srcap = self.xres[tile * 128:(tile + 1) * 128, half * 1024:(half + 1) * 1024]
                SP.wait(self.xres_tok.get((tile, half)))
                SP.wait(self.new_xres_tok.get((tile, half)))
            ins = nc.sync.dma_start(out=xr.t[:], in_=srcap)
            xr.w["dma"] = xr.dma_sig(ins)
            DVE.rdep(xr)
            DVE.rdep(self.gate)
            for n in range(2):
                bk = bks2[n]
                DVE.rdep(bk)
                tmp = self.tmp_ring.next()
                tmp.new_gen()
                DVE.wdep(tmp)
                gsl = self.gate.t[:, half * 1024 + n * 512: half * 1024 + (n + 1) * 512]
                if rowscale is None:
                    ins = nc.vector.tensor_tensor(out=tmp.t[:], in0=bk.t[:, :], in1=gsl, op=ALU.mult)
                else:
                    ins = nc.vector.scalar_tensor_tensor(out=tmp.t[:], in0=bk.t[:, :], scalar=rowscale(t), in1=gsl,
                                                         op0=ALU.mult, op1=ALU.mult)
                tok = DVE.sig(ins)
                bk.r["dve"] = tok
                tmp.w["dve"] = tok
                DVE.wait(tok)
                ins = nc.vector.tensor_tensor(out=xr.t[:, n * 512:(n + 1) * 512], in0=xr.t[:, n * 512:(n + 1) * 512],
                                              in1=tmp.t[:], op=ALU.add)
                tok = DVE.sig(ins)
                tmp.r["dve"] = tok
            self.gate.r["dve"] = tok
            xr.w["dve"] = tok
            SP.rdep(xr)
            ins = nc.sync.dma_start(out=self.xres[tile * 128:(tile + 1) * 128, half * 1024:(half + 1) * 1024], in_=xr.t[:])
            tok = xr.dma_sig(ins)
            xr.r["dma"] = tok
            self.track_dma(tok)
            self.new_xres_tok[(tile, half)] = tok
        return evac

    def commit_xres(self):
        self.xres_tok.update(self.new_xres_tok)
        self.new_xres_tok = {}

    def phase_conv(self):
        nc = self.nc
        PE, ACT, DVE, POOL, SP = self.PE, self.ACT, self.DVE, self.POOL, self.SP
        self.new_xres_tok = {}
        with ExitStack() as st:
            self.make_prep_bufs(st)
            self.make_resid_bufs(st)
            hnT = Ring([self.sb(f"hnT{i}", [128, KC, G], BF16, st) for i in range(2)])
            zT = Ring([self.sb(f"zT{i}", [128, KC, G], BF16, st) for i in range(2)])
            csb = Ring([self.sb(f"csb{i}", [128, G], F32, st) for i in range(2)])
            ub = Ring([self.sb(f"ub{i}", [128, G + 2], F32, st) for i in range(2)])
            cv = Ring([self.sb(f"cv{i}", [128, G], F32, st) for i in range(2)])
            halo = self.sb("halo", [128, KC, 2], F32, st)
            kcol = self.sb("kcol", [128, 3, KC], F32, st)
            with nc.allow_non_contiguous_dma(reason="tiny transposed load of conv taps"):
                for w in range(3):
                    ins = nc.sync.dma_start(out=kcol.t[:, w, :], in_=self.conv_kernel[w, :].rearrange("(k p) -> p k", p=128))
                    kcol.w["dma"] = kcol.dma_sig(ins)
            self.track_dma(kcol.w["dma"])
            ngroups = self.T // G
            for g in range(ngroups):
                tile0 = g * (G // 128)
                b = (g * G) // self.S
                first_in_seq = (g * G) % self.S == 0
                A, B = self.modcol(0, 0, b)
                h = hnT.next()
                h.new_gen()
                for t in range(G // 128):
                    self.prep(tile0 + t, A, B, h, t * 128)
                z = zT.next()
                z.new_gen()
                if first_in_seq:
                    halo.new_gen()
                    POOL.wdep(halo)
                    ins = nc.gpsimd.memset(halo.t[:], 0.0)
                    halo.w = {"pool": POOL.sig(ins)}
                PE.rdep(h)
                for j in range(KC):
                    bks = []
                    for which in range(3):
                        wb = self.wload(self.conv_w_in[which * KC + j, :, :])
                        bk = self.next_bank()
                        bk.new_gen()
                        PE.wdep(bk)
                        PE.rdep(wb)
                        for k in range(KC):
                            ins = nc.tensor.matmul(bk.t[:, 0:G], lhsT=wb.t[:, k * 128:(k + 1) * 128], rhs=h.t[:, k, :],
                                                   start=(k == 0), stop=(k == KC - 1))
                        tok = self.pe_sig(ins)
                        wb.r["pe"] = tok
                        bk.w["pe"] = tok
                        bks.append(bk)
                    h.r["pe"] = tok
                    cs = csb.next()
                    cs.new_gen()
                    ACT.wdep(cs)
                    ACT.rdep(bks[1])
                    ins = nc.scalar.copy(out=cs.t[:], in_=bks[1].t[:, 0:G])
                    tok = ACT.sig(ins)
                    cs.w["act"] = tok
                    bks[1].r["act"] = tok
                    u = ub.next()
                    u.new_gen()
                    DVE.wdep(u)
                    DVE.rdep(cs)
                    DVE.rdep(bks[2])
                    DVE.rdep(halo)
                    ins = nc.vector.tensor_copy(out=u.t[:, 0:2], in_=halo.t[:, j, :])
                    DVE.sig(ins)
                    ins = nc.vector.tensor_tensor(out=u.t[:, 2:G + 2], in0=cs.t[:], in1=bks[2].t[:, 0:G], op=ALU.mult)
                    tok = DVE.sig(ins)
                    cs.r["dve"] = tok
                    bks[2].r["dve"] = tok
                    DVE.wait(tok)
                    ins = nc.vector.tensor_copy(out=halo.t[:, j, :], in_=u.t[:, G:G + 2])
                    halo.w["dve"] = DVE.sig(ins)
                    DVE.rdep(kcol)
                    c = cv.next()
                    c.new_gen()
                    DVE.wdep(c)
                    ins = nc.vector.tensor_scalar(out=c.t[:], in0=u.t[:, 2:G + 2], scalar1=kcol.t[:, 2, j:j + 1], scalar2=None,
                                                  op0=ALU.mult)
                    DVE.wait(DVE.sig(ins))
                    ins = nc.vector.scalar_tensor_tensor(out=c.t[:], in0=u.t[:, 1:G + 1], scalar=kcol.t[:, 1, j:j + 1], in1=c.t[:],
                                                         op0=ALU.mult, op1=ALU.add)
                    DVE.wait(DVE.sig(ins))
                    ins = nc.vector.scalar_tensor_tensor(out=c.t[:], in0=u.t[:, 0:G], scalar=kcol.t[:, 0, j:j + 1], in1=c.t[:],
                                                         op0=ALU.mult, op1=ALU.add)
                    tok = DVE.sig(ins)
                    u.r["dve"] = tok
                    DVE.wait(tok)
                    DVE.rdep(bks[0])
                    DVE.wdep(z)
                    ins = nc.vector.tensor_tensor(out=z.t[:, j, :], in0=c.t[:], in1=bks[0].t[:, 0:G], op=ALU.mult)
                    tok = DVE.sig(ins)
                    bks[0].r["dve"] = tok
                    c.r["dve"] = tok
                    z.w["dve"] = tok
                self.pass_B(z, KC, lambda c_, half: self.conv_w_out[c_ * 128:(c_ + 1) * 128, half * 1024:(half + 1) * 1024],
                            G // 128, self.evac_resid(tile0, 0, 0))
            self.commit_xres()
        self.x0_is_input = False

    def swiglu_group(self, h, hT, nch, w1src, w3src, sring):
        nc = self.nc
        PE, ACT, DVE = self.PE, self.ACT, self.DVE
        PE.rdep(h)
        hT.new_gen()
        for c in range(nch):
            bks = []
            for src in (w1src, w3src):
                wb = self.wload(src(c))
                bk = self.next_bank()
                bk.new_gen()
                PE.wdep(bk)
                PE.rdep(wb)
                for k in range(KC):
                    ins = self.nc.tensor.matmul(bk.t[:, 0:G], lhsT=wb.t[:, k * 128:(k + 1) * 128], rhs=h.t[:, k, :],
                                                start=(k == 0), stop=(k == KC - 1))
                tok = self.pe_sig(ins)
                wb.r["pe"] = tok
                bk.w["pe"] = tok
                bks.append(bk)
            h.r["pe"] = tok
            s = sring.next()
            s.new_gen()
            ACT.wdep(s)
            ACT.rdep(bks[0])
            ins = nc.scalar.activation(out=s.t[:], in_=bks[0].t[:, 0:G], func=AF.Silu)
            tok = ACT.sig(ins)
            s.w["act"] = tok
            bks[0].r["act"] = tok
            DVE.rdep(s)
            DVE.rdep(bks[1])
            DVE.wdep(hT)
            ins = nc.vector.tensor_tensor(out=hT.t[:, c, :], in0=s.t[:], in1=bks[1].t[:, 0:G], op=ALU.mult)
            tok = DVE.sig(ins)
            s.r["dve"] = tok
            bks[1].r["dve"] = tok
            hT.w["dve"] = tok

    def phase_ffn(self):
        nc = self.nc
        self.new_xres_tok = {}
        nch = self.DFF // 128
        with ExitStack() as st:
            self.make_prep_bufs(st)
            self.make_resid_bufs(st)
            hnT = Ring([self.sb(f"fhnT{i}", [128, KC, G], BF16, st) for i in range(2)])
            hT = self.sb("fhT", [128, nch, G], BF16, st)
            sring = Ring([self.sb(f"fs{i}", [128, G], BF16, st) for i in range(3)])
            for g in range(self.T // G):
                tile0 = g * (G // 128)
                b = (g * G) // self.S
                A, B = self.modcol(0, 1, b)
                h = hnT.next()
                h.new_gen()
                for t in range(G // 128):
                    self.prep(tile0 + t, A, B, h, t * 128)
                self.swiglu_group(h, hT, nch, lambda c: self.ffn_w1[c, :, :], lambda c: self.ffn_w3[c, :, :], sring)
                self.pass_B(hT, nch, lambda c_, half: self.ffn_w2[c_ * 128:(c_ + 1) * 128, half * 1024:(half + 1) * 1024],
                            G // 128, self.evac_resid(tile0, 0, 1))
            self.commit_xres()
        self.x0_is_input = False

    def phase_attn(self):
        nc = self.nc
        PE, ACT, DVE, POOL, SP = self.PE, self.ACT, self.DVE, self.POOL, self.SP
        S, T, NT = self.S, self.T, self.NT
        TPS = S // 128
        scale = float(HD) ** -0.5
        self.new_xres_tok = {}
        qkT_d = self.dram("qkT_d", [32, 128, T], BF16)
        v_d = self.dram("v_d", [T, D], BF16)
        o_d = self.dram("o_d", [T, D], BF16)
        cum_d = self.dram("cum_d", [NH, T], F32)
        with ExitStack() as st:
            self.make_prep_bufs(st)
            hnT = Ring([self.sb(f"ahnT{i}", [128, KC, G], BF16, st) for i in range(2)])
            qsb = Ring([self.sb(f"qsb{i}", [128, G], BF16, st) for i in range(3)])
            vsb = Ring([self.sb(f"vsb{i}", [128, 1024], BF16, st) for i in range(3)])
            wf = self.sb("wf", [128, KC, NH], BF16, st)
            bf_t = self.sb("bf_t", [128, NH], F32, st)
            one_c = self.sb("one_c", [128, 1], F32, st)
            tri = self.sb("tri", [128, 128], F32, st)
            ones = self.sb("ones", [128, 128], F32, st)
            lf = self.sb("lf", [128, NT, NH], F32, st)
            zt = Ring([self.sb(f"zt{i}", [128, NH], F32, st) for i in range(2)])
            cumt = Ring([self.sb(f"cumt{i}", [128, NH], F32, st) for i in range(2)])
            cumr = Ring([self.sb(f"cumr{i}", [NH, 128], F32, st) for i in range(2)])
            ins = nc.gpsimd.dma_start(out=wf.t[:], in_=self.fox_f.rearrange("(k p) n -> p k n", p=128))
            wf.w["dma"] = wf.dma_sig(ins)
            ins = nc.sync.dma_start(out=bf_t.t[:], in_=self.fox_b_f.partition_broadcast(128))
            bf_t.w["dma"] = bf_t.dma_sig(ins)
            nc.gpsimd.memset(one_c.t[:], 1.0)
            nc.gpsimd.memset(ones.t[:], 1.0)
            nc.gpsimd.memset(tri.t[:], 1.0)
            ins = nc.gpsimd.affine_select(out=tri.t[:], in_=tri.t[:], pattern=[[1, 128]], compare_op=ALU.is_ge, fill=0.0,
                                          base=0, channel_multiplier=-1)
            tok = POOL.sig(ins)
            tri.w["pool"] = tok
            ones.w["pool"] = tok
            one_c.w["pool"] = tok
            for g in range(T // G):
                tile0 = g * (G // 128)
                b = (g * G) // S
                A, B = self.modcol(1, 0, b)
                h = hnT.next()
                h.new_gen()
                for t in range(G // 128):
                    self.prep(tile0 + t, A, B, h, t * 128)
                PE.rdep(h)
                for ch in range(32):
                    wb = self.wload(self.fox_qk[ch, :, :])
                    bk = self.next_bank()
                    bk.new_gen()
                    PE.wdep(bk)
                    PE.rdep(wb)
                    for k in range(KC):
                        ins = nc.tensor.matmul(bk.t[:, 0:G], lhsT=wb.t[:, k * 128:(k + 1) * 128], rhs=h.t[:, k, :],
                                               start=(k == 0), stop=(k == KC - 1))
                    tok = self.pe_sig(ins)
                    wb.r["pe"] = tok
                    bk.w["pe"] = tok
                    q = qsb.next()
                    q.new_gen()
                    E = ACT if ch % 2 == 0 else DVE
                    E.wdep(q)
                    E.rdep(bk)
                    if E is ACT:
                        ins = nc.scalar.copy(out=q.t[:], in_=bk.t[:, 0:G])
                    else:
                        ins = nc.vector.tensor_copy(out=q.t[:], in_=bk.t[:, 0:G])
                    tok = E.sig(ins)
                    q.w[E.name] = tok
                    bk.r[E.name] = tok
                    SP.rdep(q)
                    ins = nc.sync.dma_start(out=qkT_d[ch, :, g * G:(g + 1) * G], in_=q.t[:])
                    tok = q.dma_sig(ins)
                    q.r["dma"] = tok
                    self.track_dma(tok)
                for t in range(G // 128):
                    tile = tile0 + t
                    bk = self.next_bank()
                    bk.new_gen()
                    PE.wdep(bk)
                    PE.rdep(wf)
                    for k in range(KC):
                        ins = nc.tensor.matmul(bk.t[:, 0:NH], lhsT=h.t[:, k, t * 128:(t + 1) * 128], rhs=wf.t[:, k, :],
                                               start=(k == 0), stop=(k == KC - 1))
                    tok = self.pe_sig(ins)
                    bk.w["pe"] = tok
                    z = zt.next()
                    z.new_gen()
                    DVE.wdep(z)
                    DVE.rdep(bk)
                    DVE.rdep(bf_t)
                    ins = nc.vector.tensor_tensor(out=z.t[:], in0=bk.t[:, 0:NH], in1=bf_t.t[:], op=ALU.add)
                    tok = DVE.sig(ins)
                    z.w["dve"] = tok
                    bk.r["dve"] = tok
                    ACT.rdep(z)
                    ACT.rdep(one_c)
                    ins = nc.scalar.activation(out=z.t[:], in_=z.t[:], func=AF.Exp, scale=-1.0)
                    ACT.wait(ACT.sig(ins))
                    ins = nc.scalar.activation(out=z.t[:], in_=z.t[:], func=AF.Ln, bias=one_c.t[:, 0:1])
                    ACT.wait(ACT.sig(ins))
                    ACT.waits(lf.prev)
                    ins = nc.scalar.mul(out=lf.t[:, tile, :], in_=z.t[:], mul=-1.0)
                    tok = ACT.sig(ins)
                    lf.w["act"] = tok
                    z.r["act"] = tok
                h.r["pe"] = self.last_pe_tok
                for t in range(G // 128):
                    tile = tile0 + t
                    seq0 = (tile // TPS) * TPS
                    bk = self.next_bank()
                    bk.new_gen()
                    PE.wdep(bk)
                    PE.rdep(lf)
                    PE.rdep(tri)
                    prevs = list(range(seq0, tile))
                    ins = nc.tensor.matmul(bk.t[:, 0:NH], lhsT=tri.t[:], rhs=lf.t[:, tile, :], start=True, stop=(len(prevs) == 0))
                    for i_, pt in enumerate(prevs):
                        ins = nc.tensor.matmul(bk.t[:, 0:NH], lhsT=ones.t[:], rhs=lf.t[:, pt, :], start=False,
                                               stop=(i_ == len(prevs) - 1))
                    tok = self.pe_sig(ins)
                    bk.w["pe"] = tok
                    lf.r["pe"] = tok
                    ct = cumt.next()
                    ct.new_gen()
                    DVE.wdep(ct)
                    DVE.rdep(bk)
                    ins = nc.vector.tensor_scalar(out=ct.t[:], in0=bk.t[:, 0:NH], scalar1=-1.0 / scale, scalar2=None, op0=ALU.mult)
                    tok = DVE.sig(ins)
                    ct.w["dve"] = tok
                    bk.r["dve"] = tok
                    bk2 = self.next_bank()
                    bk2.new_gen()
                    PE.wdep(bk2)
                    PE.rdep(ct)
                    PE.rdep(self.ident_f)
                    ins = nc.tensor.transpose(out=bk2.t[0:NH, 0:128], in_=ct.t[:], identity=self.ident_f.t[:])
                    tok = self.pe_sig(ins)
                    bk2.w["pe"] = tok
                    ct.r["pe"] = tok
                    cr = cumr.next()
                    cr.new_gen()
                    DVE.wdep(cr)
                    DVE.rdep(bk2)
                    ins = nc.vector.tensor_copy(out=cr.t[:], in_=bk2.t[0:NH, 0:128])
                    tok = DVE.sig(ins)
                    cr.w["dve"] = tok
                    bk2.r["dve"] = tok
                    SP.rdep(cr)
                    ins = nc.sync.dma_start(out=cum_d[:, tile * 128:(tile + 1) * 128], in_=cr.t[:])
                    tok = cr.dma_sig(ins)
                    cr.r["dma"] = tok
                    self.track_dma(tok)
                def evac_v(t, half, bks2, tile0=tile0):
                    vb = vsb.next()
                    vb.new_gen()
                    for n in range(2):
                        E = ACT if n == 0 else DVE
                        E.wdep(vb)
                        E.rdep(bks2[n])
                        if E is ACT:
                            ins = nc.scalar.copy(out=vb.t[:, n * 512:(n + 1) * 512], in_=bks2[n].t[:, :])
                        else:
                            ins = nc.vector.tensor_copy(out=vb.t[:, n * 512:(n + 1) * 512], in_=bks2[n].t[:, :])
                        tok = E.sig(ins)
                        vb.w[E.name] = tok
                        bks2[n].r[E.name] = tok
                    SP.rdep(vb)
                    ins = nc.sync.dma_start(out=v_d[(tile0 + t) * 128:(tile0 + t + 1) * 128, half * 1024:(half + 1) * 1024], in_=vb.t[:])
                    tok = vb.dma_sig(ins)
                    vb.r["dma"] = tok
                    self.track_dma(tok)
                self.pass_B(h, KC, lambda c_, half: self.fox_v[c_ * 128:(c_ + 1) * 128, half * 1024:(half + 1) * 1024],
                            G // 128, evac_v)
        self.barrier()
        with ExitStack() as st:
            qT = Ring([self.sb(f"qT{i}", [128, S], BF16, st) for i in range(2)])
            kT = Ring([self.sb(f"kT{i}", [128, S], BF16, st) for i in range(2)])
            vt = Ring([self.sb(f"vt{i}", [128, TPS, HD], BF16, st) for i in range(2)])
            bias = Ring([self.sb(f"abias{i}", [128, S], F32, st) for i in range(2)])
            Sb = Ring([self.sb(f"Sb{i}", [128, S], F32, st) for i in range(2)])
            Pb = Ring([self.sb(f"Pb{i}", [128, S], BF16, st) for i in range(4)])
            PT = Ring([self.sb(f"PT{i}", [128, TPS, 128], BF16, st) for i in range(2)])
            Oh = Ring([self.sb(f"Oh{i}", [128, TPS, HD], BF16, st) for i in range(2)])
            st_ring = Ring([self.sb(f"ast{i}", [128, 4], F32, st) for i in range(8)])
            trimask = self.sb("trimask", [128, 128], F32, st)
            bmr = Ring([self.sb(f"bm{i}", [128, TPS, 128], F32, st) for i in range(2)])
            nc.gpsimd.memset(trimask.t[:], 0.0)
            ins = nc.gpsimd.affine_select(out=trimask.t[:], in_=trimask.t[:], pattern=[[-1, 128]], compare_op=ALU.is_ge,
                                          fill=-1.0e30, base=0, channel_multiplier=1)
            trimask.w["pool"] = POOL.sig(ins)
            steps = [(seq, hh, i) for seq in range(self.NSEQ) for hh in range(NH) for i in range(TPS)]
            head_state = {}
            step_state = {}
            DEPTH = 2

            def a1(seq, hh, i):
                if i == 0:
                    q, kk, v, bs = qT.next(), kT.next(), vt.next(), bias.next()
                    for bf_, src in ((q, qkT_d[hh, :, seq * S:(seq + 1) * S]), (kk, qkT_d[16 + hh, :, seq * S:(seq + 1) * S]),
                                     (v, v_d[seq * S:(seq + 1) * S, hh * HD:(hh + 1) * HD].rearrange("(j p) d -> p j d", p=128)),
                                     (bs, cum_d[hh, seq * S:(seq + 1) * S].partition_broadcast(128))):
                        bf_.new_gen()
                        SP.wdep(bf_)
                        ins = nc.sync.dma_start(out=bf_.t[:], in_=src)
                        bf_.w["dma"] = bf_.dma_sig(ins)
                        self.track_dma(bf_.w["dma"])
                    bm = bmr.next()
                    bm.new_gen()
                    DVE.wdep(bm)
                    DVE.rdep(bs)
                    DVE.rdep(trimask)
                    for j in range(TPS):
                        ins = nc.vector.tensor_tensor(out=bm.t[:, j, :], in0=bs.t[:, j * 128:(j + 1) * 128], in1=trimask.t[:], op=ALU.add)
                    bm.w["dve"] = DVE.sig(ins)
                    oh = Oh.next()
                    oh.new_gen()
                    head_state[(seq, hh)] = (q, kk, v, bs, bm, oh)
                q, kk, v, bs, bm, oh = head_state[(seq, hh)]
                nk = (i + 1) * 128
                nkb = (nk + 511) // 512
                PE.rdep(q)
                PE.rdep(kk)
                bks = []
                for kb in range(nkb):
                    w = min(512, nk - kb * 512)
                    bk = self.next_bank()
                    bk.new_gen()
                    PE.wdep(bk)
                    ins = nc.tensor.matmul(bk.t[:, 0:w], lhsT=q.t[:, i * 128:(i + 1) * 128], rhs=kk.t[:, kb * 512:kb * 512 + w],
                                           start=True, stop=True)
                    bk.w["pe"] = self.pe_sig(ins)
                    bks.append((bk, kb, w))
                q.r["pe"] = self.last_pe_tok
                kk.r["pe"] = self.last_pe_tok
                step_state[(seq, hh, i)] = {"bks": bks}

            def a2(seq, hh, i):
                q, kk, v, bs, bm, oh = head_state[(seq, hh)]
                stt_ = step_state[(seq, hh, i)]
                nk = (i + 1) * 128
                sb_ = Sb.next()
                sb_.new_gen()
                DVE.wdep(sb_)
                DVE.rdep(bs)
                DVE.rdep(bm)
                for bk, kb, w in stt_["bks"]:
                    DVE.rdep(bk)
                    c0 = kb * 512
                    last = (c0 + w == nk)
                    wn = w - 128 if last else w
                    if wn > 0:
                        ins = nc.vector.tensor_tensor(out=sb_.t[:, c0:c0 + wn], in0=bk.t[:, 0:wn], in1=bs.t[:, c0:c0 + wn], op=ALU.add)
                        tok = DVE.sig(ins)
                    if last:
                        ins = nc.vector.tensor_tensor(out=sb_.t[:, c0 + wn:c0 + w], in0=bk.t[:, wn:w], in1=bm.t[:, i, :], op=ALU.add)
                        tok = DVE.sig(ins)
                    bk.r["dve"] = tok
                bs.r["dve"] = tok
                bm.r["dve"] = tok
                DVE.wait(tok)
                stt = st_ring.next()
                stt.new_gen()
                DVE.wdep(stt)
                ins = nc.vector.reduce_max(out=stt.t[:, 0:1], in_=sb_.t[:, 0:nk], axis=AX.X)
                DVE.wait(DVE.sig(ins))
                ins = nc.vector.tensor_scalar(out=stt.t[:, 1:2], in0=stt.t[:, 0:1], scalar1=-scale, scalar2=None, op0=ALU.mult)
                tok = DVE.sig(ins)
                stt.w["dve"] = tok
                sb_.w["dve"] = tok
                pb = Pb.next()
                pb.new_gen()
                ACT.wdep(pb)
                ACT.rdep(stt)
                ACT.rdep(sb_)
                ins = nc.scalar.memzero(stt.t[:, 2:3])
                ACT.wait(ACT.sig(ins))
                ins = nc.scalar.activation(out=pb.t[:, 0:nk], in_=sb_.t[:, 0:nk], func=AF.Exp, scale=scale, bias=stt.t[:, 1:2],
                                           accum_out=stt.t[:, 2:3])
                tok = ACT.sig(ins)
                pb.w["act"] = tok
                sb_.r["act"] = tok
                stt.w["act"] = tok
                stt_["pb"] = pb
                stt_["stt"] = stt

            def b_all(seq, hh, i):
                q, kk, v, bs, bm, oh = head_state[(seq, hh)]
                stt_ = step_state.pop((seq, hh, i))
                pb, stt = stt_["pb"], stt_["stt"]
                pt = PT.next()
                pt.new_gen()
                PE.rdep(pb)
                PE.rdep(self.ident_bf)
                nkt = i + 1
                for j0 in range(0, nkt, 8):
                    bk = self.next_bank()
                    bk.new_gen()
                    PE.wdep(bk)
                    pv = bk.t[:].bitcast(BF16)
                    cnt = min(8, nkt - j0)
                    for jj in range(cnt):
                        kt = j0 + jj
                        ins = nc.tensor.transpose(out=pv[:, jj * 128:(jj + 1) * 128], in_=pb.t[:, kt * 128:(kt + 1) * 128],
                                                  identity=self.ident_bf.t[:])
                    tok = self.pe_sig(ins)
                    bk.w["pe"] = tok
                    DVE.rdep(bk)
                    DVE.wdep(pt)
                    ins = nc.vector.tensor_copy(out=pt.t[:, j0:j0 + cnt, :], in_=pv[:, 0:cnt * 128].rearrange("p (j q) -> p j q", q=128))
                    tok = DVE.sig(ins)
                    bk.r["dve"] = tok
                    pt.w["dve"] = tok
                pb.r["pe"] = self.last_pe_tok
                bk = self.next_bank()
                bk.new_gen()
                PE.wdep(bk)
                PE.rdep(pt)
                PE.rdep(v)
                for kt in range(nkt):
                    ins = nc.tensor.matmul(bk.t[:, 0:HD], lhsT=pt.t[:, kt, :], rhs=v.t[:, kt, :], start=(kt == 0), stop=(kt == nkt - 1))
                tok = self.pe_sig(ins)
                bk.w["pe"] = tok
                pt.r["pe"] = tok
                v.r["pe"] = tok
                DVE.rdep(stt)
                ins = nc.vector.reciprocal(out=stt.t[:, 3:4], in_=stt.t[:, 2:3])
                stt.w["dve"] = DVE.sig(ins)
                ACT.rdep(stt)
                ACT.rdep(bk)
                ACT.wdep(oh)
                ins = nc.scalar.activation(out=oh.t[:, i, :], in_=bk.t[:, 0:HD], func=AF.Copy, scale=stt.t[:, 3:4])
                tok = ACT.sig(ins)
                bk.r["act"] = tok
                oh.w["act"] = tok
                stt.r["act"] = tok
                if i == TPS - 1:
                    SP.rdep(oh)
                    ins = nc.sync.dma_start(out=o_d[seq * S:(seq + 1) * S, hh * HD:(hh + 1) * HD].rearrange("(j p) d -> p j d", p=128),
                                            in_=oh.t[:])
                    tok = oh.dma_sig(ins)
                    oh.r["dma"] = tok
                    self.track_dma(tok)

            NS = len(steps)
            for n in range(min(DEPTH, NS)):
                a1(*steps[n])
                a2(*steps[n])
            for n in range(NS):
                if n + DEPTH < NS:
                    a1(*steps[n + DEPTH])
                b_all(*steps[n])
                if n + DEPTH < NS:
                    a2(*steps[n + DEPTH])
        self.barrier()
        with ExitStack() as st:
            self.make_resid_bufs(st)
            ot_ring = Ring([self.sb(f"otok{i}", [128, D], BF16, st) for i in range(2)])
            oT = Ring([self.sb(f"oT{i}", [128, KC, G], BF16, st) for i in range(2)])
            for g in range(T // G):
                tile0 = g * (G // 128)
                o_T = oT.next()
                o_T.new_gen()
                for t in range(G // 128):
                    ot = ot_ring.next()
                    ot.new_gen()
                    SP.wdep(ot)
                    ins = nc.sync.dma_start(out=ot.t[:], in_=o_d[(tile0 + t) * 128:(tile0 + t + 1) * 128, :])
                    ot.w["dma"] = ot.dma_sig(ins)
                    self.track_dma(ot.w["dma"])
                    PE.rdep(ot)
                    for j in range(2):
                        bk = self.next_bank()
                        bk.new_gen()
                        PE.wdep(bk)
                        pv = bk.t[:].bitcast(BF16)
                        for i in range(8):
                            k = 8 * j + i
                            ins = nc.tensor.transpose(out=pv[:, i * 128:(i + 1) * 128], in_=ot.t[:, k * 128:(k + 1) * 128],
                                                      identity=self.ident_bf.t[:])
                        tok = self.pe_sig(ins)
                        bk.w["pe"] = tok
                        ot.r["pe"] = tok
                        E = ACT if j == 0 else DVE
                        E.rdep(bk)
                        E.wdep(o_T)
                        for i in range(8):
                            k = 8 * j + i
                            if E is ACT:
                                ins = nc.scalar.copy(out=o_T.t[:, k, t * 128:(t + 1) * 128], in_=pv[:, i * 128:(i + 1) * 128])
                            else:
                                ins = nc.vector.tensor_copy(out=o_T.t[:, k, t * 128:(t + 1) * 128], in_=pv[:, i * 128:(i + 1) * 128])
                        tok = E.sig(ins)
                        bk.r[E.name] = tok
                        o_T.w[E.name] = tok
                self.pass_B(o_T, KC, lambda c_, half: self.fox_w_out[c_ * 128:(c_ + 1) * 128, half * 1024:(half + 1) * 1024],
                            G // 128, self.evac_resid(tile0, 1, 0))
            self.commit_xres()
        self.x0_is_input = False

    def phase_moe(self):
        nc = self.nc
        PE, ACT, DVE, POOL, SP = self.PE, self.ACT, self.DVE, self.POOL, self.SP
        NE, T, NT = self.NE, self.T, self.NT
        nch = self.DFFE // 128
        self.new_xres_tok = {}
        with ExitStack() as st:
            self.make_prep_bufs(st)
            self.make_resid_bufs(st)
            hnT = Ring([self.sb(f"mhnT{i}", [128, KC, G], BF16, st) for i in range(1)])
            hT = self.sb("mhT", [128, nch, G], BF16, st)
            sring = Ring([self.sb(f"ms{i}", [128, G], BF16, st) for i in range(3)])
            wr = self.sb("wr", [128, KC, NE], BF16, st)
            gates = self.sb("gates", [128, NT, NE], F32, st)
            lg = Ring([self.sb(f"lg{i}", [128, 4, NE], F32, st) for i in range(2)])
            sm = Ring([self.sb(f"sm{i}", [128, 8], F32, st) for i in range(2)])
            ins = nc.gpsimd.dma_start(out=wr.t[:], in_=self.moe_router.rearrange("(k p) n -> p k n", p=128))
            wr.w["dma"] = wr.dma_sig(ins)
            for g in range(T // G):
                tile0 = g * (G // 128)
                b = (g * G) // self.S
                A, B = self.modcol(1, 1, b)
                h = hnT.next()
                h.new_gen()
                for t in range(G // 128):
                    self.prep(tile0 + t, A, B, h, t * 128)
                PE.rdep(h)
                PE.rdep(wr)
                for t in range(G // 128):
                    tile = tile0 + t
                    bk = self.next_bank()
                    bk.new_gen()
                    PE.wdep(bk)
                    for k in range(KC):
                        ins = nc.tensor.matmul(bk.t[:, 0:NE], lhsT=h.t[:, k, t * 128:(t + 1) * 128], rhs=wr.t[:, k, :],
                                               start=(k == 0), stop=(k == KC - 1))
                    tok = self.pe_sig(ins)
                    bk.w["pe"] = tok
                    L = lg.next()
                    L.new_gen()
                    s_ = sm.next()
                    s_.new_gen()
                    DVE.wdep(L)
                    DVE.wdep(s_)
                    DVE.rdep(bk)
                    ins = nc.vector.tensor_copy(out=L.t[:, 0, :], in_=bk.t[:, 0:NE])
                    tok = DVE.sig(ins)
                    bk.r["dve"] = tok
                    DVE.wait(tok)
                    ins = nc.vector.reduce_max(out=s_.t[:, 0:1], in_=L.t[:, 0, :], axis=AX.X)
                    DVE.wait(DVE.sig(ins))
                    ins = nc.vector.tensor_scalar(out=L.t[:, 1, :], in0=L.t[:, 0, :], scalar1=s_.t[:, 0:1], scalar2=None, op0=ALU.is_equal)
                    DVE.wait(DVE.sig(ins))
                    ins = nc.vector.scalar_tensor_tensor(out=L.t[:, 2, :], in0=L.t[:, 1, :], scalar=-1.0e30, in1=L.t[:, 0, :],
                                                         op0=ALU.mult, op1=ALU.add)
                    DVE.wait(DVE.sig(ins))
                    ins = nc.vector.reduce_max(out=s_.t[:, 1:2], in_=L.t[:, 2, :], axis=AX.X)
                    DVE.wait(DVE.sig(ins))
                    ins = nc.vector.tensor_scalar(out=L.t[:, 3, :], in0=L.t[:, 2, :], scalar1=s_.t[:, 1:2], scalar2=None, op0=ALU.is_equal)
                    DVE.sig(ins)
                    ins = nc.vector.tensor_tensor(out=s_.t[:, 2:3], in0=s_.t[:, 1:2], in1=s_.t[:, 0:1], op=ALU.subtract)
                    tok = DVE.sig(ins)
                    s_.w["dve"] = tok
                    ACT.rdep(s_)
                    ins = nc.scalar.activation(out=s_.t[:, 3:4], in_=s_.t[:, 2:3], func=AF.Sigmoid, scale=-1.0)
                    ACT.sig(ins)
                    ins = nc.scalar.activation(out=s_.t[:, 4:5], in_=s_.t[:, 2:3], func=AF.Sigmoid, scale=1.0)
                    tok = ACT.sig(ins)
                    s_.w["act"] = tok
                    DVE.rdep(s_)
                    DVE.waits(gates.prev)
                    ins = nc.vector.tensor_scalar(out=gates.t[:, tile, :], in0=L.t[:, 1, :], scalar1=s_.t[:, 3:4], scalar2=None, op0=ALU.mult)
                    DVE.wait(DVE.sig(ins))
                    ins = nc.vector.scalar_tensor_tensor(out=gates.t[:, tile, :], in0=L.t[:, 3, :], scalar=s_.t[:, 4:5], in1=gates.t[:, tile, :],
                                                         op0=ALU.mult, op1=ALU.add)
                    tok = DVE.sig(ins)
                    gates.w["dve"] = tok
                    L.r["dve"] = tok
                    s_.r["dve"] = tok
                for e in range(NE):
                    self.swiglu_group(h, hT, nch, lambda c, e=e: self.moe_w1[e * nch + c, :, :], lambda c, e=e: self.moe_w3[e * nch + c, :, :], sring)
                    self.DVE.rdep(gates)
                    self.pass_B(hT, nch,
                                lambda c_, half, e=e: self.moe_w2[e * self.DFFE + c_ * 128: e * self.DFFE + (c_ + 1) * 128, half * 1024:(half + 1) * 1024],
                                G // 128, self.evac_resid(tile0, 1, 1, rowscale=lambda t, e=e, tile0=tile0: gates.t[:, tile0 + t, e:e + 1]))
                    self.commit_xres()
                    self.x0_is_input = False
            self.commit_xres()

    def phase_moe_sparse(self):
        nc = self.nc
        PE, ACT, DVE, POOL, SP = self.PE, self.ACT, self.DVE, self.POOL, self.SP
        NE, T, NT, S = self.NE, self.T, self.NT, self.S
        I32 = mybir.dt.int32
        nch = self.DFFE // 128
        NG = (2 * T) // G + NE - 1
        NSLOT = NG * G
        Xg = self.dram("Xg", [NSLOT, D], BF16)
        Yg = self.dram("Yg", [NSLOT, D], F32)
        hn_d = self.dram("hn_d", [T, D], BF16)
        w1tab = self.moe_w1.rearrange("c p f -> (c p) f")
        w3tab = self.moe_w3.rearrange("c p f -> (c p) f")
        w2tab = self.moe_w2.rearrange("r (h c) -> (r h) c", h=2)
        self.new_xres_tok = {}
        with ExitStack() as ph:
            a_bf = self.sb("a_bf", [128, NT, NE], BF16, ph)
            eq1f = self.sb("eq1f", [128, NT, NE], F32, ph)
            eq2f = self.sb("eq2f", [128, NT, NE], F32, ph)
            wts = self.sb("wts", [128, NT, 2], F32, ph)
            cnt = self.sb("cnt", [128, NT, NE], F32, ph)
            slots_f = self.sb("slots_f", [128, NT, 2], F32, ph)
            slots_i = self.sb("slots_i", [128, NT, 2], I32, ph)
            triS = self.sb("triS", [128, 128], BF16, ph)
            ones_bf = self.sb("ones_bf", [128, 128], BF16, ph)
            ntot = self.sb("ntot", [128, NE], F32, ph)
            padded = self.sb("padded", [128, NE], F32, ph)
            base = self.sb("base", [128, NE], F32, ph)
            endt = self.sb("endt", [128, NE], F32, ph)
            etab = self.sb("etab", [128, NG], F32, ph)
            iota_i = self.sb("iota_i", [128, nch], I32, ph)
            iota_f = self.sb("iota_f", [128, nch], F32, ph)
            wr = self.sb("wr", [128, KC, NE], BF16, ph)
            ins = nc.gpsimd.dma_start(out=wr.t[:], in_=self.moe_router.rearrange("(k p) n -> p k n", p=128))
            wr.w["dma"] = wr.dma_sig(ins)
            nc.gpsimd.memset(ones_bf.t[:], 1.0)
            nc.gpsimd.memset(triS.t[:], 1.0)
            nc.gpsimd.affine_select(out=triS.t[:], in_=triS.t[:], pattern=[[1, 128]], compare_op=ALU.is_ge, fill=0.0,
                                    base=-1, channel_multiplier=-1)
            ins = nc.gpsimd.iota(iota_i.t[:], pattern=[[128, nch]], base=0, channel_multiplier=1)
            tok = POOL.sig(ins)
            triS.w["pool"] = tok
            ones_bf.w["pool"] = tok
            iota_i.w["pool"] = tok
            DVE.rdep(iota_i)
            ins = nc.vector.tensor_copy(out=iota_f.t[:], in_=iota_i.t[:])
            iota_f.w["dve"] = DVE.sig(ins)
            with ExitStack() as st:
                xt_ring = Ring([self.sb(f"rxt{i}", [128, D], F32, st) for i in range(2)])
                hn_ring = Ring([self.sb(f"rhn{i}", [128, D], BF16, st) for i in range(2)])
                junk = self.sb("rjunk", [128, D], BF16, st)
                ss_ring = Ring([self.sb(f"rss{i}", [128, 2], F32, st) for i in range(4)])
                Arow = self.sb("Arow", [128, D], F32, st)
                Brow = self.sb("Brow", [128, D], F32, st)
                Trow = self.sb("Trow", [128, D], F32, st)
                hTr = Ring([self.sb(f"hTr{i}", [128, KC, 128], BF16, st) for i in range(2)])
                lg = Ring([self.sb(f"slg{i}", [128, 4, NE], F32, st) for i in range(2)])
                sm = Ring([self.sb(f"ssm{i}", [128, 8], F32, st) for i in range(2)])
                hn_tok = {}
                cur_b = None
                for tile in range(NT):
                    b = (tile * 128) // S
                    if b != cur_b:
                        cur_b = b
                        for bf_ in (Arow, Brow, Trow):
                            bf_.new_gen()
                            SP.wdep(bf_)
                        SP.wait(self.mod_ready)
                        ins = nc.sync.dma_start(out=Trow.t[:], in_=self.norm_ffn[1, :].partition_broadcast(128))
                        Trow.w["dma"] = Trow.dma_sig(ins)
                        ins = nc.sync.dma_start(out=Arow.t[:], in_=self.mod_d[1, b, 4 * D:5 * D].partition_broadcast(128))
                        Arow.w["dma"] = Arow.dma_sig(ins)
                        ins = nc.sync.dma_start(out=Brow.t[:], in_=self.mod_d[1, b, 3 * D:4 * D].partition_broadcast(128))
                        Brow.w["dma"] = Brow.dma_sig(ins)
                        DVE.rdep(Arow)
                        DVE.rdep(Trow)
                        DVE.rdep(Brow)
                        ins = nc.vector.scalar_tensor_tensor(out=Arow.t[:], in0=Arow.t[:], scalar=1.0, in1=Trow.t[:], op0=ALU.add, op1=ALU.mult)
                        tok = DVE.sig(ins)
                        Arow.w["dve"] = tok
                        Trow.r["dve"] = tok
                        DVE.wait(tok)
                    xt = xt_ring.next()
                    xt.new_gen()
                    SP.wdep(xt)
                    SP.waits([self.xres_tok.get((tile, 0)), self.xres_tok.get((tile, 1))])
                    src = self.x_in if self.x0_is_input else self.xres
                    ins = nc.sync.dma_start(out=xt.t[:], in_=src[tile * 128:(tile + 1) * 128, :])
                    xt.w["dma"] = xt.dma_sig(ins)
                    ss = ss_ring.next()
                    ss.new_gen()
                    junk.new_gen()
                    ACT.wdep(ss)
                    ACT.wdep(junk)
                    ACT.rdep(xt)
                    ins = nc.scalar.memzero(ss.t[:])
                    ACT.wait(ACT.sig(ins))
                    ins = nc.scalar.activation(out=junk.t[:], in_=xt.t[:], func=AF.Square, accum_out=ss.t[:, 0:1])
                    tok = ACT.sig(ins)
                    junk.w["act"] = tok
                    ACT.wait(tok)
                    ins = nc.scalar.activation(out=ss.t[:, 1:2], in_=ss.t[:, 0:1], func=AF.Sqrt, scale=1.0 / D, bias=self.eps_col.t[:, 0:1])
                    ss.w["act"] = ACT.sig(ins)
                    DVE.rdep(ss)
                    ins = nc.vector.reciprocal(out=ss.t[:, 1:2], in_=ss.t[:, 1:2])
                    DVE.wait(DVE.sig(ins))
                    DVE.rdep(xt)
                    ins = nc.vector.scalar_tensor_tensor(out=xt.t[:], in0=xt.t[:], scalar=ss.t[:, 1:2], in1=Arow.t[:], op0=ALU.mult, op1=ALU.mult)
                    tok = DVE.sig(ins)
                    ss.r["dve"] = tok
                    DVE.wait(tok)
                    hn = hn_ring.next()
                    hn.new_gen()
                    DVE.wdep(hn)
                    ins = nc.vector.tensor_tensor(out=hn.t[:], in0=xt.t[:], in1=Brow.t[:], op=ALU.add)
                    tok = DVE.sig(ins)
                    hn.w["dve"] = tok
                    xt.r["dve"] = tok
                    Arow.r["dve"] = tok
                    Brow.r["dve"] = tok
                    SP.rdep(hn)
                    ins = nc.sync.dma_start(out=hn_d[tile * 128:(tile + 1) * 128, :], in_=hn.t[:])
                    tok = hn.dma_sig(ins)
                    hn.r["dma"] = tok
                    hn_tok[tile] = tok
                    self.track_dma(tok)
                    hr = hTr.next()
                    hr.new_gen()
                    PE.rdep(hn)
                    PE.rdep(self.ident_bf)
                    for j in range(2):
                        bk = self.next_bank()
                        bk.new_gen()
                        PE.wdep(bk)
                        pv = bk.t[:].bitcast(BF16)
                        for i in range(8):
                            k = 8 * j + i
                            ins = nc.tensor.transpose(out=pv[:, i * 128:(i + 1) * 128], in_=hn.t[:, k * 128:(k + 1) * 128],
                                                      identity=self.ident_bf.t[:])
                        tok = self.pe_sig(ins)
                        bk.w["pe"] = tok
                        hn.r["pe"] = tok
                        E = ACT if j == 0 else DVE
                        E.rdep(bk)
                        E.wdep(hr)
                        if E is ACT:
                            ins = nc.scalar.copy(out=hr.t[:, 8 * j:8 * j + 8, :], in_=pv.rearrange("p (j q) -> p j q", q=128))
                        else:
                            ins = nc.vector.tensor_copy(out=hr.t[:, 8 * j:8 * j + 8, :], in_=pv.rearrange("p (j q) -> p j q", q=128))
                        tok = E.sig(ins)
                        bk.r[E.name] = tok
                        hr.w[E.name] = tok
                    bk = self.next_bank()
                    bk.new_gen()
                    PE.wdep(bk)
                    PE.rdep(hr)
                    PE.rdep(wr)
                    for k in range(KC):
                        ins = nc.tensor.matmul(bk.t[:, 0:NE], lhsT=hr.t[:, k, :], rhs=wr.t[:, k, :], start=(k == 0), stop=(k == KC - 1))
                    tok = self.pe_sig(ins)
                    bk.w["pe"] = tok
                    hr.r["pe"] = tok
                    L = lg.next()
                    L.new_gen()
                    s_ = sm.next()
                    s_.new_gen()
                    DVE.wdep(L)
                    DVE.wdep(s_)
                    DVE.rdep(bk)
                    ins = nc.vector.tensor_copy(out=L.t[:, 0, :], in_=bk.t[:, 0:NE])
                    tok = DVE.sig(ins)
                    bk.r["dve"] = tok
                    DVE.wait(tok)
                    ins = nc.vector.reduce_max(out=s_.t[:, 0:1], in_=L.t[:, 0, :], axis=AX.X)
                    DVE.wait(DVE.sig(ins))
                    ins = nc.vector.tensor_scalar(out=eq1f.t[:, tile, :], in0=L.t[:, 0, :], scalar1=s_.t[:, 0:1], scalar2=None, op0=ALU.is_equal)
                    DVE.wait(DVE.sig(ins))
                    ins = nc.vector.scalar_tensor_tensor(out=L.t[:, 2, :], in0=eq1f.t[:, tile, :], scalar=-1.0e30, in1=L.t[:, 0, :],
                                                         op0=ALU.mult, op1=ALU.add)
                    DVE.wait(DVE.sig(ins))
                    ins = nc.vector.reduce_max(out=s_.t[:, 1:2], in_=L.t[:, 2, :], axis=AX.X)
                    DVE.wait(DVE.sig(ins))
                    ins = nc.vector.tensor_scalar(out=eq2f.t[:, tile, :], in0=L.t[:, 2, :], scalar1=s_.t[:, 1:2], scalar2=None, op0=ALU.is_equal)
                    DVE.wait(DVE.sig(ins))
                    ins = nc.vector.tensor_tensor(out=a_bf.t[:, tile, :], in0=eq1f.t[:, tile, :], in1=eq2f.t[:, tile, :], op=ALU.add)
                    a_bf.w["dve"] = DVE.sig(ins)
                    ins = nc.vector.tensor_tensor(out=s_.t[:, 2:3], in0=s_.t[:, 1:2], in1=s_.t[:, 0:1], op=ALU.subtract)
                    tok = DVE.sig(ins)
                    s_.w["dve"] = tok
                    ACT.rdep(s_)
                    ins = nc.scalar.activation(out=wts.t[:, tile, 0:1], in_=s_.t[:, 2:3], func=AF.Sigmoid, scale=-1.0)
                    ACT.sig(ins)
                    ins = nc.scalar.activation(out=wts.t[:, tile, 1:2], in_=s_.t[:, 2:3], func=AF.Sigmoid, scale=1.0)
                    tok = ACT.sig(ins)
                    wts.w["act"] = tok
                    s_.r["act"] = tok
                    L.r["dve"] = a_bf.w["dve"]
                eq1f.w["dve"] = a_bf.w["dve"]
                eq2f.w["dve"] = a_bf.w["dve"]
                PE.rdep(a_bf)
                PE.rdep(triS)
                PE.rdep(ones_bf)
                for tile in range(NT):
                    bk = self.next_bank()
                    bk.new_gen()
                    PE.wdep(bk)
                    ins = nc.tensor.matmul(bk.t[:, 0:NE], lhsT=triS.t[:], rhs=a_bf.t[:, tile, :], start=True, stop=(tile == 0))
                    for pt in range(tile):
                        ins = nc.tensor.matmul(bk.t[:, 0:NE], lhsT=ones_bf.t[:], rhs=a_bf.t[:, pt, :], start=False, stop=(pt == tile - 1))
                    tok = self.pe_sig(ins)
                    bk.w["pe"] = tok
                    DVE.rdep(bk)
                    ins = nc.vector.tensor_copy(out=cnt.t[:, tile, :], in_=bk.t[:, 0:NE])
                    tok = DVE.sig(ins)
                    bk.r["dve"] = tok
                    cnt.w["dve"] = tok
                bk = self.next_bank()
                bk.new_gen()
                PE.wdep(bk)
                for tile in range(NT):
                    ins = nc.tensor.matmul(bk.t[:, 0:NE], lhsT=ones_bf.t[:], rhs=a_bf.t[:, tile, :], start=(tile == 0), stop=(tile == NT - 1))
                tok = self.pe_sig(ins)
                bk.w["pe"] = tok
                a_bf.r["pe"] = tok
                DVE.rdep(bk)
                ins = nc.vector.tensor_copy(out=ntot.t[:], in_=bk.t[:, 0:NE])
                tok = DVE.sig(ins)
                bk.r["dve"] = tok
                DVE.wait(tok)
                ins = nc.vector.memset(padded.t[:], 0.0)
                DVE.wait(DVE.sig(ins))
                for m in range(T // G):
                    ins = nc.vector.scalar_tensor_tensor(out=padded.t[:], in0=ntot.t[:], scalar=float(G * m), in1=padded.t[:],
                                                         op0=ALU.is_gt, op1=ALU.add)
                    DVE.wait(DVE.sig(ins))
                ins = nc.vector.tensor_scalar(out=padded.t[:], in0=padded.t[:], scalar1=float(G), scalar2=None, op0=ALU.mult)
                DVE.wait(DVE.sig(ins))
                ins = nc.vector.memset(base.t[:], 0.0)
                DVE.wait(DVE.sig(ins))
                for e in range(1, NE):
                    ins = nc.vector.tensor_tensor(out=base.t[:, e:e + 1], in0=base.t[:, e - 1:e], in1=padded.t[:, e - 1:e], op=ALU.add)
                    DVE.wait(DVE.sig(ins))
                ins = nc.vector.tensor_tensor(out=endt.t[:], in0=base.t[:], in1=padded.t[:], op=ALU.add)
                DVE.wait(DVE.sig(ins))
                tmp8 = lg.next()
                for gi in range(NG):
                    ins = nc.vector.tensor_scalar(out=tmp8.t[:, 0, :], in0=endt.t[:], scalar1=float(gi * G), scalar2=None, op0=ALU.is_le)
                    DVE.wait(DVE.sig(ins))
                    ins = nc.vector.reduce_sum(out=etab.t[:, gi:gi + 1], in_=tmp8.t[:, 0, :], axis=AX.X)
                    DVE.wait(DVE.sig(ins))
                ins = nc.vector.tensor_scalar(out=etab.t[:], in0=etab.t[:], scalar1=float(NE - 1), scalar2=None, op0=ALU.min)
                etab.w["dve"] = DVE.sig(ins)
                DVE.wait(etab.w["dve"])
                for tile in range(NT):
                    ins = nc.vector.tensor_tensor(out=tmp8.t[:, 1, :], in0=cnt.t[:, tile, :], in1=base.t[:], op=ALU.add)
                    DVE.wait(DVE.sig(ins))
                    ins = nc.vector.tensor_tensor(out=tmp8.t[:, 2, :], in0=tmp8.t[:, 1, :], in1=eq1f.t[:, tile, :], op=ALU.mult)
                    DVE.sig(ins)
                    ins = nc.vector.tensor_tensor(out=tmp8.t[:, 3, :], in0=tmp8.t[:, 1, :], in1=eq2f.t[:, tile, :], op=ALU.mult)
                    DVE.wait(DVE.sig(ins))
                    ins = nc.vector.reduce_sum(out=slots_f.t[:, tile, 0:1], in_=tmp8.t[:, 2, :], axis=AX.X)
                    DVE.sig(ins)
                    ins = nc.vector.reduce_sum(out=slots_f.t[:, tile, 1:2], in_=tmp8.t[:, 3, :], axis=AX.X)
                    DVE.wait(DVE.sig(ins))
                ins = nc.vector.tensor_copy(out=slots_i.t[:], in_=slots_f.t[:])
                slots_i.w["dve"] = DVE.sig(ins)
                POOL.rdep(slots_i)
                for tile in range(NT):
                    hn = hn_ring.next()
                    hn.new_gen()
                    SP.wdep(hn)
                    SP.wait(hn_tok[tile])
                    ins = nc.sync.dma_start(out=hn.t[:], in_=hn_d[tile * 128:(tile + 1) * 128, :])
                    hn.w["dma"] = hn.dma_sig(ins)
                    POOL.rdep(hn)
                    for j in range(2):
                        ins = nc.gpsimd.indirect_dma_start(out=Xg[:, :], out_offset=bass.IndirectOffsetOnAxis(ap=slots_i.t[:, tile, j:j + 1], axis=0),
                                                           in_=hn.t[:], in_offset=None)
                        tok = hn.dma_sig(ins)
                        hn.r["sc"] = tok
                        self.track_dma(tok)
            self.barrier()
            with ExitStack() as st:
                xg_ring = Ring([self.sb(f"xg{i}", [128, D], BF16, st) for i in range(2)])
                xT = Ring([self.sb(f"gxT{i}", [128, KC, G], BF16, st) for i in range(2)])
                hT = self.sb("ghT", [128, nch, G], BF16, st)
                sring = Ring([self.sb(f"gs{i}", [128, G], BF16, st) for i in range(3)])
                ysb = Ring([self.sb(f"ysb{i}", [128, 1024], F32, st) for i in range(3)])
                idxs = Ring([self.sb(f"idx{i}", [128, 3, nch], I32, st) for i in range(2)])
                idxf = self.sb("idxf", [128, nch], F32, st)
                ecol = self.sb("ecol", [128, 1], F32, st)
                for gi in range(NG):
                    ix = idxs.next()
                    ix.new_gen()
                    DVE.wdep(ix)
                    DVE.rdep(etab)
                    DVE.rdep(iota_f)
                    ins = nc.vector.tensor_scalar(out=ecol.t[:], in0=etab.t[:, gi:gi + 1], scalar1=float(self.DFFE), scalar2=None, op0=ALU.mult)
                    DVE.wait(DVE.sig(ins))
                    ins = nc.vector.tensor_scalar(out=idxf.t[:], in0=iota_f.t[:], scalar1=ecol.t[:, 0:1], scalar2=None, op0=ALU.add)
                    DVE.wait(DVE.sig(ins))
                    ins = nc.vector.tensor_copy(out=ix.t[:, 0, :], in_=idxf.t[:])
                    DVE.sig(ins)
                    ins = nc.vector.tensor_scalar(out=ix.t[:, 1, :], in0=idxf.t[:], scalar1=2.0, scalar2=None, op0=ALU.mult)
                    DVE.sig(ins)
                    ins = nc.vector.tensor_scalar(out=ix.t[:, 2, :], in0=idxf.t[:], scalar1=2.0, scalar2=1.0, op0=ALU.mult, op1=ALU.add)
                    tok = DVE.sig(ins)
                    ix.w["dve"] = tok
                    DVE.wait(tok)
                    x_T = xT.next()
                    x_T.new_gen()
                    for t in range(G // 128):
                        xg = xg_ring.next()
                        xg.new_gen()
                        SP.wdep(xg)
                        ins = nc.sync.dma_start(out=xg.t[:], in_=Xg[gi * G + t * 128: gi * G + (t + 1) * 128, :])
                        xg.w["dma"] = xg.dma_sig(ins)
                        PE.rdep(xg)
                        for j in range(2):
                            bk = self.next_bank()
                            bk.new_gen()
                            PE.wdep(bk)
                            pv = bk.t[:].bitcast(BF16)
                            for i in range(8):
                                k = 8 * j + i
                                ins = nc.tensor.transpose(out=pv[:, i * 128:(i + 1) * 128], in_=xg.t[:, k * 128:(k + 1) * 128],
                                                          identity=self.ident_bf.t[:])
                            tok = self.pe_sig(ins)
                            bk.w["pe"] = tok
                            xg.r["pe"] = tok
                            E = ACT if j == 0 else DVE
                            E.rdep(bk)
                            E.wdep(x_T)
                            if E is ACT:
                                ins = nc.scalar.copy(out=x_T.t[:, 8 * j:8 * j + 8, t * 128:(t + 1) * 128], in_=pv.rearrange("p (j q) -> p j q", q=128))
                            else:
                                ins = nc.vector.tensor_copy(out=x_T.t[:, 8 * j:8 * j + 8, t * 128:(t + 1) * 128], in_=pv.rearrange("p (j q) -> p j q", q=128))
                            tok = E.sig(ins)
                            bk.r[E.name] = tok
                            x_T.w[E.name] = tok
                    self.swiglu_group(x_T, hT, nch, lambda c, ix=ix: (w1tab, ix.t[:, 0, c:c + 1], ix),
                                      lambda c, ix=ix: (w3tab, ix.t[:, 0, c:c + 1], ix), sring)

                    def evac_y(t, half, bks2, gi=gi):
                        yb = ysb.next()
                        yb.new_gen()
                        for n in range(2):
                            E = ACT if n == 0 else DVE
                            E.wdep(yb)
                            E.rdep(bks2[n])
                            if E is ACT:
                                ins = nc.scalar.copy(out=yb.t[:, n * 512:(n + 1) * 512], in_=bks2[n].t[:, :])
                            else:
                                ins = nc.vector.tensor_copy(out=yb.t[:, n * 512:(n + 1) * 512], in_=bks2[n].t[:, :])
                            tok = E.sig(ins)
                            yb.w[E.name] = tok
                            bks2[n].r[E.name] = tok
                        SP.rdep(yb)
                        ins = nc.sync.dma_start(out=Yg[gi * G + t * 128: gi * G + (t + 1) * 128, half * 1024:(half + 1) * 1024], in_=yb.t[:])
                        tok = yb.dma_sig(ins)
                        yb.r["dma"] = tok
                        self.track_dma(tok)
                    self.pass_B(hT, nch, lambda c_, half, ix=ix: (w2tab, ix.t[:, 1 + half, c_:c_ + 1], ix), G // 128, evac_y)
            self.barrier()
            with ExitStack() as st:
                self.make_resid_bufs(st)
                xc = Ring([self.sb(f"cx{i}", [128, D], F32, st) for i in range(2)])
                y1r = Ring([self.sb(f"cy1{i}", [128, D], F32, st) for i in range(2)])
                y2r = Ring([self.sb(f"cy2{i}", [128, D], F32, st) for i in range(2)])
                fuse_final = "final" in self.phases
                if fuse_final:
                    ss_ring = Ring([self.sb(f"css{i}", [128, 2], F32, st) for i in range(4)])
                    cjunk = self.sb("cjunk", [128, D], BF16, st)
                    ins = nc.sync.dma_start(out=self.fin_g.t[:], in_=self.norm_final.partition_broadcast(128))
                    self.fin_g.w["dma"] = self.fin_g.dma_sig(ins)
                    self.track_dma(self.fin_g.w["dma"])
                    self.final_done = True
                for tile in range(NT):
                    b = (tile * 128) // S
                    self.load_gate(1, 1, b)
                    x_ = xc.next()
                    x_.new_gen()
                    SP.wdep(x_)
                    SP.waits([self.xres_tok.get((tile, 0)), self.xres_tok.get((tile, 1))])
                    src = self.x_in if self.x0_is_input else self.xres
                    ins = nc.sync.dma_start(out=x_.t[:], in_=src[tile * 128:(tile + 1) * 128, :])
                    x_.w["dma"] = x_.dma_sig(ins)
                    ys = []
                    for j, ring in enumerate((y1r, y2r)):
                        y_ = ring.next()
                        y_.new_gen()
                        POOL.wdep(y_)
                        ins = nc.gpsimd.indirect_dma_start(out=y_.t[:], out_offset=None, in_=Yg[:, :],
                                                           in_offset=bass.IndirectOffsetOnAxis(ap=slots_i.t[:, tile, j:j + 1], axis=0))
                        y_.w["dma"] = y_.dma_sig(ins)
                        ys.append(y_)
                    DVE.rdep(ys[0])
                    DVE.rdep(ys[1])
                    DVE.rdep(x_)
                    DVE.rdep(self.gate)
                    DVE.rdep(wts)
                    ins = nc.vector.tensor_scalar(out=ys[0].t[:], in0=ys[0].t[:], scalar1=wts.t[:, tile, 0:1], scalar2=None, op0=ALU.mult)
                    DVE.wait(DVE.sig(ins))
                    ins = nc.vector.scalar_tensor_tensor(out=ys[0].t[:], in0=ys[1].t[:], scalar=wts.t[:, tile, 1:2], in1=ys[0].t[:],
                                                         op0=ALU.mult, op1=ALU.add)
                    tok = DVE.sig(ins)
                    ys[1].r["dve"] = tok
                    DVE.wait(tok)
                    ins = nc.vector.tensor_tensor(out=ys[0].t[:], in0=ys[0].t[:], in1=self.gate.t[:], op=ALU.mult)
                    tok = DVE.sig(ins)
                    self.gate.r["dve"] = tok
                    DVE.wait(tok)
                    ins = nc.vector.tensor_tensor(out=x_.t[:], in0=x_.t[:], in1=ys[0].t[:], op=ALU.add)
                    tok = DVE.sig(ins)
                    ys[0].r["dve"] = tok
                    x_.w["dve"] = tok
                    if fuse_final:
                        ss = ss_ring.next()
                        ss.new_gen()
                        cjunk.new_gen()
                        ACT.wdep(ss)
                        ACT.wdep(cjunk)
                        ACT.rdep(x_)
                        ins = nc.scalar.memzero(ss.t[:])
                        ACT.wait(ACT.sig(ins))
                        ins = nc.scalar.activation(out=cjunk.t[:], in_=x_.t[:], func=AF.Square, accum_out=ss.t[:, 0:1])
                        tok = ACT.sig(ins)
                        cjunk.w["act"] = tok
                        ACT.wait(tok)
                        ins = nc.scalar.activation(out=ss.t[:, 1:2], in_=ss.t[:, 0:1], func=AF.Sqrt, scale=1.0 / D, bias=self.eps_col.t[:, 0:1])
                        ss.w["act"] = ACT.sig(ins)
                        DVE.rdep(ss)
                        ins = nc.vector.reciprocal(out=ss.t[:, 1:2], in_=ss.t[:, 1:2])
                        DVE.wait(DVE.sig(ins))
                        DVE.rdep(self.fin_g)
                        ins = nc.vector.scalar_tensor_tensor(out=x_.t[:], in0=x_.t[:], scalar=ss.t[:, 1:2], in1=self.fin_g.t[:], op0=ALU.mult, op1=ALU.mult)
                        tok = DVE.sig(ins)
                        ss.r["dve"] = tok
                        x_.w["dve"] = tok
                    SP.rdep(x_)
                    dst = self.out if fuse_final else self.xres
                    ins = nc.sync.dma_start(out=dst[tile * 128:(tile + 1) * 128, :], in_=x_.t[:])
                    tok = x_.dma_sig(ins)
                    x_.r["dma"] = tok
                    self.track_dma(tok)
                    self.new_xres_tok[(tile, 0)] = tok
                    self.new_xres_tok[(tile, 1)] = tok
                self.commit_xres()
        self.x0_is_input = False

    def phase_final(self):
        nc = self.nc
        PE, ACT, DVE, POOL, SP = self.PE, self.ACT, self.DVE, self.POOL, self.SP
        with ExitStack() as st:
            xt_ring = Ring([self.sb(f"fxt{i}", [128, D], F32, st) for i in range(3)])
            junk = self.sb("fjunk", [128, D], BF16, st)
            ss_ring = Ring([self.sb(f"fss{i}", [128, 2], F32, st) for i in range(4)])
            fg = self.fin_g
            ins = nc.sync.dma_start(out=fg.t[:], in_=self.norm_final.partition_broadcast(128))
            fg.w["dma"] = fg.dma_sig(ins)
            self.track_dma(fg.w["dma"])
            for tile in range(self.NT):
                xt = xt_ring.next()
                xt.new_gen()
                SP.wdep(xt)
                if self.x0_is_input:
                    srcap = self.x_in[tile * 128:(tile + 1) * 128, :]
                else:
                    srcap = self.xres[tile * 128:(tile + 1) * 128, :]
                    SP.waits([self.xres_tok.get((tile, 0)), self.xres_tok.get((tile, 1))])
                ins = nc.sync.dma_start(out=xt.t[:], in_=srcap)
                xt.w["dma"] = xt.dma_sig(ins)
                ss = ss_ring.next()
                ss.new_gen()
                junk.new_gen()
                ACT.wdep(ss)
                ACT.wdep(junk)
                ACT.rdep(xt)
                ins = nc.scalar.memzero(ss.t[:])
                ACT.wait(ACT.sig(ins))
                ins = nc.scalar.activation(out=junk.t[:], in_=xt.t[:], func=AF.Square, accum_out=ss.t[:, 0:1])
                tok = ACT.sig(ins)
                ss.w["act"] = tok
                junk.w["act"] = tok
                ACT.wait(tok)
                ins = nc.scalar.activation(out=ss.t[:, 1:2], in_=ss.t[:, 0:1], func=AF.Sqrt, scale=1.0 / D, bias=self.eps_col.t[:, 0:1])
                ss.w["act"] = ACT.sig(ins)
                DVE.rdep(ss)
                ins = nc.vector.reciprocal(out=ss.t[:, 1:2], in_=ss.t[:, 1:2])
                DVE.wait(DVE.sig(ins))
                DVE.rdep(fg)
                DVE.rdep(xt)
                ins = nc.vector.scalar_tensor_tensor(out=xt.t[:], in0=xt.t[:], scalar=ss.t[:, 1:2], in1=fg.t[:], op0=ALU.mult, op1=ALU.mult)
                tok = DVE.sig(ins)
                ss.r["dve"] = tok
                xt.w["dve"] = tok
                SP.rdep(xt)
                ins = nc.sync.dma_start(out=self.out[tile * 128:(tile + 1) * 128, :], in_=xt.t[:])
                tok = xt.dma_sig(ins)
                xt.r["dma"] = tok
                self.track_dma(tok)


def relayout_A(w, nchunks):
    w = np.asarray(w, dtype=np.float32)
    return np.ascontiguousarray(w.reshape(KC, 128, nchunks, 128).transpose(2, 1, 0, 3)).reshape(nchunks, 128, D)


def prepare_inputs(inp, NE=8):
    shared = {}
    shared["ada_w"] = np.ascontiguousarray(inp["ada_w"], dtype=np.float32)
    shared["ada_b"] = np.ascontiguousarray(inp["ada_b"], dtype=np.float32)
    shared["norm_mix"] = np.ascontiguousarray(inp["norm_mix"], dtype=np.float32)
    shared["norm_ffn"] = np.ascontiguousarray(inp["norm_ffn"], dtype=np.float32)
    shared["norm_final"] = np.ascontiguousarray(inp["norm_final"], dtype=np.float32)
    shared["conv_w_in"] = relayout_A(inp["conv_w_in"][0], 48)
    shared["conv_kernel"] = np.ascontiguousarray(inp["conv_kernel"][0], dtype=np.float32)
    shared["conv_w_out"] = np.ascontiguousarray(inp["conv_w_out"][0], dtype=np.float32)
    fw = np.asarray(inp["fox_w_in"][0], dtype=np.float32)
    shared["fox_qk"] = relayout_A(fw[:, 0:2 * D], 32)
    shared["fox_v"] = np.ascontiguousarray(fw[:, 2 * D:3 * D])
    shared["fox_f"] = np.ascontiguousarray(fw[:, 3 * D:3 * D + NH])
    shared["fox_b_f"] = np.ascontiguousarray(inp["fox_b_f"][0], dtype=np.float32)
    shared["fox_w_out"] = np.ascontiguousarray(inp["fox_w_out"][0], dtype=np.float32)
    dff = inp["ffn_w1"].shape[-1]
    shared["ffn_w1"] = relayout_A(inp["ffn_w1"][0], dff // 128)
    shared["ffn_w3"] = relayout_A(inp["ffn_w3"][0], dff // 128)
    shared["ffn_w2"] = np.ascontiguousarray(inp["ffn_w2"][0], dtype=np.float32)
    shared["moe_router"] = np.ascontiguousarray(inp["moe_router"][0][:, :NE], dtype=np.float32)
    dffe = inp["moe_w1"].shape[-1]
    shared["moe_w1"] = np.concatenate([relayout_A(inp["moe_w1"][0][e], dffe // 128) for e in range(NE)], axis=0)
    shared["moe_w3"] = np.concatenate([relayout_A(inp["moe_w3"][0][e], dffe // 128) for e in range(NE)], axis=0)
    shared["moe_w2"] = np.ascontiguousarray(np.asarray(inp["moe_w2"][0][:NE], dtype=np.float32).reshape(NE * dffe, D))
    return shared


def kernel(**inputs):
    x = np.asarray(inputs["x"], dtype=np.float32)
    c = np.asarray(inputs["c"], dtype=np.float32)
    ncores = 8
    bsz, S, _ = x.shape
    nseq = bsz // ncores
    shared = prepare_inputs(inputs)
    kb = KB(NSEQ=nseq, S=S)
    nc = kb.build()
    in_maps = []
    for i in range(ncores):
        m = dict(shared)
        m["x"] = np.ascontiguousarray(x[i * nseq:(i + 1) * nseq].reshape(nseq * S, D))
        m["c"] = np.ascontiguousarray(c[i * nseq:(i + 1) * nseq])
        in_maps.append(m)
    res = run_bass_kernel_spmd(nc, in_maps, core_ids=list(range(ncores)))
    out = np.concatenate([r["out"].reshape(nseq, S, D) for r in res.results], axis=0)
    return out.astype(np.float32, copy=False)
```

```python
import numpy as np
from contextlib import ExitStack

import concourse.bass as bass
import concourse.mybir as mybir
from concourse.bass_utils import run_bass_kernel_spmd

F32 = mybir.dt.float32
BF16 = mybir.dt.bfloat16
AF = mybir.ActivationFunctionType
ALU = mybir.AluOpType
AX = mybir.AxisListType

D = 2048
KC = 16
HD = 128
NH = 16
EPS = 1e-6
SEM_LIMIT = 12000
G = 512


class Buf:
    def __init__(self, kb, t, name):
        self.kb = kb
        self.t = t
        self.name = name
        self.w = {}
        self.r = {}
        self.prev = []
        self.dsem = None
        self.dcount = 0

    def new_gen(self):
        self.prev = list(self.w.values()) + list(self.r.values())
        self.w = {}
        self.r = {}

    def dma_sig(self, ins):
        if self.dsem is None:
            self.dsem = self.kb.new_sem("d_" + self.name)
        self.dcount += 16
        ins.then_inc(self.dsem, 16)
        return (self.dsem, self.dcount)


class Eng:
    def __init__(self, kb, name, eng):
        self.kb = kb
        self.name = name
        self.eng = eng
        self.sem = None
        self.count = 0
        self.nsem = 0
        self.waited = {}

    def sig(self, ins):
        if self.sem is None or self.count >= SEM_LIMIT:
            self.sem = self.kb.new_sem(f"e_{self.name}{self.nsem}")
            self.nsem += 1
            self.count = 0
        self.count += 1
        ins.then_inc(self.sem, 1)
        return (self.sem, self.count)

    def wait(self, tok):
        if tok is None:
            return
        sem, val = tok
        key = id(sem)
        if self.waited.get(key, 0) >= val:
            return
        self.eng.wait_ge(sem, val)
        self.waited[key] = val

    def waits(self, toks):
        for t in toks:
            self.wait(t)

    def wdep(self, buf):
        self.waits(buf.prev)

    def rdep(self, buf):
        self.waits(buf.w.values())


class Ring:
    def __init__(self, bufs):
        self.bufs = bufs
        self.i = 0

    def next(self):
        b = self.bufs[self.i % len(self.bufs)]
        self.i += 1
        return b


class KB:
    def __init__(self, NSEQ=2, S=2048, NE=8, DFF=5632, DFFE=7168, phases=("ada", "conv", "ffn", "attn", "moe", "final"),
                 debug=False, moe_sparse=True):
        self.moe_sparse = moe_sparse
        self.NSEQ, self.S, self.NE, self.DFF, self.DFFE = NSEQ, S, NE, DFF, DFFE
        self.T = NSEQ * S
        self.NT = self.T // 128
        self.phases = phases
        self.debug = debug
        self.nc = bass.Bass("TRN2", target_bir_lowering=False)
        self.es = ExitStack()
        self.sems = []
        self.dma_toks = []
        nc = self.nc
        self.PE = Eng(self, "pe", nc.tensor)
        self.ACT = Eng(self, "act", nc.scalar)
        self.DVE = Eng(self, "dve", nc.vector)
        self.POOL = Eng(self, "pool", nc.gpsimd)
        self.SP = Eng(self, "sp", nc.sync)
        self.engs = [self.PE, self.ACT, self.DVE, self.POOL, self.SP]

    def new_sem(self, name):
        s = self.es.enter_context(self.nc.semaphore(name))
        self.sems.append(s)
        return s

    def sb(self, name, shape, dtype, stack=None):
        self.uid = getattr(self, "uid", 0) + 1
        name = f"{name}_{self.uid}"
        t = (stack or self.es).enter_context(self.nc.sbuf_tensor(name, shape, dtype))
        return Buf(self, t, name)

    def dram(self, name, shape, dtype, kind="Internal"):
        t = self.nc.dram_tensor(name, shape, dtype, kind=kind)
        return t.ap()

    def barrier(self):
        nc = self.nc
        toks = []
        toks.append(self.DVE.sig(nc.vector.memset(self.bar_dve.t[:], 0.0)))
        toks.append(self.ACT.sig(nc.scalar.copy(out=self.bar_act.t[:], in_=self.bar_src.t[:])))
        toks.append(self.POOL.sig(nc.gpsimd.memset(self.bar_pool.t[:], 0.0)))
        if self.last_pe_tok is not None:
            toks.append(self.last_pe_tok)
        toks += self.dma_toks
        self.dma_toks = []
        for e in self.engs:
            e.waits(toks)

    def track_dma(self, tok):
        self.dma_toks.append(tok)
        if len(self.dma_toks) > 64:
            last = {}
            for s, v in self.dma_toks:
                if id(s) not in last or last[id(s)][1] < v:
                    last[id(s)] = (s, v)
            self.dma_toks = list(last.values())

    def build(self):
        nc = self.nc
        NSEQ, T, NE = self.NSEQ, self.T, self.NE
        self.last_pe_tok = None
        P = self.phases
        self.input_names = []

        def inp(name, shape, need=True):
            if not need:
                return None
            self.input_names.append(name)
            return self.dram(name, shape, F32, "ExternalInput")

        self.x_in = inp("x", [T, D])
        self.c_in = inp("c", [NSEQ, D], "ada" in P)
        self.ada_w = inp("ada_w", [2, D, 6 * D], "ada" in P)
        self.ada_b = inp("ada_b", [2, 6 * D], "ada" in P)
        self.norm_mix = inp("norm_mix", [2, D], "ada" in P)
        self.norm_ffn = inp("norm_ffn", [2, D], "ada" in P)
        self.norm_final = inp("norm_final", [D], "final" in P)
        self.conv_w_in = inp("conv_w_in", [48, 128, D], "conv" in P)
        self.conv_kernel = inp("conv_kernel", [3, D], "conv" in P)
        self.conv_w_out = inp("conv_w_out", [D, D], "conv" in P)
        self.fox_qk = inp("fox_qk", [32, 128, D], "attn" in P)
        self.fox_v = inp("fox_v", [D, D], "attn" in P)
        self.fox_f = inp("fox_f", [D, NH], "attn" in P)
        self.fox_b_f = inp("fox_b_f", [NH], "attn" in P)
        self.fox_w_out = inp("fox_w_out", [D, D], "attn" in P)
        self.ffn_w1 = inp("ffn_w1", [self.DFF // 128, 128, D], "ffn" in P)
        self.ffn_w3 = inp("ffn_w3", [self.DFF // 128, 128, D], "ffn" in P)
        self.ffn_w2 = inp("ffn_w2", [self.DFF, D], "ffn" in P)
        self.moe_router = inp("moe_router", [D, NE], "moe" in P)
        self.moe_w1 = inp("moe_w1", [NE * (self.DFFE // 128), 128, D], "moe" in P)
        self.moe_w3 = inp("moe_w3", [NE * (self.DFFE // 128), 128, D], "moe" in P)
        self.moe_w2 = inp("moe_w2", [NE * self.DFFE, D], "moe" in P)
        self.out = self.dram("out", [T, D], F32, "ExternalOutput")
        self.xres = self.dram("xres", [T, D], F32, "ExternalOutput" if self.debug else "Internal")
        self.mod_d = self.dram("mod_d", [2, NSEQ, 6 * D], F32, "ExternalOutput" if self.debug else "Internal")
        self.xres_tok = {}
        self.x0_is_input = True

        self.ident_bf = self.sb("ident_bf", [128, 128], BF16)
        self.ident_f = self.sb("ident_f", [128, 128], F32)
        self.bar_dve = self.sb("bar_dve", [128, 1], F32)
        self.bar_act = self.sb("bar_act", [128, 1], F32)
        self.bar_pool = self.sb("bar_pool", [128, 1], F32)
        self.bar_src = self.sb("bar_src", [128, 1], F32)
        self.eps_col = self.sb("eps_col", [128, 1], F32)
        self.wring = Ring([self.sb(f"w{i}", [128, D], BF16) for i in range(8)])
        self.modcols = self.sb("modcols", [128, 2 * 2 * NSEQ * 2, KC], F32)
        self.fin_g = self.sb("fin_g", [128, D], F32)
        self.banks = []
        for i in range(8):
            t = self.es.enter_context(nc.psum_tensor(f"bank{i}", [128, 512], F32))
            self.banks.append(Buf(self, t, f"bank{i}"))
        self.bank_i = 0

        for idt in (self.ident_bf, self.ident_f):
            nc.gpsimd.memset(idt.t[:], 0.0)
            ins = nc.gpsimd.affine_select(out=idt.t[:], in_=idt.t[:], pattern=[[-1, 128]],
                                          compare_op=ALU.not_equal, fill=1.0, base=0, channel_multiplier=1)
            idt.w["pool"] = self.POOL.sig(ins)
        nc.gpsimd.memset(self.eps_col.t[:], EPS)
        ins = nc.gpsimd.memset(self.bar_src.t[:], 0.0)
        self.bar_src.w["pool"] = self.POOL.sig(ins)
        self.eps_col.w["pool"] = self.bar_src.w["pool"]
        self.ACT.rdep(self.bar_src)
        self.ACT.rdep(self.eps_col)

        self.precast_layer0()
        if "ada" in self.phases:
            self.phase_ada()
            self.barrier()
        if "conv" in self.phases:
            self.phase_conv()
            self.barrier()
        if "ffn" in self.phases:
            self.phase_ffn()
            self.barrier()
        if "attn" in self.phases:
            self.phase_attn()
            self.barrier()
        if "moe" in self.phases:
            if self.moe_sparse:
                self.phase_moe_sparse()
            else:
                self.phase_moe()
            self.barrier()
        if "final" in self.phases and not getattr(self, "final_done", False):
            self.phase_final()
        self.barrier()
        self.es.close()
        return nc

    def next_bank(self):
        b = self.banks[self.bank_i % 8]
        self.bank_i += 1
        return b

    def wload(self, src_ap, cols=D):
        b = self.wring.next()
        b.new_gen()
        self.POOL.wdep(b)
        if isinstance(src_ap, tuple):
            table, idx_ap, idx_buf = src_ap
            self.POOL.rdep(idx_buf)
            ins = self.nc.gpsimd.indirect_dma_start(out=b.t[:, 0:cols], out_offset=None, in_=table,
                                                    in_offset=bass.IndirectOffsetOnAxis(ap=idx_ap, axis=0))
            idx_buf.r["pool_dma"] = None
        else:
            ins = self.nc.gpsimd.dma_start(out=b.t[:, 0:cols], in_=src_ap)
        b.w["dma"] = b.dma_sig(ins)
        if isinstance(src_ap, tuple):
            src_ap[2].r["wdma" + b.name] = b.w["dma"]
        self.track_dma(b.w["dma"])
        return b

    def pe_sig(self, ins):
        tok = self.PE.sig(ins)
        self.last_pe_tok = tok
        return tok

    def modcol(self, layer, sub, b):
        idx = ((layer * 2 + sub) * self.NSEQ + b) * 2
        return self.modcols.t[:, idx, :], self.modcols.t[:, idx + 1, :]

    def precast_layer0(self):
        nc = self.nc
        todo = []
        if "conv" in self.phases:
            todo += [("conv_w_in", self.conv_w_in.rearrange("c p f -> (c p) f")), ("conv_w_out", self.conv_w_out)]
        if "ffn" in self.phases:
            todo += [("ffn_w1", self.ffn_w1.rearrange("c p f -> (c p) f")), ("ffn_w3", self.ffn_w3.rearrange("c p f -> (c p) f")),
                     ("ffn_w2", self.ffn_w2)]
        self.pc_sem = None
        npc = 0
        for name, src in todo:
            rows = src.shape[0]
            dst = self.dram(name + "_bf", [rows, D], BF16)
            if self.pc_sem is None:
                self.pc_sem = self.new_sem("precast")
            for r in range(0, rows, 128):
                ins = nc.gpsimd.dma_start(out=dst[r:r + 128, :], in_=src[r:r + 128, :])
                ins.then_inc(self.pc_sem, 16)
                npc += 1
            if name in ("conv_w_in", "ffn_w1", "ffn_w3"):
                setattr(self, name, dst.rearrange("(c p) f -> c p f", p=128))
            else:
                setattr(self, name, dst)
        if npc:
            self.track_dma((self.pc_sem, 16 * npc))

    def phase_ada(self):
        nc = self.nc
        NSEQ = self.NSEQ
        PE, ACT, DVE, POOL, SP = self.PE, self.ACT, self.DVE, self.POOL, self.SP
        with ExitStack() as st:
            cT = self.sb("cT", [128, KC, NSEQ], F32, st)
            cTa = self.sb("cTa", [128, KC, NSEQ], BF16, st)
            bias = Ring([self.sb(f"adab{i}", [NSEQ, D], F32, st) for i in range(2)])
            mrow = Ring([self.sb(f"mrow{i}", [NSEQ, D], F32, st) for i in range(2)])
            cols = self.sb("adacols", [128, 3, KC], F32, st)
            with nc.allow_non_contiguous_dma(reason="tiny transposed load of conditioning vector"):
                for b in range(NSEQ):
                    ins = nc.sync.dma_start(out=cT.t[:, :, b], in_=self.c_in[b, :].rearrange("(k p) -> p k", p=128))
                    cT.w["dma"] = cT.dma_sig(ins)
            ACT.rdep(cT)
            ins = nc.scalar.activation(out=cTa.t[:], in_=cT.t[:], func=AF.Silu)
            cTa.w["act"] = ACT.sig(ins)
            mod_store_toks = []
            for layer in range(2):
                for n in range(6):
                    bt = bias.next()
                    bt.new_gen()
                    SP.wdep(bt)
                    for b in range(NSEQ):
                        ins = nc.sync.dma_start(out=bt.t[b:b + 1, :], in_=self.ada_b[layer:layer + 1, n * D:(n + 1) * D])
                        bt.w["dma"] = bt.dma_sig(ins)
                    bks = [self.next_bank() for _ in range(4)]
                    for bk in bks:
                        bk.new_gen()
                        PE.wdep(bk)
                    PE.rdep(cTa)
                    for k in range(KC):
                        wb = self.wload(self.ada_w[layer, k * 128:(k + 1) * 128, n * D:(n + 1) * D])
                        PE.rdep(wb)
                        for j in range(4):
                            ins = nc.tensor.matmul(bks[j].t[0:NSEQ, :], lhsT=cTa.t[:, k, :], rhs=wb.t[:, j * 512:(j + 1) * 512],
                                                   start=(k == 0), stop=(k == KC - 1))
                        wb.r["pe"] = self.pe_sig(ins)
                    for bk in bks:
                        bk.w["pe"] = wb.r["pe"]
                    mr = mrow.next()
                    mr.new_gen()
                    DVE.wdep(mr)
                    DVE.rdep(bt)
                    for j in range(4):
                        DVE.rdep(bks[j])
                        ins = nc.vector.tensor_tensor(out=mr.t[:, j * 512:(j + 1) * 512], in0=bks[j].t[0:NSEQ, :],
                                                      in1=bt.t[:, j * 512:(j + 1) * 512], op=ALU.add)
                        tok = DVE.sig(ins)
                        bks[j].r["dve"] = tok
                    mr.w["dve"] = tok
                    bt.r["dve"] = tok
                    SP.rdep(mr)
                    ins = nc.sync.dma_start(out=self.mod_d[layer, :, n * D:(n + 1) * D], in_=mr.t[:])
                    tok = mr.dma_sig(ins)
                    mr.r["dma"] = tok
                    mod_store_toks.append(tok)
                    self.track_dma(tok)
            SP.waits(mod_store_toks)
            with nc.allow_non_contiguous_dma(reason="tiny transposed loads of modulation vectors"):
                for layer in range(2):
                    for sub in range(2):
                        gsrc = (self.norm_mix if sub == 0 else self.norm_ffn)[layer, :]
                        for b in range(NSEQ):
                            cols.new_gen()
                            SP.wdep(cols)
                            srcs = [gsrc, self.mod_d[layer, b, (3 * sub + 1) * D:(3 * sub + 2) * D],
                                    self.mod_d[layer, b, (3 * sub) * D:(3 * sub + 1) * D]]
                            for i, s_ in enumerate(srcs):
                                ins = nc.sync.dma_start(out=cols.t[:, i, :], in_=s_.rearrange("(k p) -> p k", p=128))
                                cols.w["dma"] = cols.dma_sig(ins)
                            self.track_dma(cols.w["dma"])
                            A, B = self.modcol(layer, sub, b)
                            DVE.rdep(cols)
                            ins = nc.vector.scalar_tensor_tensor(out=A, in0=cols.t[:, 1, :], scalar=1.0, in1=cols.t[:, 0, :],
                                                                 op0=ALU.add, op1=ALU.mult)
                            DVE.sig(ins)
                            ins = nc.vector.tensor_copy(out=B, in_=cols.t[:, 2, :])
                            tok = DVE.sig(ins)
                            cols.r["dve"] = tok
                            self.modcols.w["dve"] = tok
            self.mod_ready = tok

    def make_prep_bufs(self, st):
        self.xt_ring = Ring([self.sb(f"xt{i}", [128, D], F32, st) for i in range(2)])
        self.xs_ring = Ring([self.sb(f"xs{i}", [128, D], BF16, st) for i in range(2)])
        self.junk = self.sb("junk", [128, D], BF16, st)
        self.ss_ring = Ring([self.sb(f"ss{i}", [128, 2], F32, st) for i in range(4)])

    def prep(self, tile, A, B, dstT, col0, src=None):
        nc = self.nc
        PE, ACT, DVE, POOL, SP = self.PE, self.ACT, self.DVE, self.POOL, self.SP
        xt = self.xt_ring.next()
        xt.new_gen()
        SP.wdep(xt)
        if self.x0_is_input:
            srcap = self.x_in[tile * 128:(tile + 1) * 128, :]
        else:
            srcap = self.xres[tile * 128:(tile + 1) * 128, :]
            SP.waits([self.xres_tok.get((tile, 0)), self.xres_tok.get((tile, 1))])
        ins = nc.sync.dma_start(out=xt.t[:], in_=srcap)
        xt.w["dma"] = xt.dma_sig(ins)
        self.track_dma(xt.w["dma"])
        ss = self.ss_ring.next()
        ss.new_gen()
        self.junk.new_gen()
        ACT.wdep(ss)
        ACT.wdep(self.junk)
        ACT.rdep(xt)
        ins = nc.scalar.memzero(ss.t[:])
        ACT.wait(ACT.sig(ins))
        ins = nc.scalar.activation(out=self.junk.t[:], in_=xt.t[:], func=AF.Square, accum_out=ss.t[:, 0:1])
        tok = ACT.sig(ins)
        ss.w["act"] = tok
        self.junk.w["act"] = tok
        xt.r["act"] = tok
        ACT.wait(tok)
        ins = nc.scalar.activation(out=ss.t[:, 1:2], in_=ss.t[:, 0:1], func=AF.Sqrt, scale=1.0 / D, bias=self.eps_col.t[:, 0:1])
        ss.w["act"] = ACT.sig(ins)
        DVE.rdep(ss)
        ins = nc.vector.reciprocal(out=ss.t[:, 1:2], in_=ss.t[:, 1:2])
        DVE.wait(DVE.sig(ins))
        xs = self.xs_ring.next()
        xs.new_gen()
        DVE.wdep(xs)
        DVE.rdep(xt)
        ins = nc.vector.tensor_scalar(out=xs.t[:], in0=xt.t[:], scalar1=ss.t[:, 1:2], scalar2=None, op0=ALU.mult)
        tok = DVE.sig(ins)
        xs.w["dve"] = tok
        xt.r["dve"] = tok
        ss.r["dve"] = tok
        PE.rdep(xs)
        PE.rdep(self.ident_bf)
        for j in range(4):
            bk = self.next_bank()
            bk.new_gen()
            PE.wdep(bk)
            pv = bk.t[:].bitcast(BF16)
            for i in range(4):
                k = 4 * j + i
                ins = nc.tensor.transpose(out=pv[:, i * 128:(i + 1) * 128], in_=xs.t[:, k * 128:(k + 1) * 128],
                                          identity=self.ident_bf.t[:])
            tok = self.pe_sig(ins)
            bk.w["pe"] = tok
            xs.r["pe"] = tok
            E = ACT if j % 2 == 0 else DVE
            E.rdep(bk)
            E.wdep(dstT)
            E.rdep(self.modcols)
            for i in range(4):
                k = 4 * j + i
                if E is ACT:
                    ins = nc.scalar.activation(out=dstT.t[:, k, col0:col0 + 128], in_=pv[:, i * 128:(i + 1) * 128],
                                               func=AF.Identity, scale=A[:, k:k + 1], bias=B[:, k:k + 1])
                else:
                    ins = nc.vector.tensor_scalar(out=dstT.t[:, k, col0:col0 + 128], in0=pv[:, i * 128:(i + 1) * 128],
                                                  scalar1=A[:, k:k + 1], scalar2=B[:, k:k + 1], op0=ALU.mult, op1=ALU.add)
            tok = E.sig(ins)
            bk.r[E.name] = tok
            dstT.w[E.name] = tok

    def make_resid_bufs(self, st):
        self.xr_ring = Ring([self.sb(f"xr{i}", [128, 1024], F32, st) for i in range(3)])
        self.tmp_ring = Ring([self.sb(f"tmp{i}", [128, 512], F32, st) for i in range(3)])
        self.gate = self.sb("gate", [128, D], F32, st)
        self.gate_key = None

    def load_gate(self, layer, sub, b):
        key = (layer, sub, b)
        if self.gate_key == key:
            return
        self.gate_key = key
        nc = self.nc
        self.gate.new_gen()
        self.SP.wdep(self.gate)
        self.SP.wait(self.mod_ready)
        src = self.mod_d[layer, b, (3 * sub + 2) * D:(3 * sub + 3) * D].partition_broadcast(128)
        ins = nc.sync.dma_start(out=self.gate.t[:], in_=src)
        self.gate.w["dma"] = self.gate.dma_sig(ins)
        self.track_dma(self.gate.w["dma"])

    def pass_B(self, hT, nch, w2src, ntiles, evac):
        nc = self.nc
        PE = self.PE
        assert ntiles == 4
        for half in range(2):
            bks = {}
            for t in range(ntiles):
                for n in range(2):
                    bk = self.next_bank()
                    bk.new_gen()
                    PE.wdep(bk)
                    bks[(t, n)] = bk
            PE.rdep(hT)
            for c in range(nch):
                wb = self.wload(w2src(c, half), cols=1024)
                PE.rdep(wb)
                for t in range(ntiles):
                    for n in range(2):
                        ins = nc.tensor.matmul(bks[(t, n)].t[:, :], lhsT=hT.t[:, c, t * 128:(t + 1) * 128],
                                               rhs=wb.t[:, n * 512:(n + 1) * 512], start=(c == 0), stop=(c == nch - 1))
                wb.r["pe"] = self.pe_sig(ins)
            hT.r["pe"] = wb.r["pe"]
            for bk in bks.values():
                bk.w["pe"] = wb.r["pe"]
            for t in range(ntiles):
                evac(t, half, [bks[(t, 0)], bks[(t, 1)]])

    def evac_resid(self, tile0, layer, sub, rowscale=None):
        nc = self.nc
        DVE, SP = self.DVE, self.SP

        def evac(t, half, bks2):
            tile = tile0 + t
            b = (tile * 128) // self.S
            self.load_gate(layer, sub, b)
            xr = self.xr_ring.next()
            xr.new_gen()
            SP.wdep(xr)
            if self.x0_is_input:
                srcap = self.x_in[tile * 128:(tile + 1) * 128, half * 1024:(half + 1) * 1024]
            else:
                srcap = self.xres[tile * 128:(tile + 1) * 128, half * 1024:(half + 1) * 1024]
                SP.wait(self.xres_tok.get((tile, half)))
                SP.wait(self.new_xres_tok.get((tile, half)))
            ins = nc.sync.dma_start(out=xr.t[:], in_=srcap)
            xr.w["dma"] = xr.dma_sig(ins)
            DVE.rdep(xr)
            DVE.rdep(self.gate)
            for n in range(2):
                bk = bks2[n]
                DVE.rdep(bk)
                tmp = self.tmp_ring.next()
                tmp.new_gen()
                DVE.wdep(tmp)
                gsl = self.gate.t[:, half * 1024 + n * 512: half * 1024 + (n + 1) * 512]
                if rowscale is None:
                    ins = nc.vector.tensor_tensor(out=tmp.t[:], in0=bk.t[:, :], in1=gsl, op=ALU.mult)
                else:
                    ins = nc.vector.scalar_tensor_tensor(out=tmp.t[:], in0=bk.t[:, :], scalar=rowscale(t), in1=gsl,
                                                         op0=ALU.mult, op1=ALU.mult)
                tok = DVE.sig(ins)
                bk.r["dve"] = tok
                tmp.w["dve"] = tok
                DVE.wait(tok)
                ins = nc.vector.tensor_tensor(out=xr.t[:, n * 512:(n + 1) * 512], in0=xr.t[:, n * 512:(n + 1) * 512],
                                              in1=tmp.t[:], op=ALU.add)
                tok = DVE.sig(ins)
                tmp.r["dve"] = tok
            self.gate.r["dve"] = tok
            xr.w["dve"] = tok
            SP.rdep(xr)
            ins = nc.sync.dma_start(out=self.xres[tile * 128:(tile + 1) * 128, half * 1024:(half + 1) * 1024], in_=xr.t[:])
            tok = xr.dma_sig(ins)
            xr.r["dma"] = tok
            self.track_dma(tok)
            self.new_xres_tok[(tile, half)] = tok
        return evac

    def commit_xres(self):
        self.xres_tok.update(self.new_xres_tok)
        self.new_xres_tok = {}

    def phase_conv(self):
        nc = self.nc
        PE, ACT, DVE, POOL, SP = self.PE, self.ACT, self.DVE, self.POOL, self.SP
        self.new_xres_tok = {}
        with ExitStack() as st:
            self.make_prep_bufs(st)
            self.make_resid_bufs(st)
            hnT = Ring([self.sb(f"hnT{i}", [128, KC, G], BF16, st) for i in range(2)])
            zT = Ring([self.sb(f"zT{i}", [128, KC, G], BF16, st) for i in range(2)])
            csb = Ring([self.sb(f"csb{i}", [128, G], F32, st) for i in range(2)])
            ub = Ring([self.sb(f"ub{i}", [128, G + 2], F32, st) for i in range(2)])
            cv = Ring([self.sb(f"cv{i}", [128, G], F32, st) for i in range(2)])
            halo = self.sb("halo", [128, KC, 2], F32, st)
            kcol = self.sb("kcol", [128, 3, KC], F32, st)
            with nc.allow_non_contiguous_dma(reason="tiny transposed load of conv taps"):
                for w in range(3):
                    ins = nc.sync.dma_start(out=kcol.t[:, w, :], in_=self.conv_kernel[w, :].rearrange("(k p) -> p k", p=128))
                    kcol.w["dma"] = kcol.dma_sig(ins)
            self.track_dma(kcol.w["dma"])
            ngroups = self.T // G
            conv_next = {}
            for g in range(ngroups):
                tile0 = g * (G // 128)
                b = (g * G) // self.S
                first_in_seq = (g * G) % self.S == 0
                if g == 0:
                    A, B = self.modcol(0, 0, b)
                    h = hnT.next()
                    h.new_gen()
                    for t in range(G // 128):
                        self.prep(tile0 + t, A, B, h, t * 128)
                else:
                    h = conv_next.pop(g)
                z = zT.next()
                z.new_gen()
                if first_in_seq:
                    halo.new_gen()
                    POOL.wdep(halo)
                    ins = nc.gpsimd.memset(halo.t[:], 0.0)
                    halo.w = {"pool": POOL.sig(ins)}
                PE.rdep(h)
                for j in range(KC):
                    if j == KC // 2 and g + 1 < ngroups:
                        g2 = g + 1
                        A2, B2 = self.modcol(0, 0, (g2 * G) // self.S)
                        h2 = hnT.next()
                        h2.new_gen()
                        for t in range(G // 128):
                            self.prep(g2 * (G // 128) + t, A2, B2, h2, t * 128)
                        conv_next[g2] = h2
                        PE.rdep(h)
                    bks = []
                    for which in range(3):
                        wb = self.wload(self.conv_w_in[which * KC + j, :, :])
                        bk = self.next_bank()
                        bk.new_gen()
                        PE.wdep(bk)
                        PE.rdep(wb)
                        for k in range(KC):
                            ins = nc.tensor.matmul(bk.t[:, 0:G], lhsT=wb.t[:, k * 128:(k + 1) * 128], rhs=h.t[:, k, :],
                                                   start=(k == 0), stop=(k == KC - 1))
                        tok = self.pe_sig(ins)
                        wb.r["pe"] = tok
                        bk.w["pe"] = tok
                        bks.append(bk)
                    h.r["pe"] = tok
                    cs = csb.next()
                    cs.new_gen()
                    ACT.wdep(cs)
                    ACT.rdep(bks[1])
                    ins = nc.scalar.copy(out=cs.t[:], in_=bks[1].t[:, 0:G])
                    tok = ACT.sig(ins)
                    cs.w["act"] = tok
                    bks[1].r["act"] = tok
                    u = ub.next()
                    u.new_gen()
                    DVE.wdep(u)
                    DVE.rdep(cs)
                    DVE.rdep(bks[2])
                    DVE.rdep(halo)
                    ins = nc.vector.tensor_copy(out=u.t[:, 0:2], in_=halo.t[:, j, :])
                    DVE.sig(ins)
                    ins = nc.vector.tensor_tensor(out=u.t[:, 2:G + 2], in0=cs.t[:], in1=bks[2].t[:, 0:G], op=ALU.mult)
                    tok = DVE.sig(ins)
                    cs.r["dve"] = tok
                    bks[2].r["dve"] = tok
                    DVE.wait(tok)
                    ins = nc.vector.tensor_copy(out=halo.t[:, j, :], in_=u.t[:, G:G + 2])
                    halo.w["dve"] = DVE.sig(ins)
                    DVE.rdep(kcol)
                    c = cv.next()
                    c.new_gen()
                    DVE.wdep(c)
                    ins = nc.vector.tensor_scalar(out=c.t[:], in0=u.t[:, 2:G + 2], scalar1=kcol.t[:, 2, j:j + 1], scalar2=None,
                                                  op0=ALU.mult)
                    DVE.wait(DVE.sig(ins))
                    ins = nc.vector.scalar_tensor_tensor(out=c.t[:], in0=u.t[:, 1:G + 1], scalar=kcol.t[:, 1, j:j + 1], in1=c.t[:],
                                                         op0=ALU.mult, op1=ALU.add)
                    DVE.wait(DVE.sig(ins))
                    ins = nc.vector.scalar_tensor_tensor(out=c.t[:], in0=u.t[:, 0:G], scalar=kcol.t[:, 0, j:j + 1], in1=c.t[:],
                                                         op0=ALU.mult, op1=ALU.add)
                    tok = DVE.sig(ins)
                    u.r["dve"] = tok
                    DVE.wait(tok)
                    DVE.rdep(bks[0])
                    DVE.wdep(z)
                    ins = nc.vector.tensor_tensor(out=z.t[:, j, :], in0=c.t[:], in1=bks[0].t[:, 0:G], op=ALU.mult)
                    tok = DVE.sig(ins)
                    bks[0].r["dve"] = tok
                    c.r["dve"] = tok
                    z.w["dve"] = tok
                self.pass_B(z, KC, lambda c_, half: self.conv_w_out[c_ * 128:(c_ + 1) * 128, half * 1024:(half + 1) * 1024],
                            G // 128, self.evac_resid(tile0, 0, 0))
            self.commit_xres()
        self.x0_is_input = False

    def swiglu_group(self, h, hT, nch, w1src, w3src, sring, hook=None):
        nc = self.nc
        PE, ACT, DVE = self.PE, self.ACT, self.DVE
        PE.rdep(h)
        hT.new_gen()
        for c in range(nch):
            if hook is not None and c == nch // 2:
                hook()
                PE.rdep(h)
            bks = []
            for src in (w1src, w3src):
                wb = self.wload(src(c))
                bk = self.next_bank()
                bk.new_gen()
                PE.wdep(bk)
                PE.rdep(wb)
                for k in range(KC):
                    ins = self.nc.tensor.matmul(bk.t[:, 0:G], lhsT=wb.t[:, k * 128:(k + 1) * 128], rhs=h.t[:, k, :],
                                                start=(k == 0), stop=(k == KC - 1))
                tok = self.pe_sig(ins)
                wb.r["pe"] = tok
                bk.w["pe"] = tok
                bks.append(bk)
            h.r["pe"] = tok
            s = sring.next()
            s.new_gen()
            ACT.wdep(s)
            ACT.rdep(bks[0])
            ins = nc.scalar.activation(out=s.t[:], in_=bks[0].t[:, 0:G], func=AF.Silu)
            tok = ACT.sig(ins)
            s.w["act"] = tok
            bks[0].r["act"] = tok
            DVE.rdep(s)
            DVE.rdep(bks[1])
            DVE.wdep(hT)
            ins = nc.vector.tensor_tensor(out=hT.t[:, c, :], in0=s.t[:], in1=bks[1].t[:, 0:G], op=ALU.mult)
            tok = DVE.sig(ins)
            s.r["dve"] = tok
            bks[1].r["dve"] = tok
            hT.w["dve"] = tok

    def phase_ffn(self):
        nc = self.nc
        self.new_xres_tok = {}
        nch = self.DFF // 128
        with ExitStack() as st:
            self.make_prep_bufs(st)
            self.make_resid_bufs(st)
            hnT = Ring([self.sb(f"fhnT{i}", [128, KC, G], BF16, st) for i in range(2)])
            hT = self.sb("fhT", [128, nch, G], BF16, st)
            sring = Ring([self.sb(f"fs{i}", [128, G], BF16, st) for i in range(3)])
            ngr = self.T // G
            nxt = {}

            def do_prep(g_):
                b_ = (g_ * G) // self.S
                A_, B_ = self.modcol(0, 1, b_)
                h_ = hnT.next()
                h_.new_gen()
                for t_ in range(G // 128):
                    self.prep(g_ * (G // 128) + t_, A_, B_, h_, t_ * 128)
                nxt[g_] = h_

            do_prep(0)
            for g in range(ngr):
                tile0 = g * (G // 128)
                h = nxt.pop(g)
                hook = (lambda g=g: do_prep(g + 1)) if g + 1 < ngr else None
                self.swiglu_group(h, hT, nch, lambda c: self.ffn_w1[c, :, :], lambda c: self.ffn_w3[c, :, :], sring, hook=hook)
                self.pass_B(hT, nch, lambda c_, half: self.ffn_w2[c_ * 128:(c_ + 1) * 128, half * 1024:(half + 1) * 1024],
                            G // 128, self.evac_resid(tile0, 0, 1))
            self.commit_xres()
        self.x0_is_input = False

    def phase_attn(self):
        nc = self.nc
        PE, ACT, DVE, POOL, SP = self.PE, self.ACT, self.DVE, self.POOL, self.SP
        S, T, NT = self.S, self.T, self.NT
        TPS = S // 128
        scale = float(HD) ** -0.5
        self.new_xres_tok = {}
        qkT_d = self.dram("qkT_d", [32, 128, T], BF16)
        v_d = self.dram("v_d", [T, D], BF16)
        o_d = self.dram("o_d", [T, D], BF16)
        cum_d = self.dram("cum_d", [NH, T], F32)
        with ExitStack() as st:
            self.make_prep_bufs(st)
            hnT = Ring([self.sb(f"ahnT{i}", [128, KC, G], BF16, st) for i in range(2)])
            qsb = Ring([self.sb(f"qsb{i}", [128, G], BF16, st) for i in range(3)])
            vsb = Ring([self.sb(f"vsb{i}", [128, 1024], BF16, st) for i in range(3)])
            wf = self.sb("wf", [128, KC, NH], BF16, st)
            bf_t = self.sb("bf_t", [128, NH], F32, st)
            one_c = self.sb("one_c", [128, 1], F32, st)
            tri = self.sb("tri", [128, 128], F32, st)
            ones = self.sb("ones", [128, 128], F32, st)
            lf = self.sb("lf", [128, NT, NH], F32, st)
            zt = Ring([self.sb(f"zt{i}", [128, NH], F32, st) for i in range(2)])
            cumt = Ring([self.sb(f"cumt{i}", [128, NH], F32, st) for i in range(2)])
            cumr = Ring([self.sb(f"cumr{i}", [NH, 128], F32, st) for i in range(2)])
            ins = nc.gpsimd.dma_start(out=wf.t[:], in_=self.fox_f.rearrange("(k p) n -> p k n", p=128))
            wf.w["dma"] = wf.dma_sig(ins)
            ins = nc.sync.dma_start(out=bf_t.t[:], in_=self.fox_b_f.partition_broadcast(128))
            bf_t.w["dma"] = bf_t.dma_sig(ins)
            nc.gpsimd.memset(one_c.t[:], 1.0)
            nc.gpsimd.memset(ones.t[:], 1.0)
            nc.gpsimd.memset(tri.t[:], 1.0)
            ins = nc.gpsimd.affine_select(out=tri.t[:], in_=tri.t[:], pattern=[[1, 128]], compare_op=ALU.is_ge, fill=0.0,
                                          base=0, channel_multiplier=-1)
            tok = POOL.sig(ins)
            tri.w["pool"] = tok
            ones.w["pool"] = tok
            one_c.w["pool"] = tok
            for g in range(T // G):
                tile0 = g * (G // 128)
                b = (g * G) // S
                A, B = self.modcol(1, 0, b)
                h = hnT.next()
                h.new_gen()
                for t in range(G // 128):
                    self.prep(tile0 + t, A, B, h, t * 128)
                PE.rdep(h)
                for ch in range(32):
                    wb = self.wload(self.fox_qk[ch, :, :])
                    bk = self.next_bank()
                    bk.new_gen()
                    PE.wdep(bk)
                    PE.rdep(wb)
                    for k in range(KC):
                        ins = nc.tensor.matmul(bk.t[:, 0:G], lhsT=wb.t[:, k * 128:(k + 1) * 128], rhs=h.t[:, k, :],
                                               start=(k == 0), stop=(k == KC - 1))
                    tok = self.pe_sig(ins)
                    wb.r["pe"] = tok
                    bk.w["pe"] = tok
                    q = qsb.next()
                    q.new_gen()
                    E = ACT if ch % 2 == 0 else DVE
                    E.wdep(q)
                    E.rdep(bk)
                    if E is ACT:
                        ins = nc.scalar.copy(out=q.t[:], in_=bk.t[:, 0:G])
                    else:
                        ins = nc.vector.tensor_copy(out=q.t[:], in_=bk.t[:, 0:G])
                    tok = E.sig(ins)
                    q.w[E.name] = tok
                    bk.r[E.name] = tok
                    SP.rdep(q)
                    ins = nc.sync.dma_start(out=qkT_d[ch, :, g * G:(g + 1) * G], in_=q.t[:])
                    tok = q.dma_sig(ins)
                    q.r["dma"] = tok
                    self.track_dma(tok)
                for t in range(G // 128):
                    tile = tile0 + t
                    bk = self.next_bank()
                    bk.new_gen()
                    PE.wdep(bk)
                    PE.rdep(wf)
                    for k in range(KC):
                        ins = nc.tensor.matmul(bk.t[:, 0:NH], lhsT=h.t[:, k, t * 128:(t + 1) * 128], rhs=wf.t[:, k, :],
                                               start=(k == 0), stop=(k == KC - 1))
                    tok = self.pe_sig(ins)
                    bk.w["pe"] = tok
                    z = zt.next()
                    z.new_gen()
                    DVE.wdep(z)
                    DVE.rdep(bk)
                    DVE.rdep(bf_t)
                    ins = nc.vector.tensor_tensor(out=z.t[:], in0=bk.t[:, 0:NH], in1=bf_t.t[:], op=ALU.add)
                    tok = DVE.sig(ins)
                    z.w["dve"] = tok
                    bk.r["dve"] = tok
                    ACT.rdep(z)
                    ACT.rdep(one_c)
                    ins = nc.scalar.activation(out=z.t[:], in_=z.t[:], func=AF.Exp, scale=-1.0)
                    ACT.wait(ACT.sig(ins))
                    ins = nc.scalar.activation(out=z.t[:], in_=z.t[:], func=AF.Ln, bias=one_c.t[:, 0:1])
                    ACT.wait(ACT.sig(ins))
                    ACT.waits(lf.prev)
                    ins = nc.scalar.mul(out=lf.t[:, tile, :], in_=z.t[:], mul=-1.0)
                    tok = ACT.sig(ins)
                    lf.w["act"] = tok
                    z.r["act"] = tok
                h.r["pe"] = self.last_pe_tok
                for t in range(G // 128):
                    tile = tile0 + t
                    seq0 = (tile // TPS) * TPS
                    bk = self.next_bank()
                    bk.new_gen()
                    PE.wdep(bk)
                    PE.rdep(lf)
                    PE.rdep(tri)
                    prevs = list(range(seq0, tile))
                    ins = nc.tensor.matmul(bk.t[:, 0:NH], lhsT=tri.t[:], rhs=lf.t[:, tile, :], start=True, stop=(len(prevs) == 0))
                    for i_, pt in enumerate(prevs):
                        ins = nc.tensor.matmul(bk.t[:, 0:NH], lhsT=ones.t[:], rhs=lf.t[:, pt, :], start=False,
                                               stop=(i_ == len(prevs) - 1))
                    tok = self.pe_sig(ins)
                    bk.w["pe"] = tok
                    lf.r["pe"] = tok
                    ct = cumt.next()
                    ct.new_gen()
                    DVE.wdep(ct)
                    DVE.rdep(bk)
                    ins = nc.vector.tensor_scalar(out=ct.t[:], in0=bk.t[:, 0:NH], scalar1=-1.0 / scale, scalar2=None, op0=ALU.mult)
                    tok = DVE.sig(ins)
                    ct.w["dve"] = tok
                    bk.r["dve"] = tok
                    bk2 = self.next_bank()
                    bk2.new_gen()
                    PE.wdep(bk2)
                    PE.rdep(ct)
                    PE.rdep(self.ident_f)
                    ins = nc.tensor.transpose(out=bk2.t[0:NH, 0:128], in_=ct.t[:], identity=self.ident_f.t[:])
                    tok = self.pe_sig(ins)
                    bk2.w["pe"] = tok
                    ct.r["pe"] = tok
                    cr = cumr.next()
                    cr.new_gen()
                    DVE.wdep(cr)
                    DVE.rdep(bk2)
                    ins = nc.vector.tensor_copy(out=cr.t[:], in_=bk2.t[0:NH, 0:128])
                    tok = DVE.sig(ins)
                    cr.w["dve"] = tok
                    bk2.r["dve"] = tok
                    SP.rdep(cr)
                    ins = nc.sync.dma_start(out=cum_d[:, tile * 128:(tile + 1) * 128], in_=cr.t[:])
                    tok = cr.dma_sig(ins)
                    cr.r["dma"] = tok
                    self.track_dma(tok)
                def evac_v(t, half, bks2, tile0=tile0):
                    vb = vsb.next()
                    vb.new_gen()
                    for n in range(2):
                        E = ACT if n == 0 else DVE
                        E.wdep(vb)
                        E.rdep(bks2[n])
                        if E is ACT:
                            ins = nc.scalar.copy(out=vb.t[:, n * 512:(n + 1) * 512], in_=bks2[n].t[:, :])
                        else:
                            ins = nc.vector.tensor_copy(out=vb.t[:, n * 512:(n + 1) * 512], in_=bks2[n].t[:, :])
                        tok = E.sig(ins)
                        vb.w[E.name] = tok
                        bks2[n].r[E.name] = tok
                    SP.rdep(vb)
                    ins = nc.sync.dma_start(out=v_d[(tile0 + t) * 128:(tile0 + t + 1) * 128, half * 1024:(half + 1) * 1024], in_=vb.t[:])
                    tok = vb.dma_sig(ins)
                    vb.r["dma"] = tok
                    self.track_dma(tok)
                self.pass_B(h, KC, lambda c_, half: self.fox_v[c_ * 128:(c_ + 1) * 128, half * 1024:(half + 1) * 1024],
                            G // 128, evac_v)
        self.barrier()
        with ExitStack() as st:
            qT = Ring([self.sb(f"qT{i}", [128, S], BF16, st) for i in range(2)])
            kT = Ring([self.sb(f"kT{i}", [128, S], BF16, st) for i in range(2)])
            vt = Ring([self.sb(f"vt{i}", [128, TPS, HD], BF16, st) for i in range(2)])
            bias = Ring([self.sb(f"abias{i}", [128, S], F32, st) for i in range(2)])
            Sb = Ring([self.sb(f"Sb{i}", [128, S], F32, st) for i in range(2)])
            Pb = Ring([self.sb(f"Pb{i}", [128, S], BF16, st) for i in range(4)])
            PT = Ring([self.sb(f"PT{i}", [128, TPS, 128], BF16, st) for i in range(2)])
            Oh = Ring([self.sb(f"Oh{i}", [128, TPS, HD], BF16, st) for i in range(2)])
            st_ring = Ring([self.sb(f"ast{i}", [128, 4], F32, st) for i in range(8)])
            trimask = self.sb("trimask", [128, 128], F32, st)
            bmr = Ring([self.sb(f"bm{i}", [128, TPS, 128], F32, st) for i in range(2)])
            nc.gpsimd.memset(trimask.t[:], 0.0)
            ins = nc.gpsimd.affine_select(out=trimask.t[:], in_=trimask.t[:], pattern=[[-1, 128]], compare_op=ALU.is_ge,
                                          fill=-1.0e30, base=0, channel_multiplier=1)
            trimask.w["pool"] = POOL.sig(ins)
            steps = [(seq, hh, i) for seq in range(self.NSEQ) for hh in range(NH) for i in range(TPS)]
            head_state = {}
            step_state = {}
            DEPTH = 2

            def a1(seq, hh, i):
                if i == 0:
                    q, kk, v, bs = qT.next(), kT.next(), vt.next(), bias.next()
                    for bf_, src in ((q, qkT_d[hh, :, seq * S:(seq + 1) * S]), (kk, qkT_d[16 + hh, :, seq * S:(seq + 1) * S]),
                                     (v, v_d[seq * S:(seq + 1) * S, hh * HD:(hh + 1) * HD].rearrange("(j p) d -> p j d", p=128)),
                                     (bs, cum_d[hh, seq * S:(seq + 1) * S].partition_broadcast(128))):
                        bf_.new_gen()
                        SP.wdep(bf_)
                        ins = nc.sync.dma_start(out=bf_.t[:], in_=src)
                        bf_.w["dma"] = bf_.dma_sig(ins)
                        self.track_dma(bf_.w["dma"])
                    bm = bmr.next()
                    bm.new_gen()
                    DVE.wdep(bm)
                    DVE.rdep(bs)
                    DVE.rdep(trimask)
                    for j in range(TPS):
                        ins = nc.vector.tensor_tensor(out=bm.t[:, j, :], in0=bs.t[:, j * 128:(j + 1) * 128], in1=trimask.t[:], op=ALU.add)
                    bm.w["dve"] = DVE.sig(ins)
                    oh = Oh.next()
                    oh.new_gen()
                    head_state[(seq, hh)] = (q, kk, v, bs, bm, oh)
                q, kk, v, bs, bm, oh = head_state[(seq, hh)]
                nk = (i + 1) * 128
                nkb = (nk + 511) // 512
                PE.rdep(q)
                PE.rdep(kk)
                bks = []
                for kb in range(nkb):
                    w = min(512, nk - kb * 512)
                    bk = self.next_bank()
                    bk.new_gen()
                    PE.wdep(bk)
                    ins = nc.tensor.matmul(bk.t[:, 0:w], lhsT=q.t[:, i * 128:(i + 1) * 128], rhs=kk.t[:, kb * 512:kb * 512 + w],
                                           start=True, stop=True)
                    bk.w["pe"] = self.pe_sig(ins)
                    bks.append((bk, kb, w))
                q.r["pe"] = self.last_pe_tok
                kk.r["pe"] = self.last_pe_tok
                step_state[(seq, hh, i)] = {"bks": bks}

            def a2(seq, hh, i):
                q, kk, v, bs, bm, oh = head_state[(seq, hh)]
                stt_ = step_state[(seq, hh, i)]
                nk = (i + 1) * 128
                sb_ = Sb.next()
                sb_.new_gen()
                DVE.wdep(sb_)
                DVE.rdep(bs)
                DVE.rdep(bm)
                for bk, kb, w in stt_["bks"]:
                    DVE.rdep(bk)
                    c0 = kb * 512
                    last = (c0 + w == nk)
                    wn = w - 128 if last else w
                    if wn > 0:
                        ins = nc.vector.tensor_tensor(out=sb_.t[:, c0:c0 + wn], in0=bk.t[:, 0:wn], in1=bs.t[:, c0:c0 + wn], op=ALU.add)
                        tok = DVE.sig(ins)
                    if last:
                        ins = nc.vector.tensor_tensor(out=sb_.t[:, c0 + wn:c0 + w], in0=bk.t[:, wn:w], in1=bm.t[:, i, :], op=ALU.add)
                        tok = DVE.sig(ins)
                    bk.r["dve"] = tok
                bs.r["dve"] = tok
                bm.r["dve"] = tok
                DVE.wait(tok)
                stt = st_ring.next()
                stt.new_gen()
                DVE.wdep(stt)
                ins = nc.vector.reduce_max(out=stt.t[:, 0:1], in_=sb_.t[:, 0:nk], axis=AX.X)
                DVE.wait(DVE.sig(ins))
                ins = nc.vector.tensor_scalar(out=stt.t[:, 1:2], in0=stt.t[:, 0:1], scalar1=-scale, scalar2=None, op0=ALU.mult)
                tok = DVE.sig(ins)
                stt.w["dve"] = tok
                sb_.w["dve"] = tok
                pb = Pb.next()
                pb.new_gen()
                ACT.wdep(pb)
                ACT.rdep(stt)
                ACT.rdep(sb_)
                ins = nc.scalar.memzero(stt.t[:, 2:3])
                ACT.wait(ACT.sig(ins))
                ins = nc.scalar.activation(out=pb.t[:, 0:nk], in_=sb_.t[:, 0:nk], func=AF.Exp, scale=scale, bias=stt.t[:, 1:2],
                                           accum_out=stt.t[:, 2:3])
                tok = ACT.sig(ins)
                pb.w["act"] = tok
                sb_.r["act"] = tok
                stt.w["act"] = tok
                stt_["pb"] = pb
                stt_["stt"] = stt

            def b_all(seq, hh, i):
                q, kk, v, bs, bm, oh = head_state[(seq, hh)]
                stt_ = step_state.pop((seq, hh, i))
                pb, stt = stt_["pb"], stt_["stt"]
                pt = PT.next()
                pt.new_gen()
                PE.rdep(pb)
                PE.rdep(self.ident_bf)
                nkt = i + 1
                for j0 in range(0, nkt, 8):
                    bk = self.next_bank()
                    bk.new_gen()
                    PE.wdep(bk)
                    pv = bk.t[:].bitcast(BF16)
                    cnt = min(8, nkt - j0)
                    for jj in range(cnt):
                        kt = j0 + jj
                        ins = nc.tensor.transpose(out=pv[:, jj * 128:(jj + 1) * 128], in_=pb.t[:, kt * 128:(kt + 1) * 128],
                                                  identity=self.ident_bf.t[:])
                    tok = self.pe_sig(ins)
                    bk.w["pe"] = tok
                    DVE.rdep(bk)
                    DVE.wdep(pt)
                    ins = nc.vector.tensor_copy(out=pt.t[:, j0:j0 + cnt, :], in_=pv[:, 0:cnt * 128].rearrange("p (j q) -> p j q", q=128))
                    tok = DVE.sig(ins)
                    bk.r["dve"] = tok
                    pt.w["dve"] = tok
                pb.r["pe"] = self.last_pe_tok
                bk = self.next_bank()
                bk.new_gen()
                PE.wdep(bk)
                PE.rdep(pt)
                PE.rdep(v)
                for kt in range(nkt):
                    ins = nc.tensor.matmul(bk.t[:, 0:HD], lhsT=pt.t[:, kt, :], rhs=v.t[:, kt, :], start=(kt == 0), stop=(kt == nkt - 1))
                tok = self.pe_sig(ins)
                bk.w["pe"] = tok
                pt.r["pe"] = tok
                v.r["pe"] = tok
                DVE.rdep(stt)
                ins = nc.vector.reciprocal(out=stt.t[:, 3:4], in_=stt.t[:, 2:3])
                stt.w["dve"] = DVE.sig(ins)
                ACT.rdep(stt)
                ACT.rdep(bk)
                ACT.wdep(oh)
                ins = nc.scalar.activation(out=oh.t[:, i, :], in_=bk.t[:, 0:HD], func=AF.Copy, scale=stt.t[:, 3:4])
                tok = ACT.sig(ins)
                bk.r["act"] = tok
                oh.w["act"] = tok
                stt.r["act"] = tok
                if i == TPS - 1:
                    SP.rdep(oh)
                    ins = nc.sync.dma_start(out=o_d[seq * S:(seq + 1) * S, hh * HD:(hh + 1) * HD].rearrange("(j p) d -> p j d", p=128),
                                            in_=oh.t[:])
                    tok = oh.dma_sig(ins)
                    oh.r["dma"] = tok
                    self.track_dma(tok)

            NS = len(steps)
            for n in range(min(DEPTH, NS)):
                a1(*steps[n])
                a2(*steps[n])
            for n in range(NS):
                if n + DEPTH < NS:
                    a1(*steps[n + DEPTH])
                b_all(*steps[n])
                if n + DEPTH < NS:
                    a2(*steps[n + DEPTH])
        self.barrier()
        with ExitStack() as st:
            self.make_resid_bufs(st)
            ot_ring = Ring([self.sb(f"otok{i}", [128, D], BF16, st) for i in range(2)])
            oT = Ring([self.sb(f"oT{i}", [128, KC, G], BF16, st) for i in range(2)])
            for g in range(T // G):
                tile0 = g * (G // 128)
                o_T = oT.next()
                o_T.new_gen()
                for t in range(G // 128):
                    ot = ot_ring.next()
                    ot.new_gen()
                    SP.wdep(ot)
                    ins = nc.sync.dma_start(out=ot.t[:], in_=o_d[(tile0 + t) * 128:(tile0 + t + 1) * 128, :])
                    ot.w["dma"] = ot.dma_sig(ins)
                    self.track_dma(ot.w["dma"])
                    PE.rdep(ot)
                    for j in range(2):
                        bk = self.next_bank()
                        bk.new_gen()
                        PE.wdep(bk)
                        pv = bk.t[:].bitcast(BF16)
                        for i in range(8):
                            k = 8 * j + i
                            ins = nc.tensor.transpose(out=pv[:, i * 128:(i + 1) * 128], in_=ot.t[:, k * 128:(k + 1) * 128],
                                                      identity=self.ident_bf.t[:])
                        tok = self.pe_sig(ins)
                        bk.w["pe"] = tok
                        ot.r["pe"] = tok
                        E = ACT if j == 0 else DVE
                        E.rdep(bk)
                        E.wdep(o_T)
                        for i in range(8):
                            k = 8 * j + i
                            if E is ACT:
                                ins = nc.scalar.copy(out=o_T.t[:, k, t * 128:(t + 1) * 128], in_=pv[:, i * 128:(i + 1) * 128])
                            else:
                                ins = nc.vector.tensor_copy(out=o_T.t[:, k, t * 128:(t + 1) * 128], in_=pv[:, i * 128:(i + 1) * 128])
                        tok = E.sig(ins)
                        bk.r[E.name] = tok
                        o_T.w[E.name] = tok
                self.pass_B(o_T, KC, lambda c_, half: self.fox_w_out[c_ * 128:(c_ + 1) * 128, half * 1024:(half + 1) * 1024],
                            G // 128, self.evac_resid(tile0, 1, 0))
            self.commit_xres()
        self.x0_is_input = False

    def phase_moe(self):
        nc = self.nc
        PE, ACT, DVE, POOL, SP = self.PE, self.ACT, self.DVE, self.POOL, self.SP
        NE, T, NT = self.NE, self.T, self.NT
        nch = self.DFFE // 128
        self.new_xres_tok = {}
        with ExitStack() as st:
            self.make_prep_bufs(st)
            self.make_resid_bufs(st)
            hnT = Ring([self.sb(f"mhnT{i}", [128, KC, G], BF16, st) for i in range(1)])
            hT = self.sb("mhT", [128, nch, G], BF16, st)
            sring = Ring([self.sb(f"ms{i}", [128, G], BF16, st) for i in range(3)])
            wr = self.sb("wr", [128, KC, NE], BF16, st)
            gates = self.sb("gates", [128, NT, NE], F32, st)
            lg = Ring([self.sb(f"lg{i}", [128, 4, NE], F32, st) for i in range(2)])
            sm = Ring([self.sb(f"sm{i}", [128, 8], F32, st) for i in range(2)])
            ins = nc.gpsimd.dma_start(out=wr.t[:], in_=self.moe_router.rearrange("(k p) n -> p k n", p=128))
            wr.w["dma"] = wr.dma_sig(ins)
            for g in range(T // G):
                tile0 = g * (G // 128)
                b = (g * G) // self.S
                A, B = self.modcol(1, 1, b)
                h = hnT.next()
                h.new_gen()
                for t in range(G // 128):
                    self.prep(tile0 + t, A, B, h, t * 128)
                PE.rdep(h)
                PE.rdep(wr)
                for t in range(G // 128):
                    tile = tile0 + t
                    bk = self.next_bank()
                    bk.new_gen()
                    PE.wdep(bk)
                    for k in range(KC):
                        ins = nc.tensor.matmul(bk.t[:, 0:NE], lhsT=h.t[:, k, t * 128:(t + 1) * 128], rhs=wr.t[:, k, :],
                                               start=(k == 0), stop=(k == KC - 1))
                    tok = self.pe_sig(ins)
                    bk.w["pe"] = tok
                    L = lg.next()
                    L.new_gen()
                    s_ = sm.next()
                    s_.new_gen()
                    DVE.wdep(L)
                    DVE.wdep(s_)
                    DVE.rdep(bk)
                    ins = nc.vector.tensor_copy(out=L.t[:, 0, :], in_=bk.t[:, 0:NE])
                    tok = DVE.sig(ins)
                    bk.r["dve"] = tok
                    DVE.wait(tok)
                    ins = nc.vector.reduce_max(out=s_.t[:, 0:1], in_=L.t[:, 0, :], axis=AX.X)
                    DVE.wait(DVE.sig(ins))
                    ins = nc.vector.tensor_scalar(out=L.t[:, 1, :], in0=L.t[:, 0, :], scalar1=s_.t[:, 0:1], scalar2=None, op0=ALU.is_equal)
                    DVE.wait(DVE.sig(ins))
                    ins = nc.vector.scalar_tensor_tensor(out=L.t[:, 2, :], in0=L.t[:, 1, :], scalar=-1.0e30, in1=L.t[:, 0, :],
                                                         op0=ALU.mult, op1=ALU.add)
                    DVE.wait(DVE.sig(ins))
                    ins = nc.vector.reduce_max(out=s_.t[:, 1:2], in_=L.t[:, 2, :], axis=AX.X)
                    DVE.wait(DVE.sig(ins))
                    ins = nc.vector.tensor_scalar(out=L.t[:, 3, :], in0=L.t[:, 2, :], scalar1=s_.t[:, 1:2], scalar2=None, op0=ALU.is_equal)
                    DVE.sig(ins)
                    ins = nc.vector.tensor_tensor(out=s_.t[:, 2:3], in0=s_.t[:, 1:2], in1=s_.t[:, 0:1], op=ALU.subtract)
                    tok = DVE.sig(ins)
                    s_.w["dve"] = tok
                    ACT.rdep(s_)
                    ins = nc.scalar.activation(out=s_.t[:, 3:4], in_=s_.t[:, 2:3], func=AF.Sigmoid, scale=-1.0)
                    ACT.sig(ins)
                    ins = nc.scalar.activation(out=s_.t[:, 4:5], in_=s_.t[:, 2:3], func=AF.Sigmoid, scale=1.0)
                    tok = ACT.sig(ins)
                    s_.w["act"] = tok
                    DVE.rdep(s_)
                    DVE.waits(gates.prev)
                    ins = nc.vector.tensor_scalar(out=gates.t[:, tile, :], in0=L.t[:, 1, :], scalar1=s_.t[:, 3:4], scalar2=None, op0=ALU.mult)
                    DVE.wait(DVE.sig(ins))
                    ins = nc.vector.scalar_tensor_tensor(out=gates.t[:, tile, :], in0=L.t[:, 3, :], scalar=s_.t[:, 4:5], in1=gates.t[:, tile, :],
                                                         op0=ALU.mult, op1=ALU.add)
                    tok = DVE.sig(ins)
                    gates.w["dve"] = tok
                    L.r["dve"] = tok
                    s_.r["dve"] = tok
                for e in range(NE):
                    self.swiglu_group(h, hT, nch, lambda c, e=e: self.moe_w1[e * nch + c, :, :], lambda c, e=e: self.moe_w3[e * nch + c, :, :], sring)
                    self.DVE.rdep(gates)
                    self.pass_B(hT, nch,
                                lambda c_, half, e=e: self.moe_w2[e * self.DFFE + c_ * 128: e * self.DFFE + (c_ + 1) * 128, half * 1024:(half + 1) * 1024],
                                G // 128, self.evac_resid(tile0, 1, 1, rowscale=lambda t, e=e, tile0=tile0: gates.t[:, tile0 + t, e:e + 1]))
                    self.commit_xres()
                    self.x0_is_input = False
            self.commit_xres()

    def phase_moe_sparse(self):
        nc = self.nc
        PE, ACT, DVE, POOL, SP = self.PE, self.ACT, self.DVE, self.POOL, self.SP
        NE, T, NT, S = self.NE, self.T, self.NT, self.S
        I32 = mybir.dt.int32
        nch = self.DFFE // 128
        NG = (2 * T) // G + NE - 1
        NSLOT = NG * G
        Xg = self.dram("Xg", [NSLOT, D], BF16)
        Yg = self.dram("Yg", [NSLOT, D], F32)
        hn_d = self.dram("hn_d", [T, D], BF16)
        w1tab = self.moe_w1.rearrange("c p f -> (c p) f")
        w3tab = self.moe_w3.rearrange("c p f -> (c p) f")
        w2tab = self.moe_w2.rearrange("r (h c) -> (r h) c", h=2)
        self.new_xres_tok = {}
        with ExitStack() as ph:
            a_bf = self.sb("a_bf", [128, NT, NE], BF16, ph)
            eq1f = self.sb("eq1f", [128, NT, NE], F32, ph)
            eq2f = self.sb("eq2f", [128, NT, NE], F32, ph)
            wts = self.sb("wts", [128, NT, 2], F32, ph)
            cnt = self.sb("cnt", [128, NT, NE], F32, ph)
            slots_f = self.sb("slots_f", [128, NT, 2], F32, ph)
            slots_i = self.sb("slots_i", [128, NT, 2], I32, ph)
            triS = self.sb("triS", [128, 128], BF16, ph)
            ones_bf = self.sb("ones_bf", [128, 128], BF16, ph)
            ntot = self.sb("ntot", [128, NE], F32, ph)
            padded = self.sb("padded", [128, NE], F32, ph)
            base = self.sb("base", [128, NE], F32, ph)
            endt = self.sb("endt", [128, NE], F32, ph)
            etab = self.sb("etab", [128, NG], F32, ph)
            iota_i = self.sb("iota_i", [128, nch], I32, ph)
            iota_f = self.sb("iota_f", [128, nch], F32, ph)
            wr = self.sb("wr", [128, KC, NE], BF16, ph)
            ins = nc.gpsimd.dma_start(out=wr.t[:], in_=self.moe_router.rearrange("(k p) n -> p k n", p=128))
            wr.w["dma"] = wr.dma_sig(ins)
            nc.gpsimd.memset(ones_bf.t[:], 1.0)
            nc.gpsimd.memset(triS.t[:], 1.0)
            nc.gpsimd.affine_select(out=triS.t[:], in_=triS.t[:], pattern=[[1, 128]], compare_op=ALU.is_ge, fill=0.0,
                                    base=-1, channel_multiplier=-1)
            ins = nc.gpsimd.iota(iota_i.t[:], pattern=[[128, nch]], base=0, channel_multiplier=1)
            tok = POOL.sig(ins)
            triS.w["pool"] = tok
            ones_bf.w["pool"] = tok
            iota_i.w["pool"] = tok
            DVE.rdep(iota_i)
            ins = nc.vector.tensor_copy(out=iota_f.t[:], in_=iota_i.t[:])
            iota_f.w["dve"] = DVE.sig(ins)
            with ExitStack() as st:
                xt_ring = Ring([self.sb(f"rxt{i}", [128, D], F32, st) for i in range(2)])
                hn_ring = Ring([self.sb(f"rhn{i}", [128, D], BF16, st) for i in range(2)])
                junk = self.sb("rjunk", [128, D], BF16, st)
                ss_ring = Ring([self.sb(f"rss{i}", [128, 2], F32, st) for i in range(4)])
                Arow = self.sb("Arow", [128, D], F32, st)
                Brow = self.sb("Brow", [128, D], F32, st)
                Trow = self.sb("Trow", [128, D], F32, st)
                hTr = Ring([self.sb(f"hTr{i}", [128, KC, 128], BF16, st) for i in range(2)])
                lg = Ring([self.sb(f"slg{i}", [128, 4, NE], F32, st) for i in range(2)])
                sm = Ring([self.sb(f"ssm{i}", [128, 8], F32, st) for i in range(2)])
                hn_tok = {}
                cur_b = None
                for tile in range(NT):
                    b = (tile * 128) // S
                    if b != cur_b:
                        cur_b = b
                        for bf_ in (Arow, Brow, Trow):
                            bf_.new_gen()
                            SP.wdep(bf_)
                        SP.wait(self.mod_ready)
                        ins = nc.sync.dma_start(out=Trow.t[:], in_=self.norm_ffn[1, :].partition_broadcast(128))
                        Trow.w["dma"] = Trow.dma_sig(ins)
                        ins = nc.sync.dma_start(out=Arow.t[:], in_=self.mod_d[1, b, 4 * D:5 * D].partition_broadcast(128))
                        Arow.w["dma"] = Arow.dma_sig(ins)
                        ins = nc.sync.dma_start(out=Brow.t[:], in_=self.mod_d[1, b, 3 * D:4 * D].partition_broadcast(128))
                        Brow.w["dma"] = Brow.dma_sig(ins)
                        DVE.rdep(Arow)
                        DVE.rdep(Trow)
                        DVE.rdep(Brow)
                        ins = nc.vector.scalar_tensor_tensor(out=Arow.t[:], in0=Arow.t[:], scalar=1.0, in1=Trow.t[:], op0=ALU.add, op1=ALU.mult)
                        tok = DVE.sig(ins)
                        Arow.w["dve"] = tok
                        Trow.r["dve"] = tok
                        DVE.wait(tok)
                    xt = xt_ring.next()
                    xt.new_gen()
                    SP.wdep(xt)
                    SP.waits([self.xres_tok.get((tile, 0)), self.xres_tok.get((tile, 1))])
                    src = self.x_in if self.x0_is_input else self.xres
                    ins = nc.sync.dma_start(out=xt.t[:], in_=src[tile * 128:(tile + 1) * 128, :])
                    xt.w["dma"] = xt.dma_sig(ins)
                    ss = ss_ring.next()
                    ss.new_gen()
                    junk.new_gen()
                    ACT.wdep(ss)
                    ACT.wdep(junk)
                    ACT.rdep(xt)
                    ins = nc.scalar.memzero(ss.t[:])
                    ACT.wait(ACT.sig(ins))
                    ins = nc.scalar.activation(out=junk.t[:], in_=xt.t[:], func=AF.Square, accum_out=ss.t[:, 0:1])
                    tok = ACT.sig(ins)
                    junk.w["act"] = tok
                    ACT.wait(tok)
                    ins = nc.scalar.activation(out=ss.t[:, 1:2], in_=ss.t[:, 0:1], func=AF.Sqrt, scale=1.0 / D, bias=self.eps_col.t[:, 0:1])
                    ss.w["act"] = ACT.sig(ins)
                    DVE.rdep(ss)
                    ins = nc.vector.reciprocal(out=ss.t[:, 1:2], in_=ss.t[:, 1:2])
                    DVE.wait(DVE.sig(ins))
                    DVE.rdep(xt)
                    ins = nc.vector.scalar_tensor_tensor(out=xt.t[:], in0=xt.t[:], scalar=ss.t[:, 1:2], in1=Arow.t[:], op0=ALU.mult, op1=ALU.mult)
                    tok = DVE.sig(ins)
                    ss.r["dve"] = tok
                    DVE.wait(tok)
                    hn = hn_ring.next()
                    hn.new_gen()
                    DVE.wdep(hn)
                    ins = nc.vector.tensor_tensor(out=hn.t[:], in0=xt.t[:], in1=Brow.t[:], op=ALU.add)
                    tok = DVE.sig(ins)
                    hn.w["dve"] = tok
                    xt.r["dve"] = tok
                    Arow.r["dve"] = tok
                    Brow.r["dve"] = tok
                    SP.rdep(hn)
                    ins = nc.sync.dma_start(out=hn_d[tile * 128:(tile + 1) * 128, :], in_=hn.t[:])
                    tok = hn.dma_sig(ins)
                    hn.r["dma"] = tok
                    hn_tok[tile] = tok
                    self.track_dma(tok)
                    hr = hTr.next()
                    hr.new_gen()
                    PE.rdep(hn)
                    PE.rdep(self.ident_bf)
                    for j in range(2):
                        bk = self.next_bank()
                        bk.new_gen()
                        PE.wdep(bk)
                        pv = bk.t[:].bitcast(BF16)
                        for i in range(8):
                            k = 8 * j + i
                            ins = nc.tensor.transpose(out=pv[:, i * 128:(i + 1) * 128], in_=hn.t[:, k * 128:(k + 1) * 128],
                                                      identity=self.ident_bf.t[:])
                        tok = self.pe_sig(ins)
                        bk.w["pe"] = tok
                        hn.r["pe"] = tok
                        E = ACT if j == 0 else DVE
                        E.rdep(bk)
                        E.wdep(hr)
                        if E is ACT:
                            ins = nc.scalar.copy(out=hr.t[:, 8 * j:8 * j + 8, :], in_=pv.rearrange("p (j q) -> p j q", q=128))
                        else:
                            ins = nc.vector.tensor_copy(out=hr.t[:, 8 * j:8 * j + 8, :], in_=pv.rearrange("p (j q) -> p j q", q=128))
                        tok = E.sig(ins)
                        bk.r[E.name] = tok
                        hr.w[E.name] = tok
                    bk = self.next_bank()
                    bk.new_gen()
                    PE.wdep(bk)
                    PE.rdep(hr)
                    PE.rdep(wr)
                    for k in range(KC):
                        ins = nc.tensor.matmul(bk.t[:, 0:NE], lhsT=hr.t[:, k, :], rhs=wr.t[:, k, :], start=(k == 0), stop=(k == KC - 1))
                    tok = self.pe_sig(ins)
                    bk.w["pe"] = tok
                    hr.r["pe"] = tok
                    L = lg.next()
                    L.new_gen()
                    s_ = sm.next()
                    s_.new_gen()
                    DVE.wdep(L)
                    DVE.wdep(s_)
                    DVE.rdep(bk)
                    ins = nc.vector.tensor_copy(out=L.t[:, 0, :], in_=bk.t[:, 0:NE])
                    tok = DVE.sig(ins)
                    bk.r["dve"] = tok
                    DVE.wait(tok)
                    ins = nc.vector.reduce_max(out=s_.t[:, 0:1], in_=L.t[:, 0, :], axis=AX.X)
                    DVE.wait(DVE.sig(ins))
                    ins = nc.vector.tensor_scalar(out=eq1f.t[:, tile, :], in0=L.t[:, 0, :], scalar1=s_.t[:, 0:1], scalar2=None, op0=ALU.is_equal)
                    DVE.wait(DVE.sig(ins))
                    ins = nc.vector.scalar_tensor_tensor(out=L.t[:, 2, :], in0=eq1f.t[:, tile, :], scalar=-1.0e30, in1=L.t[:, 0, :],
                                                         op0=ALU.mult, op1=ALU.add)
                    DVE.wait(DVE.sig(ins))
                    ins = nc.vector.reduce_max(out=s_.t[:, 1:2], in_=L.t[:, 2, :], axis=AX.X)
                    DVE.wait(DVE.sig(ins))
                    ins = nc.vector.tensor_scalar(out=eq2f.t[:, tile, :], in0=L.t[:, 2, :], scalar1=s_.t[:, 1:2], scalar2=None, op0=ALU.is_equal)
                    DVE.wait(DVE.sig(ins))
                    ins = nc.vector.tensor_tensor(out=a_bf.t[:, tile, :], in0=eq1f.t[:, tile, :], in1=eq2f.t[:, tile, :], op=ALU.add)
                    a_bf.w["dve"] = DVE.sig(ins)
                    ins = nc.vector.tensor_tensor(out=s_.t[:, 2:3], in0=s_.t[:, 1:2], in1=s_.t[:, 0:1], op=ALU.subtract)
                    tok = DVE.sig(ins)
                    s_.w["dve"] = tok
                    ACT.rdep(s_)
                    ins = nc.scalar.activation(out=wts.t[:, tile, 0:1], in_=s_.t[:, 2:3], func=AF.Sigmoid, scale=-1.0)
                    ACT.sig(ins)
                    ins = nc.scalar.activation(out=wts.t[:, tile, 1:2], in_=s_.t[:, 2:3], func=AF.Sigmoid, scale=1.0)
                    tok = ACT.sig(ins)
                    wts.w["act"] = tok
                    s_.r["act"] = tok
                    L.r["dve"] = a_bf.w["dve"]
                eq1f.w["dve"] = a_bf.w["dve"]
                eq2f.w["dve"] = a_bf.w["dve"]
                PE.rdep(a_bf)
                PE.rdep(triS)
                PE.rdep(ones_bf)
                for tile in range(NT):
                    bk = self.next_bank()
                    bk.new_gen()
                    PE.wdep(bk)
                    ins = nc.tensor.matmul(bk.t[:, 0:NE], lhsT=triS.t[:], rhs=a_bf.t[:, tile, :], start=True, stop=(tile == 0))
                    for pt in range(tile):
                        ins = nc.tensor.matmul(bk.t[:, 0:NE], lhsT=ones_bf.t[:], rhs=a_bf.t[:, pt, :], start=False, stop=(pt == tile - 1))
                    tok = self.pe_sig(ins)
                    bk.w["pe"] = tok
                    DVE.rdep(bk)
                    ins = nc.vector.tensor_copy(out=cnt.t[:, tile, :], in_=bk.t[:, 0:NE])
                    tok = DVE.sig(ins)
                    bk.r["dve"] = tok
                    cnt.w["dve"] = tok
                bk = self.next_bank()
                bk.new_gen()
                PE.wdep(bk)
                for tile in range(NT):
                    ins = nc.tensor.matmul(bk.t[:, 0:NE], lhsT=ones_bf.t[:], rhs=a_bf.t[:, tile, :], start=(tile == 0), stop=(tile == NT - 1))
                tok = self.pe_sig(ins)
                bk.w["pe"] = tok
                a_bf.r["pe"] = tok
                DVE.rdep(bk)
                ins = nc.vector.tensor_copy(out=ntot.t[:], in_=bk.t[:, 0:NE])
                tok = DVE.sig(ins)
                bk.r["dve"] = tok
                DVE.wait(tok)
                ins = nc.vector.memset(padded.t[:], 0.0)
                DVE.wait(DVE.sig(ins))
                for m in range(T // G):
                    ins = nc.vector.scalar_tensor_tensor(out=padded.t[:], in0=ntot.t[:], scalar=float(G * m), in1=padded.t[:],
                                                         op0=ALU.is_gt, op1=ALU.add)
                    DVE.wait(DVE.sig(ins))
                ins = nc.vector.tensor_scalar(out=padded.t[:], in0=padded.t[:], scalar1=float(G), scalar2=None, op0=ALU.mult)
                DVE.wait(DVE.sig(ins))
                ins = nc.vector.memset(base.t[:], 0.0)
                DVE.wait(DVE.sig(ins))
                for e in range(1, NE):
                    ins = nc.vector.tensor_tensor(out=base.t[:, e:e + 1], in0=base.t[:, e - 1:e], in1=padded.t[:, e - 1:e], op=ALU.add)
                    DVE.wait(DVE.sig(ins))
                ins = nc.vector.tensor_tensor(out=endt.t[:], in0=base.t[:], in1=padded.t[:], op=ALU.add)
                DVE.wait(DVE.sig(ins))
                tmp8 = lg.next()
                for gi in range(NG):
                    ins = nc.vector.tensor_scalar(out=tmp8.t[:, 0, :], in0=endt.t[:], scalar1=float(gi * G), scalar2=None, op0=ALU.is_le)
                    DVE.wait(DVE.sig(ins))
                    ins = nc.vector.reduce_sum(out=etab.t[:, gi:gi + 1], in_=tmp8.t[:, 0, :], axis=AX.X)
                    DVE.wait(DVE.sig(ins))
                ins = nc.vector.tensor_scalar(out=etab.t[:], in0=etab.t[:], scalar1=float(NE - 1), scalar2=None, op0=ALU.min)
                etab.w["dve"] = DVE.sig(ins)
                DVE.wait(etab.w["dve"])
                for tile in range(NT):
                    ins = nc.vector.tensor_tensor(out=tmp8.t[:, 1, :], in0=cnt.t[:, tile, :], in1=base.t[:], op=ALU.add)
                    DVE.wait(DVE.sig(ins))
                    ins = nc.vector.tensor_tensor(out=tmp8.t[:, 2, :], in0=tmp8.t[:, 1, :], in1=eq1f.t[:, tile, :], op=ALU.mult)
                    DVE.sig(ins)
                    ins = nc.vector.tensor_tensor(out=tmp8.t[:, 3, :], in0=tmp8.t[:, 1, :], in1=eq2f.t[:, tile, :], op=ALU.mult)
                    DVE.wait(DVE.sig(ins))
                    ins = nc.vector.reduce_sum(out=slots_f.t[:, tile, 0:1], in_=tmp8.t[:, 2, :], axis=AX.X)
                    DVE.sig(ins)
                    ins = nc.vector.reduce_sum(out=slots_f.t[:, tile, 1:2], in_=tmp8.t[:, 3, :], axis=AX.X)
                    DVE.wait(DVE.sig(ins))
                ins = nc.vector.tensor_copy(out=slots_i.t[:], in_=slots_f.t[:])
                slots_i.w["dve"] = DVE.sig(ins)
                POOL.rdep(slots_i)
                for tile in range(NT):
                    hn = hn_ring.next()
                    hn.new_gen()
                    SP.wdep(hn)
                    SP.wait(hn_tok[tile])
                    ins = nc.sync.dma_start(out=hn.t[:], in_=hn_d[tile * 128:(tile + 1) * 128, :])
                    hn.w["dma"] = hn.dma_sig(ins)
                    POOL.rdep(hn)
                    for j in range(2):
                        ins = nc.gpsimd.indirect_dma_start(out=Xg[:, :], out_offset=bass.IndirectOffsetOnAxis(ap=slots_i.t[:, tile, j:j + 1], axis=0),
                                                           in_=hn.t[:], in_offset=None)
                        tok = hn.dma_sig(ins)
                        hn.r["sc"] = tok
                        self.track_dma(tok)
            self.barrier()
            with ExitStack() as st:
                xg_ring = Ring([self.sb(f"xg{i}", [128, D], BF16, st) for i in range(2)])
                xT = Ring([self.sb(f"gxT{i}", [128, KC, G], BF16, st) for i in range(2)])
                hT = self.sb("ghT", [128, nch, G], BF16, st)
                sring = Ring([self.sb(f"gs{i}", [128, G], BF16, st) for i in range(3)])
                ysb = Ring([self.sb(f"ysb{i}", [128, 1024], F32, st) for i in range(3)])
                idxs = Ring([self.sb(f"idx{i}", [128, 3, nch], I32, st) for i in range(2)])
                idxf = self.sb("idxf", [128, nch], F32, st)
                ecol = self.sb("ecol", [128, 1], F32, st)
                for gi in range(NG):
                    ix = idxs.next()
                    ix.new_gen()
                    DVE.wdep(ix)
                    DVE.rdep(etab)
                    DVE.rdep(iota_f)
                    ins = nc.vector.tensor_scalar(out=ecol.t[:], in0=etab.t[:, gi:gi + 1], scalar1=float(self.DFFE), scalar2=None, op0=ALU.mult)
                    DVE.wait(DVE.sig(ins))
                    ins = nc.vector.tensor_scalar(out=idxf.t[:], in0=iota_f.t[:], scalar1=ecol.t[:, 0:1], scalar2=None, op0=ALU.add)
                    DVE.wait(DVE.sig(ins))
                    ins = nc.vector.tensor_copy(out=ix.t[:, 0, :], in_=idxf.t[:])
                    DVE.sig(ins)
                    ins = nc.vector.tensor_scalar(out=ix.t[:, 1, :], in0=idxf.t[:], scalar1=2.0, scalar2=None, op0=ALU.mult)
                    DVE.sig(ins)
                    ins = nc.vector.tensor_scalar(out=ix.t[:, 2, :], in0=idxf.t[:], scalar1=2.0, scalar2=1.0, op0=ALU.mult, op1=ALU.add)
                    tok = DVE.sig(ins)
                    ix.w["dve"] = tok
                    DVE.wait(tok)
                    x_T = xT.next()
                    x_T.new_gen()
                    for t in range(G // 128):
                        xg = xg_ring.next()
                        xg.new_gen()
                        SP.wdep(xg)
                        ins = nc.sync.dma_start(out=xg.t[:], in_=Xg[gi * G + t * 128: gi * G + (t + 1) * 128, :])
                        xg.w["dma"] = xg.dma_sig(ins)
                        PE.rdep(xg)
                        for j in range(2):
                            bk = self.next_bank()
                            bk.new_gen()
                            PE.wdep(bk)
                            pv = bk.t[:].bitcast(BF16)
                            for i in range(8):
                                k = 8 * j + i
                                ins = nc.tensor.transpose(out=pv[:, i * 128:(i + 1) * 128], in_=xg.t[:, k * 128:(k + 1) * 128],
                                                          identity=self.ident_bf.t[:])
                            tok = self.pe_sig(ins)
                            bk.w["pe"] = tok
                            xg.r["pe"] = tok
                            E = ACT if j == 0 else DVE
                            E.rdep(bk)
                            E.wdep(x_T)
                            if E is ACT:
                                ins = nc.scalar.copy(out=x_T.t[:, 8 * j:8 * j + 8, t * 128:(t + 1) * 128], in_=pv.rearrange("p (j q) -> p j q", q=128))
                            else:
                                ins = nc.vector.tensor_copy(out=x_T.t[:, 8 * j:8 * j + 8, t * 128:(t + 1) * 128], in_=pv.rearrange("p (j q) -> p j q", q=128))
                            tok = E.sig(ins)
                            bk.r[E.name] = tok
                            x_T.w[E.name] = tok
                    self.swiglu_group(x_T, hT, nch, lambda c, ix=ix: (w1tab, ix.t[:, 0, c:c + 1], ix),
                                      lambda c, ix=ix: (w3tab, ix.t[:, 0, c:c + 1], ix), sring)

                    def evac_y(t, half, bks2, gi=gi):
                        yb = ysb.next()
                        yb.new_gen()
                        for n in range(2):
                            E = ACT if n == 0 else DVE
                            E.wdep(yb)
                            E.rdep(bks2[n])
                            if E is ACT:
                                ins = nc.scalar.copy(out=yb.t[:, n * 512:(n + 1) * 512], in_=bks2[n].t[:, :])
                            else:
                                ins = nc.vector.tensor_copy(out=yb.t[:, n * 512:(n + 1) * 512], in_=bks2[n].t[:, :])
                            tok = E.sig(ins)
                            yb.w[E.name] = tok
                            bks2[n].r[E.name] = tok
                        SP.rdep(yb)
                        ins = nc.sync.dma_start(out=Yg[gi * G + t * 128: gi * G + (t + 1) * 128, half * 1024:(half + 1) * 1024], in_=yb.t[:])
                        tok = yb.dma_sig(ins)
                        yb.r["dma"] = tok
                        self.track_dma(tok)
                    self.pass_B(hT, nch, lambda c_, half, ix=ix: (w2tab, ix.t[:, 1 + half, c_:c_ + 1], ix), G // 128, evac_y)
            self.barrier()
            with ExitStack() as st:
                self.make_resid_bufs(st)
                xc = Ring([self.sb(f"cx{i}", [128, D], F32, st) for i in range(2)])
                y1r = Ring([self.sb(f"cy1{i}", [128, D], F32, st) for i in range(2)])
                y2r = Ring([self.sb(f"cy2{i}", [128, D], F32, st) for i in range(2)])
                fuse_final = "final" in self.phases
                if fuse_final:
                    ss_ring = Ring([self.sb(f"css{i}", [128, 2], F32, st) for i in range(4)])
                    cjunk = self.sb("cjunk", [128, D], BF16, st)
                    ins = nc.sync.dma_start(out=self.fin_g.t[:], in_=self.norm_final.partition_broadcast(128))
                    self.fin_g.w["dma"] = self.fin_g.dma_sig(ins)
                    self.track_dma(self.fin_g.w["dma"])
                    self.final_done = True
                for tile in range(NT):
                    b = (tile * 128) // S
                    self.load_gate(1, 1, b)
                    x_ = xc.next()
                    x_.new_gen()
                    SP.wdep(x_)
                    SP.waits([self.xres_tok.get((tile, 0)), self.xres_tok.get((tile, 1))])
                    src = self.x_in if self.x0_is_input else self.xres
                    ins = nc.sync.dma_start(out=x_.t[:], in_=src[tile * 128:(tile + 1) * 128, :])
                    x_.w["dma"] = x_.dma_sig(ins)
                    ys = []
                    for j, ring in enumerate((y1r, y2r)):
                        y_ = ring.next()
                        y_.new_gen()
                        POOL.wdep(y_)
                        ins = nc.gpsimd.indirect_dma_start(out=y_.t[:], out_offset=None, in_=Yg[:, :],
                                                           in_offset=bass.IndirectOffsetOnAxis(ap=slots_i.t[:, tile, j:j + 1], axis=0))
                        y_.w["dma"] = y_.dma_sig(ins)
                        ys.append(y_)
                    DVE.rdep(ys[0])
                    DVE.rdep(ys[1])
                    DVE.rdep(x_)
                    DVE.rdep(self.gate)
                    DVE.rdep(wts)
                    ins = nc.vector.tensor_scalar(out=ys[0].t[:], in0=ys[0].t[:], scalar1=wts.t[:, tile, 0:1], scalar2=None, op0=ALU.mult)
                    DVE.wait(DVE.sig(ins))
                    ins = nc.vector.scalar_tensor_tensor(out=ys[0].t[:], in0=ys[1].t[:], scalar=wts.t[:, tile, 1:2], in1=ys[0].t[:],
                                                         op0=ALU.mult, op1=ALU.add)
                    tok = DVE.sig(ins)
                    ys[1].r["dve"] = tok
                    DVE.wait(tok)
                    ins = nc.vector.tensor_tensor(out=ys[0].t[:], in0=ys[0].t[:], in1=self.gate.t[:], op=ALU.mult)
                    tok = DVE.sig(ins)
                    self.gate.r["dve"] = tok
                    DVE.wait(tok)
                    ins = nc.vector.tensor_tensor(out=x_.t[:], in0=x_.t[:], in1=ys[0].t[:], op=ALU.add)
                    tok = DVE.sig(ins)
                    ys[0].r["dve"] = tok
                    x_.w["dve"] = tok
                    if fuse_final:
                        ss = ss_ring.next()
                        ss.new_gen()
                        cjunk.new_gen()
                        ACT.wdep(ss)
                        ACT.wdep(cjunk)
                        ACT.rdep(x_)
                        ins = nc.scalar.memzero(ss.t[:])
                        ACT.wait(ACT.sig(ins))
                        ins = nc.scalar.activation(out=cjunk.t[:], in_=x_.t[:], func=AF.Square, accum_out=ss.t[:, 0:1])
                        tok = ACT.sig(ins)
                        cjunk.w["act"] = tok
                        ACT.wait(tok)
                        ins = nc.scalar.activation(out=ss.t[:, 1:2], in_=ss.t[:, 0:1], func=AF.Sqrt, scale=1.0 / D, bias=self.eps_col.t[:, 0:1])
                        ss.w["act"] = ACT.sig(ins)
                        DVE.rdep(ss)
                        ins = nc.vector.reciprocal(out=ss.t[:, 1:2], in_=ss.t[:, 1:2])
                        DVE.wait(DVE.sig(ins))
                        DVE.rdep(self.fin_g)
                        ins = nc.vector.scalar_tensor_tensor(out=x_.t[:], in0=x_.t[:], scalar=ss.t[:, 1:2], in1=self.fin_g.t[:], op0=ALU.mult, op1=ALU.mult)
                        tok = DVE.sig(ins)
                        ss.r["dve"] = tok
                        x_.w["dve"] = tok
                    SP.rdep(x_)
                    dst = self.out if fuse_final else self.xres
                    ins = nc.sync.dma_start(out=dst[tile * 128:(tile + 1) * 128, :], in_=x_.t[:])
                    tok = x_.dma_sig(ins)
                    x_.r["dma"] = tok
                    self.track_dma(tok)
                    self.new_xres_tok[(tile, 0)] = tok
                    self.new_xres_tok[(tile, 1)] = tok
                self.commit_xres()
        self.x0_is_input = False

    def phase_final(self):
        nc = self.nc
        PE, ACT, DVE, POOL, SP = self.PE, self.ACT, self.DVE, self.POOL, self.SP
        with ExitStack() as st:
            xt_ring = Ring([self.sb(f"fxt{i}", [128, D], F32, st) for i in range(3)])
            junk = self.sb("fjunk", [128, D], BF16, st)
            ss_ring = Ring([self.sb(f"fss{i}", [128, 2], F32, st) for i in range(4)])
            fg = self.fin_g
            ins = nc.sync.dma_start(out=fg.t[:], in_=self.norm_final.partition_broadcast(128))
            fg.w["dma"] = fg.dma_sig(ins)
            self.track_dma(fg.w["dma"])
            for tile in range(self.NT):
                xt = xt_ring.next()
                xt.new_gen()
                SP.wdep(xt)
                if self.x0_is_input:
                    srcap = self.x_in[tile * 128:(tile + 1) * 128, :]
                else:
                    srcap = self.xres[tile * 128:(tile + 1) * 128, :]
                    SP.waits([self.xres_tok.get((tile, 0)), self.xres_tok.get((tile, 1))])
                ins = nc.sync.dma_start(out=xt.t[:], in_=srcap)
                xt.w["dma"] = xt.dma_sig(ins)
                ss = ss_ring.next()
                ss.new_gen()
                junk.new_gen()
                ACT.wdep(ss)
                ACT.wdep(junk)
                ACT.rdep(xt)
                ins = nc.scalar.memzero(ss.t[:])
                ACT.wait(ACT.sig(ins))
                ins = nc.scalar.activation(out=junk.t[:], in_=xt.t[:], func=AF.Square, accum_out=ss.t[:, 0:1])
                tok = ACT.sig(ins)
                ss.w["act"] = tok
                junk.w["act"] = tok
                ACT.wait(tok)
                ins = nc.scalar.activation(out=ss.t[:, 1:2], in_=ss.t[:, 0:1], func=AF.Sqrt, scale=1.0 / D, bias=self.eps_col.t[:, 0:1])
                ss.w["act"] = ACT.sig(ins)
                DVE.rdep(ss)
                ins = nc.vector.reciprocal(out=ss.t[:, 1:2], in_=ss.t[:, 1:2])
                DVE.wait(DVE.sig(ins))
                DVE.rdep(fg)
                DVE.rdep(xt)
                ins = nc.vector.scalar_tensor_tensor(out=xt.t[:], in0=xt.t[:], scalar=ss.t[:, 1:2], in1=fg.t[:], op0=ALU.mult, op1=ALU.mult)
                tok = DVE.sig(ins)
                ss.r["dve"] = tok
                xt.w["dve"] = tok
                SP.rdep(xt)
                ins = nc.sync.dma_start(out=self.out[tile * 128:(tile + 1) * 128, :], in_=xt.t[:])
                tok = xt.dma_sig(ins)
                xt.r["dma"] = tok
                self.track_dma(tok)


def relayout_A(w, nchunks):
    w = np.asarray(w, dtype=np.float32)
    return np.ascontiguousarray(w.reshape(KC, 128, nchunks, 128).transpose(2, 1, 0, 3)).reshape(nchunks, 128, D)


def prepare_inputs(inp, NE=8):
    shared = {}
    shared["ada_w"] = np.ascontiguousarray(inp["ada_w"], dtype=np.float32)
    shared["ada_b"] = np.ascontiguousarray(inp["ada_b"], dtype=np.float32)
    shared["norm_mix"] = np.ascontiguousarray(inp["norm_mix"], dtype=np.float32)
    shared["norm_ffn"] = np.ascontiguousarray(inp["norm_ffn"], dtype=np.float32)
    shared["norm_final"] = np.ascontiguousarray(inp["norm_final"], dtype=np.float32)
    shared["conv_w_in"] = relayout_A(inp["conv_w_in"][0], 48)
    shared["conv_kernel"] = np.ascontiguousarray(inp["conv_kernel"][0], dtype=np.float32)
    shared["conv_w_out"] = np.ascontiguousarray(inp["conv_w_out"][0], dtype=np.float32)
    fw = np.asarray(inp["fox_w_in"][0], dtype=np.float32)
    shared["fox_qk"] = relayout_A(fw[:, 0:2 * D], 32)
    shared["fox_v"] = np.ascontiguousarray(fw[:, 2 * D:3 * D])
    shared["fox_f"] = np.ascontiguousarray(fw[:, 3 * D:3 * D + NH])
    shared["fox_b_f"] = np.ascontiguousarray(inp["fox_b_f"][0], dtype=np.float32)
    shared["fox_w_out"] = np.ascontiguousarray(inp["fox_w_out"][0], dtype=np.float32)
    dff = inp["ffn_w1"].shape[-1]
    shared["ffn_w1"] = relayout_A(inp["ffn_w1"][0], dff // 128)
    shared["ffn_w3"] = relayout_A(inp["ffn_w3"][0], dff // 128)
    shared["ffn_w2"] = np.ascontiguousarray(inp["ffn_w2"][0], dtype=np.float32)
    shared["moe_router"] = np.ascontiguousarray(inp["moe_router"][0][:, :NE], dtype=np.float32)
    dffe = inp["moe_w1"].shape[-1]
    shared["moe_w1"] = np.concatenate([relayout_A(inp["moe_w1"][0][e], dffe // 128) for e in range(NE)], axis=0)
    shared["moe_w3"] = np.concatenate([relayout_A(inp["moe_w3"][0][e], dffe // 128) for e in range(NE)], axis=0)
    shared["moe_w2"] = np.ascontiguousarray(np.asarray(inp["moe_w2"][0][:NE], dtype=np.float32).reshape(NE * dffe, D))
    return shared


def kernel(**inputs):
    x = np.asarray(inputs["x"], dtype=np.float32)
    c = np.asarray(inputs["c"], dtype=np.float32)
    ncores = 8
    bsz, S, _ = x.shape
    nseq = bsz // ncores
    shared = prepare_inputs(inputs)
    kb = KB(NSEQ=nseq, S=S)
    nc = kb.build()
    in_maps = []
    for i in range(ncores):
        m = dict(shared)
        m["x"] = np.ascontiguousarray(x[i * nseq:(i + 1) * nseq].reshape(nseq * S, D))
        m["c"] = np.ascontiguousarray(c[i * nseq:(i + 1) * nseq])
        in_maps.append(m)
    res = run_bass_kernel_spmd(nc, in_maps, core_ids=list(range(ncores)))
    out = np.concatenate([r["out"].reshape(nseq, S, D) for r in res.results], axis=0)
    return out.astype(np.float32, copy=False)
```
